# Optimizing a Trainium2 kernel written in Bass

```python
import math
import jax, jax.numpy as jnp
from jax import lax
import numpy as np

D_MODEL = 1024
BATCH = 8
SEQ = 8192
DEPTH = 2

CHUNK = 64
N_MEM = 256
D_MIX = D_MODEL
D_GLA = D_MIX // 2
D_SGU = D_MIX - D_GLA
GLA_HEADS = 4
D_QK = D_GLA // 2
GLA_DK = D_QK // GLA_HEADS
GLA_DV = D_GLA // GLA_HEADS
GATE_RANK = 16
GATE_TEMP = 16.0
SGU_GROUPS = 4
SGU_BLOCK = 128
SGU_CH = D_SGU // SGU_GROUPS
XATTN_HEADS = 4
XATTN_DH = D_MODEL // XATTN_HEADS
N_EXPERTS = 16
N_EXPERT_GROUPS = 4
EXPERTS_PER_GROUP = N_EXPERTS // N_EXPERT_GROUPS
TOP_K = 2
D_EXPERT = D_MODEL // 2
DN_ALPHA = (2.0 * DEPTH) ** 0.25
DN_BETA = (8.0 * DEPTH) ** -0.25
LN_EPS = 1e-5
RMS_EPS = 1e-6
D_IN = 2 * D_QK + 2 * D_GLA + GATE_RANK + 2 * D_SGU

kernel_name = "hybrid_gla_gmlp_memxattn_groupmoe_deepnorm"


def layer_norm(x, g, b):
    xf = x.astype(jnp.float32)
    mu = jnp.mean(xf, axis=-1, keepdims=True)
    var = jnp.mean(jnp.square(xf - mu), axis=-1, keepdims=True)
    y = (xf - mu) * lax.rsqrt(var + LN_EPS)
    return (y * g.astype(jnp.float32) + b.astype(jnp.float32)).astype(x.dtype)


def split_proj(p):
    sizes = (D_QK, D_QK, D_GLA, D_GLA, GATE_RANK, D_SGU, D_SGU)
    outs, start = [], 0
    for s in sizes:
        outs.append(p[..., start:start + s])
        start += s
    return outs


def gla_group(q, k, v, r, a_low, w_a2, b_a, gn_g):
    B, S, _ = q.shape
    nc = S // CHUNK
    log_a = jax.nn.log_sigmoid((a_low @ w_a2 + b_a).astype(jnp.float32)) / GATE_TEMP

    def to_chunks(t, d):
        return t.astype(jnp.float32).reshape(B, nc, CHUNK, GLA_HEADS, d).transpose(1, 0, 3, 2, 4)

    qc = to_chunks(q, GLA_DK) * (GLA_DK ** -0.5)
    kc = to_chunks(k, GLA_DK)
    vc = to_chunks(v, GLA_DV)
    gc = to_chunks(log_a, GLA_DK)

    def step(state, inp):
        q_c, k_c, v_c, g_c = inp
        bcum = jnp.cumsum(g_c, axis=2)
        b_last = bcum[:, :, -1:, :]
        k_dec = k_c * jnp.exp(b_last - bcum)
        state = jnp.exp(b_last[:, :, 0, :])[..., None] * state + jnp.einsum('bhck,bhcv->bhkv', k_dec, v_c)
        o = jnp.einsum('bhck,bhkv->bhcv', q_c, state)
        return state, o

    s0 = jnp.zeros((B, GLA_HEADS, GLA_DK, GLA_DV), jnp.float32)
    _, o = lax.scan(step, s0, (qc, kc, vc, gc))
    o = o.transpose(1, 0, 3, 2, 4).reshape(B, S, GLA_HEADS, GLA_DV)
    o = o * lax.rsqrt(jnp.mean(jnp.square(o), axis=-1, keepdims=True) + RMS_EPS)
    o = o.reshape(B, S, D_GLA) * gn_g.astype(jnp.float32)
    out = o * jax.nn.silu(r.astype(jnp.float32))
    return out.astype(q.dtype)


def sgu_group(u, v, w_s, b_s, ln_g, ln_b):
    B, S, _ = u.shape
    nb = S // SGU_BLOCK
    u = jax.nn.gelu(u)
    v = jax.nn.gelu(v).reshape(B, S, SGU_GROUPS, SGU_CH)
    v = layer_norm(v, ln_g.reshape(SGU_GROUPS, SGU_CH), ln_b.reshape(SGU_GROUPS, SGU_CH))
    v = v.reshape(B, nb, SGU_BLOCK, SGU_GROUPS, SGU_CH)
    pos = jnp.arange(SGU_BLOCK)
    mask = (pos[None, :] // CHUNK) <= (pos[:, None] // CHUNK)
    w_m = jnp.where(mask[None], w_s, jnp.zeros_like(w_s))
    mixed = jnp.einsum('gts,bnsgc->bntgc', w_m, v) + b_s.T[None, None, :, :, None]
    out = u.reshape(B, nb, SGU_BLOCK, SGU_GROUPS, SGU_CH) * mixed
    return out.reshape(B, S, D_SGU)


def mem_xattn(x, mem, wq, wk, wv, wo):
    B, S, _ = x.shape
    M = mem.shape[1]
    q = (x @ wq).reshape(B, S, XATTN_HEADS, XATTN_DH)
    k = (mem @ wk).reshape(B, M, XATTN_HEADS, XATTN_DH)
    v = (mem @ wv).reshape(B, M, XATTN_HEADS, XATTN_DH)
    s = jnp.einsum('bshd,bmhd->bhsm', q, k).astype(jnp.float32) * (XATTN_DH ** -0.5)
    p = jax.nn.softmax(s, axis=-1).astype(v.dtype)
    o = jnp.einsum('bhsm,bmhd->bshd', p, v).reshape(B, S, D_MODEL)
    return o @ wo


def grouped_moe(x, w_router, b_router, w_gate, w_up, w_down):
    B, S, D = x.shape
    xt = x.reshape(B * S, D)
    scores = jax.nn.softmax((xt @ w_router + b_router).astype(jnp.float32), axis=-1)
    grp = scores.reshape(-1, N_EXPERT_GROUPS, EXPERTS_PER_GROUP)
    group_score = jnp.sum(lax.top_k(grp, TOP_K)[0], axis=-1)
    g_sel = jnp.argmax(group_score, axis=-1)
    in_grp = jnp.sum(grp * jax.nn.one_hot(g_sel, N_EXPERT_GROUPS, dtype=jnp.float32)[:, :, None], axis=1)
    top_w, top_i = lax.top_k(in_grp, TOP_K)
    top_w = top_w / jnp.sum(top_w, axis=-1, keepdims=True)
    eid = g_sel[:, None] * EXPERTS_PER_GROUP + top_i
    gate = jnp.sum(jax.nn.one_hot(eid, N_EXPERTS, dtype=jnp.float32) * top_w[..., None], axis=1)
    gate = gate.astype(x.dtype)
    y = jnp.zeros_like(xt)
    for e in range(N_EXPERTS):
        h = jax.nn.silu(xt @ w_gate[e]) * (xt @ w_up[e])
        y = y + gate[:, e:e + 1] * (h @ w_down[e])
    return y.reshape(B, S, D)


def setup_inputs(seed: int = 0) -> dict:
    key = jax.random.key(seed)
    ks = jax.random.split(key, 24)
    f32 = jnp.float32

    def nrm(k, shape, fan_in, scale=1.0):
        return jax.random.normal(k, shape, f32) * (scale * fan_in ** -0.5)

    def gain(k, shape):
        return 1.0 + 0.02 * jax.random.normal(k, shape, f32)

    return {
        "x": jax.random.normal(ks[0], (BATCH, SEQ, D_MODEL), f32),
        "mem": jax.random.normal(ks[1], (BATCH, N_MEM, D_MODEL), f32),
        "w_in": nrm(ks[2], (DEPTH, D_MODEL, D_IN), D_MODEL),
        "w_a2": nrm(ks[3], (DEPTH, GATE_RANK, D_QK), GATE_RANK),
        "b_a": 1.0 + 0.5 * jax.random.normal(ks[4], (DEPTH, D_QK), f32),
        "gla_norm_g": gain(ks[5], (DEPTH, D_GLA)),
        "w_s": nrm(ks[6], (DEPTH, SGU_GROUPS, SGU_BLOCK, SGU_BLOCK), SGU_BLOCK),
        "b_s": gain(ks[7], (DEPTH, SGU_GROUPS, SGU_BLOCK)),
        "sgu_ln_g": gain(ks[8], (DEPTH, D_SGU)),
        "sgu_ln_b": 0.02 * jax.random.normal(ks[9], (DEPTH, D_SGU), f32),
        "w_out": nrm(ks[10], (DEPTH, D_MIX, D_MODEL), D_MIX, DN_BETA),
        "wq_x": nrm(ks[11], (DEPTH, D_MODEL, D_MODEL), D_MODEL),
        "wk_x": nrm(ks[12], (DEPTH, D_MODEL, D_MODEL), D_MODEL),
        "wv_x": nrm(ks[13], (DEPTH, D_MODEL, D_MODEL), D_MODEL),
        "wo_x": nrm(ks[14], (DEPTH, D_MODEL, D_MODEL), D_MODEL, DN_BETA),
        "w_router": nrm(ks[15], (D_MODEL, N_EXPERTS), D_MODEL),
        "b_router": 0.01 * jax.random.normal(ks[16], (N_EXPERTS,), f32),
        "w_gate": nrm(ks[17], (DEPTH, N_EXPERTS, D_MODEL, D_EXPERT), D_MODEL),
        "w_up": nrm(ks[18], (DEPTH, N_EXPERTS, D_MODEL, D_EXPERT), D_MODEL),
        "w_down": nrm(ks[19], (DEPTH, N_EXPERTS, D_EXPERT, D_MODEL), D_EXPERT, DN_BETA),
        "ln_g": gain(ks[20], (DEPTH, 3, D_MODEL)),
        "ln_b": 0.02 * jax.random.normal(ks[21], (DEPTH, 3, D_MODEL), f32),
    }


def reference(x, mem, w_in, w_a2, b_a, gla_norm_g, w_s, b_s, sgu_ln_g, sgu_ln_b, w_out,
              wq_x, wk_x, wv_x, wo_x, w_router, b_router, w_gate, w_up, w_down, ln_g, ln_b):
    for l in range(DEPTH):
        q, k, v, r, a_low, u, v_sgu = split_proj(x @ w_in[l])
        y_gla = gla_group(q, k, v, r, a_low, w_a2[l], b_a[l], gla_norm_g[l])
        y_sgu = sgu_group(u, v_sgu, w_s[l], b_s[l], sgu_ln_g[l], sgu_ln_b[l])
        h = jnp.concatenate([y_gla, y_sgu], axis=-1) @ w_out[l]
        x = layer_norm(DN_ALPHA * x + h, ln_g[l, 0], ln_b[l, 0])
        h = mem_xattn(x, mem, wq_x[l], wk_x[l], wv_x[l], wo_x[l])
        x = layer_norm(DN_ALPHA * x + h, ln_g[l, 1], ln_b[l, 1])
        h = grouped_moe(x, w_router, b_router, w_gate[l], w_up[l], w_down[l])
        x = layer_norm(DN_ALPHA * x + h, ln_g[l, 2], ln_b[l, 2])
    return x
```

```python
import numpy as np
from contextlib import ExitStack
import concourse.bass as bass
import concourse.mybir as mybir
from concourse.bass_utils import run_bass_kernel_spmd

F32 = mybir.dt.float32
BF16 = mybir.dt.bfloat16
AF = mybir.ActivationFunctionType
ALU = mybir.AluOpType
AX = mybir.AxisListType

D = 1024
NCH = 8
TT = 512
DIN = 2576
NEXP = 16
DEPTH = 2
NMEM = 256
ALPHA = float((2.0 * DEPTH) ** 0.25)
LN_EPS = 1e-5
RMS_EPS = 1e-6
RING = 6
SAME_ENG_SYNC = True


class Ctx:
    def __init__(self, name, sem, step):
        self.name = name
        self.sem = sem
        self.step = step
        self.count = 0


class Eng(Ctx):
    def __init__(self, name, sem):
        super().__init__(name, sem, 1)
        self.ops = []
        self.waited = {}


class Buf:
    def __init__(self, name):
        self.name = name
        self.last_write = None
        self.reads = []


def _compact(lst):
    best = {}
    for (c, v) in lst:
        if v > best.get(c, 0):
            best[c] = v
    return list(best.items())


class Tracker:
    def __init__(self):
        self.nwaits = 0
        self.nops = 0

    def _need(self, eng, deps):
        best = {}
        for (c, v) in deps:
            if c is eng and (not SAME_ENG_SYNC or eng.name == "pe" or v > eng.count):
                continue
            if v > best.get(c, 0):
                best[c] = v
        for c, v in best.items():
            if eng.waited.get(c, 0) >= v:
                continue
            eng.waited[c] = v
            eng.ops.append(("wait", c, v))
            self.nwaits += 1

    @staticmethod
    def _deps(reads, writes):
        deps = []
        for b in reads:
            if b.last_write is not None:
                deps.append(b.last_write)
        for b in writes:
            if b.last_write is not None:
                deps.append(b.last_write)
            deps.extend(b.reads)
        return deps

    def op(self, eng, fn, reads=(), writes=(), inc=True):
        self._need(eng, self._deps(reads, writes))
        val = eng.count + 1
        if inc:
            eng.count = val
        eng.ops.append(("op", fn, inc))
        self.nops += 1
        for b in reads:
            b.reads.append((eng, val))
            if len(b.reads) > 48:
                b.reads = _compact(b.reads)
        for b in writes:
            b.last_write = (eng, val)
            b.reads = []

    def dma(self, queue, ctx, fn, reads=(), writes=(), serial=False):
        deps = self._deps(reads, writes)
        if serial and ctx.count > 0:
            deps.append((ctx, ctx.count))
        self._need(queue, deps)
        ctx.count += 16
        val = ctx.count
        queue.ops.append(("dma", fn, ctx))
        self.nops += 1
        for b in reads:
            b.reads.append((ctx, val))
        for b in writes:
            b.last_write = (ctx, val)
            b.reads = []

    def wait_all(self, eng, ctxs):
        for c in ctxs:
            if c.count > 0 and eng.waited.get(c, 0) < c.count:
                eng.waited[c] = c.count
                eng.ops.append(("wait", c, c.count))

    @staticmethod
    def replay(eng, handle):
        for o in eng.ops:
            if o[0] == "wait":
                handle.wait_ge(o[1].sem, o[2])
            elif o[0] == "op":
                ins = o[1](handle)
                if o[2]:
                    ins.then_inc(eng.sem, 1)
            else:
                ins = o[1](handle)
                ins.then_inc(o[2].sem, 16)


def build_nc(S, depth=DEPTH, dbg=None):
    NT = S // TT
    nc = bass.Bass("TRN2", target_bir_lowering=False)

    def din(name, shape):
        return nc.dram_tensor(name, list(shape), F32, kind="ExternalInput").ap()

    xT = din("xT", [D, S])
    memT = din("memT", [D, NMEM])
    w_in = din("w_in", [DEPTH, D, DIN])
    w_a2 = din("w_a2", [DEPTH, 16, 256])
    b_a = din("b_a", [DEPTH, 1, 256])
    gng_t = din("gng_t", [DEPTH, 128, 4])
    w_sT = din("w_sT", [DEPTH, 4, 128, 128])
    b_s = din("b_s", [DEPTH, 1, 512])
    slng_t = din("slng_t", [DEPTH, 128, 4])
    slnb = din("slnb", [DEPTH, 1, 512])
    w_out = din("w_out", [DEPTH, D, D])
    wq_x = din("wq_x", [DEPTH, D, D])
    wk_x = din("wk_x", [DEPTH, D, D])
    wv_x = din("wv_x", [DEPTH, D, D])
    wo_x = din("wo_x", [DEPTH, D, D])
    w_router = din("w_router", [D, NEXP])
    b_router = din("b_router", [1, NEXP])
    w_gate = din("w_gate", [DEPTH, NEXP, D, 512])
    w_up = din("w_up", [DEPTH, NEXP, D, 512])
    w_down = din("w_down", [DEPTH, NEXP, 512, D])
    lng_t = din("lng_t", [128, DEPTH * 3 * NCH])
    lnb_t = din("lnb_t", [128, DEPTH * 3 * NCH])
    yT = nc.dram_tensor("yT", [D, S], F32, kind="ExternalOutput").ap()
    dbg_out = None
    if dbg is not None:
        dbg_out = nc.dram_tensor("dbg", [D, TT], F32, kind="ExternalOutput").ap()

    UPL = 5 + 2 + 2 + 2 + 2 + 2 + 3 * NEXP
    NU = depth * UPL
    wsc = nc.dram_tensor("wscratch", [NU, 128, 4096], BF16, kind="Internal").ap()

    T = Tracker()
    es = ExitStack()
    with es:
        def sb(name, shape, dt=F32):
            return es.enter_context(nc.sbuf_tensor(name, list(shape), dt))

        def sem(name):
            return es.enter_context(nc.semaphore(name))

        PE = Eng("pe", sem("s_pe"))
        ACT = Eng("act", sem("s_act"))
        DVE = Eng("dve", sem("s_dve"))
        POOL = Eng("pool", sem("s_pool"))
        SP = Eng("sp", sem("s_sp"))
        all_ctx = []

        def newctx(name):
            c = Ctx(name, sem("s_" + name), 16)
            all_ctx.append(c)
            return c

        banks = []
        for i in range(8):
            t = es.enter_context(nc.psum_tensor("bank%d" % i, [128, 512], F32))
            banks.append((t, Buf("bank%d" % i)))

        class Rot:
            def __init__(self, ids):
                self.ids = ids
                self.i = 0

            def next(self):
                b = banks[self.ids[self.i % len(self.ids)]]
                self.i += 1
                return b

        ARENA = 66560
        arena = sb("arena", [128, ARENA], mybir.dt.uint8)
        arena_bufs = []

        class Arena:
            def __init__(self):
                self.off = 0
                self.prev = []
                self.cur = []

            def reset(self):
                self.prev = self.prev + self.cur
                hz = []
                for b in self.prev:
                    if b.last_write is not None:
                        hz.append(b.last_write)
                    hz.extend(b.reads)
                self.hz = _compact(hz)
                self.prev = []
                self.cur = []
                self.off = 0
                self.offs = {}

            def tile(self, name, free_shape, dt=F32):
                n = 1
                for s in free_shape:
                    n *= s
                nbytes = n * (4 if dt == F32 else 2)
                nbytes = (nbytes + 63) // 64 * 64
                assert self.off + nbytes <= ARENA, (name, self.off, nbytes)
                v = arena[:, self.off:self.off + nbytes].bitcast(dt)
                if dt == F32:
                    v = arena[:, self.off:self.off + nbytes].bitcast(F32)
                self.off += nbytes
                v = v[:, 0:n]
                if len(free_shape) == 2:
                    v = v.rearrange("p (a b) -> p a b", a=free_shape[0])
                elif len(free_shape) == 3:
                    v = v.rearrange("p (a b c) -> p a b c", a=free_shape[0], b=free_shape[1])
                b = Buf(name)
                b.reads = list(self.hz)
                self.cur.append(b)
                self.offs[name] = self.off - nbytes
                return v, b

            def tile_at(self, name, off, free_shape, dt, inherit):
                save = self.off
                self.off = off
                v, b = self.tile(name, free_shape, dt)
                self.off = save
                for o in inherit:
                    if o.last_write is not None:
                        b.reads.append(o.last_write)
                    b.reads.extend(o.reads)
                return v, b

        AR = Arena()
        AR.hz = []
        AR.offs = {}

        cc = newctx("cc")
        const_bufs = []

        def cload(tile_ap, dram_ap, name):
            b = Buf(name)
            T.dma(SP, cc, lambda e, o=tile_ap, i=dram_ap: e.dma_start(out=o, in_=i), writes=[b])
            const_bufs.append(b)
            return b

        rowi = sb("rowi", [128, 128]); b_rowi = Buf("rowi")
        coli = sb("coli", [128, 128]); b_coli = Buf("coli")
        T.op(POOL, lambda e: e.iota(rowi[:], [[0, 128]], base=0, channel_multiplier=1,
                                    allow_small_or_imprecise_dtypes=True), writes=[b_rowi])
        T.op(POOL, lambda e: e.iota(coli[:], [[1, 128]], base=0, channel_multiplier=0,
                                    allow_small_or_imprecise_dtypes=True), writes=[b_coli])
        identf = sb("identf", [128, 128]); b_identf = Buf("identf")
        T.op(DVE, lambda e: e.tensor_tensor(out=identf[:], in0=rowi[:], in1=coli[:], op=ALU.is_equal),
             reads=[b_rowi, b_coli], writes=[b_identf])
        identb = sb("identb", [128, 128], BF16); b_identb = Buf("identb")
        T.op(DVE, lambda e: e.tensor_copy(out=identb[:], in_=identf[:]), reads=[b_identf], writes=[b_identb])
        maskd = sb("maskd", [128, 128]); b_maskd = Buf("maskd")
        tmpa = sb("tmpa", [128, 128]); b_tmpa = Buf("tmpa")
        tmpb = sb("tmpb", [128, 128]); b_tmpb = Buf("tmpb")
        T.op(DVE, lambda e: e.tensor_tensor(out=maskd[:], in0=rowi[:], in1=coli[:], op=ALU.is_gt),
             reads=[b_rowi, b_coli], writes=[b_maskd])
        T.op(DVE, lambda e: e.tensor_single_scalar(out=tmpa[:], in_=rowi[:], scalar=64.0, op=ALU.is_ge),
             reads=[b_rowi], writes=[b_tmpa])
        T.op(DVE, lambda e: e.tensor_single_scalar(out=tmpb[:], in_=coli[:], scalar=64.0, op=ALU.is_ge),
             reads=[b_coli], writes=[b_tmpb])
        T.op(DVE, lambda e: e.tensor_tensor(out=tmpb[:], in0=tmpa[:], in1=tmpb[:], op=ALU.is_equal),
             reads=[b_tmpa, b_tmpb], writes=[b_tmpb])
        T.op(DVE, lambda e: e.scalar_tensor_tensor(out=maskd[:], in0=maskd[:], scalar=-1.0 / 16.0, in1=tmpb[:],
                                                   op0=ALU.mult, op1=ALU.mult),
             reads=[b_maskd, b_tmpb], writes=[b_maskd])
        csel = sb("csel", [128, 16]); b_csel = Buf("csel")
        T.op(POOL, lambda e: e.memset(csel[:], 0.0), writes=[b_csel])
        T.op(DVE, lambda e: e.tensor_scalar(out=csel[:, 1:2], in0=tmpa[:, 0:1], scalar1=-1.0 / 16.0, scalar2=None,
                                            op0=ALU.mult), reads=[b_tmpa], writes=[b_csel])
        T.op(DVE, lambda e: e.tensor_scalar(out=csel[:, 0:1], in0=tmpa[:, 0:1], scalar1=1.0 / 16.0,
                                            scalar2=-1.0 / 16.0, op0=ALU.mult, op1=ALU.add),
             reads=[b_tmpa], writes=[b_csel])
        onesb = sb("onesb", [128, 128], BF16); b_onesb = Buf("onesb")
        T.op(POOL, lambda e: e.memset(onesb[:], 1.0), writes=[b_onesb])
        onesm = sb("onesm", [128, 128], BF16); b_onesm = Buf("onesm")
        T.op(POOL, lambda e: e.memset(onesm[:], 1.0 / 1024.0), writes=[b_onesm])
        onesr = sb("onesr", [1, 128]); b_onesr = Buf("onesr")
        T.op(POOL, lambda e: e.memset(onesr[:], 1.0), writes=[b_onesr])
        onescol = sb("onescol", [128, 1]); b_onescol = Buf("onescol")
        T.op(POOL, lambda e: e.memset(onescol[:], 1.0), writes=[b_onescol])

        lng = sb("lng", [128, DEPTH * 3 * NCH]); b_lng = cload(lng[:], lng_t, "lng")
        lnb = sb("lnb", [128, DEPTH * 3 * NCH]); b_lnb = cload(lnb[:], lnb_t, "lnb")
        wr = sb("wr", [128, NCH, NEXP]); b_wr = cload(wr[:], w_router.rearrange("(k p) e -> p k e", p=128), "wr")
        br = sb("br", [1, NEXP]); b_br = cload(br[:], b_router, "br")
        wa2 = sb("wa2", [32, depth, 256]); b_wa2 = Buf("wa2")
        alwf = sb("alwf", [128, depth, NCH, 16]); b_alwf = Buf("alwf")
        gng = sb("gng", [128, depth, 4]); b_gng = Buf("gng")
        slng = sb("slng", [128, depth, 4]); b_slng = Buf("slng")
        wsf, b_wsf = AR.tile("wsf", [depth, 4, 128])
        bsr, b_bsr = AR.tile("bsr", [depth, 512])
        slnbr, b_slnbr = AR.tile("slnbr", [depth, 512])
        for l in range(depth):
            T.dma(SP, cc, lambda e, l=l: e.dma_start(out=wa2[0:16, l, :], in_=w_a2[l]), writes=[b_wa2])
            T.dma(SP, cc, lambda e, l=l: e.dma_start(out=wa2[16:17, l, :], in_=b_a[l]), writes=[b_wa2])
            T.dma(SP, cc, lambda e, l=l: e.dma_start(
                out=alwf[:, l, :, :], in_=w_in[l, :, 1536:1552].rearrange("(k p) c -> p k c", p=128)), writes=[b_alwf])
            T.dma(SP, cc, lambda e, l=l: e.dma_start(out=gng[:, l, :], in_=gng_t[l]), writes=[b_gng])
            T.dma(SP, cc, lambda e, l=l: e.dma_start(out=slng[:, l, :], in_=slng_t[l]), writes=[b_slng])
            T.dma(SP, cc, lambda e, l=l: e.dma_start(
                out=wsf[:, l, :, :], in_=w_sT[l].rearrange("g s t -> s g t")), writes=[b_wsf])
            T.dma(SP, cc, lambda e, l=l: e.dma_start(out=bsr[0:1, l, :], in_=b_s[l]), writes=[b_bsr])
            T.dma(SP, cc, lambda e, l=l: e.dma_start(out=slnbr[0:1, l, :], in_=slnb[l]), writes=[b_slnbr])
        memf, b_memf = AR.tile("memf", [NCH, NMEM])
        T.dma(SP, cc, lambda e: e.dma_start(out=memf, in_=memT.rearrange("(k p) m -> p k m", p=128)), writes=[b_memf])
        for b in const_bufs + [b_wa2, b_alwf, b_gng, b_slng, b_wsf, b_bsr, b_slnbr, b_memf]:
            b.last_write = (cc, cc.count)

        alwb = sb("alwb", [128, depth, NCH, 16], BF16); b_alwb = Buf("alwb")
        T.op(DVE, lambda e: e.tensor_copy(out=alwb[:], in_=alwf[:]), reads=[b_alwf], writes=[b_alwb])
        T.op(DVE, lambda e: e.tensor_scalar(out=gng[:], in0=gng[:], scalar1=float(np.sqrt(128.0)), scalar2=None,
                                            op0=ALU.mult), reads=[b_gng], writes=[b_gng])
        wmT = sb("wmT", [128, depth, 4, 128], BF16); b_wmT = Buf("wmT")
        for l in range(depth):
            for g in range(4):
                T.op(POOL, lambda e, l=l, g=g: e.memset(wsf[64:128, l, g, 0:64], 0.0), reads=[], writes=[b_wsf])
        T.op(DVE, lambda e: e.tensor_copy(out=wmT[:], in_=wsf), reads=[b_wsf], writes=[b_wmT])
        memb, b_memb = AR.tile("memb", [NCH, NMEM], BF16)
        T.op(DVE, lambda e: e.tensor_copy(out=memb, in_=memf), reads=[b_memf], writes=[b_memb])
        bterm = sb("bterm", [128, depth, 4, 128]); b_bterm = Buf("bterm")
        rsr = sb("rsr", [1, 128]); b_rsr = Buf("rsr")
        crot = Rot([0, 1])
        for l in range(depth):
            for g in range(4):
                pt, pb = crot.next()
                T.op(PE, lambda e, pt=pt, l=l, g=g: e.matmul(pt[0:1, 0:128], onescol[:, 0:1], wsf[:, l, g, :],
                                                              start=True, stop=True),
                     reads=[b_onescol, b_wsf], writes=[pb])
                T.op(ACT, lambda e, pt=pt: e.copy(out=rsr[:], in_=pt[0:1, 0:128]), reads=[pb], writes=[b_rsr])
                pt2, pb2 = crot.next()
                T.op(PE, lambda e, pt2=pt2, l=l, g=g: e.matmul(pt2[:, 0:128], slnbr[0:1, l, g * 128:(g + 1) * 128],
                                                                rsr[0:1, :], start=True, stop=False),
                     reads=[b_slnbr, b_rsr], writes=[pb2], inc=False)
                T.op(PE, lambda e, pt2=pt2, l=l, g=g: e.matmul(pt2[:, 0:128], onesr[0:1, :],
                                                                bsr[0:1, l, g * 128:(g + 1) * 128],
                                                                start=False, stop=True),
                     reads=[b_onesr, b_bsr], writes=[pb2])
                T.op(ACT, lambda e, pt2=pt2, l=l, g=g: e.copy(out=bterm[:, l, g, :], in_=pt2[:, 0:128]),
                     reads=[pb2], writes=[b_bterm])

        castctx = [newctx("cast%d" % i) for i in range(8)]
        unit_buf = [Buf("unit%d" % u) for u in range(NU)]
        cast_i = [0]

        def kp(ap2d):
            return ap2d.rearrange("(k p) c -> p k c", p=128)

        UIDX = {}
        USRC = {}
        for l in range(depth):
            base = l * UPL
            names = ["wk0", "wk1", "wv0", "wv1", "inA", "inB", "inC", "inD", "inE", "wout0", "wout1",
                     "wq0", "wq1", "wo0", "wo1"]
            for i, n in enumerate(names):
                UIDX[(l, n)] = base + i
            USRC[(l, "wk0")] = (kp(wk_x[l, :, 0:512]), 8); USRC[(l, "wk1")] = (kp(wk_x[l, :, 512:1024]), 8)
            USRC[(l, "wv0")] = (kp(wv_x[l, :, 0:512]), 8); USRC[(l, "wv1")] = (kp(wv_x[l, :, 512:1024]), 8)
            USRC[(l, "inA")] = (kp(w_in[l, :, 0:512]), 8); USRC[(l, "inB")] = (kp(w_in[l, :, 512:1024]), 8)
            USRC[(l, "inC")] = (kp(w_in[l, :, 1024:1536]), 8); USRC[(l, "inD")] = (kp(w_in[l, :, 1552:2064]), 8)
            USRC[(l, "inE")] = (kp(w_in[l, :, 2064:2576]), 8)
            USRC[(l, "wout0")] = (kp(w_out[l, :, 0:512]), 8); USRC[(l, "wout1")] = (kp(w_out[l, :, 512:1024]), 8)
            USRC[(l, "wq0")] = (kp(wq_x[l, :, 0:512]), 8); USRC[(l, "wq1")] = (kp(wq_x[l, :, 512:1024]), 8)
            USRC[(l, "wo0")] = (kp(wo_x[l, :, 0:512]), 8); USRC[(l, "wo1")] = (kp(wo_x[l, :, 512:1024]), 8)
            for e_ in range(NEXP):
                UIDX[(l, "wg", e_)] = base + 15 + 3 * e_
                UIDX[(l, "wu", e_)] = base + 15 + 3 * e_ + 1
                UIDX[(l, "wd", e_)] = base + 15 + 3 * e_ + 2
                USRC[(l, "wg", e_)] = (kp(w_gate[l, e_]), 8)
                USRC[(l, "wu", e_)] = (kp(w_up[l, e_]), 8)
                USRC[(l, "wd", e_)] = (kp(w_down[l, e_]), 4)

        cast_done = set()

        def emit_cast(key):
            if key in cast_done:
                return
            cast_done.add(key)
            u = UIDX[key]
            src, k = USRC[key]
            c = castctx[cast_i[0] % len(castctx)]
            cast_i[0] += 1
            T.dma(POOL, c, lambda e, u=u, s=src, k=k: e.dma_start(
                out=wsc[u].rearrange("p (k c) -> p k c", k=k), in_=s), writes=[unit_buf[u]], serial=True)

        ring = []
        for i in range(RING):
            t = sb("ring%d" % i, [128, 4096], BF16)
            ring.append((t, Buf("ring%d" % i), newctx("ring%d" % i)))

        stream = []
        for l in range(depth):
            for n in ["wk0", "wk1", "wv0", "wv1"]:
                stream.append((l, n))
        for ti in range(NT):
            for l in range(depth):
                for n in ["inE", "inA", "inB", "inD", "inC", "wout0", "wout1", "wq0", "wq1", "wo0", "wo1"]:
                    stream.append((l, n))
                for e_ in range(NEXP):
                    stream.append((l, "wg", e_)); stream.append((l, "wu", e_)); stream.append((l, "wd", e_))
        issued = [0]
        taken = [0]
        casted = [0]
        released = set()
        loaded = {}
        LOOKAHEAD = RING - 1
        CASTAHEAD = 10

        def pump():
            while casted[0] < len(stream) and casted[0] < taken[0] + LOOKAHEAD + CASTAHEAD:
                emit_cast(stream[casted[0]])
                casted[0] += 1
            while issued[0] < len(stream) and issued[0] < taken[0] + LOOKAHEAD:
                k = issued[0]
                if k >= RING and (k - RING) not in released:
                    break
                u = UIDX[stream[k]]
                t, b, c = ring[k % RING]
                T.dma(SP, c, lambda e, t=t, u=u: e.dma_start(out=t[:], in_=wsc[u]), reads=[unit_buf[u]], writes=[b])
                loaded[k] = (t, b, k)
                issued[0] += 1

        def take(key):
            k = taken[0]
            assert stream[k] == key, (stream[k], key)
            pump()
            assert k in loaded, ("ring stall", key, k, issued[0], sorted(released)[-4:])
            r = loaded.pop(k)
            taken[0] += 1
            pump()
            return r

        def rel(unit):
            released.add(unit[2])
            pump()

        xres = sb("xres", [128, NCH, TT]); b_xres = [Buf("xres%d" % c) for c in range(NCH)]
        lt = [(sb("lt%d" % i, [128, TT]), Buf("lt%d" % i)) for i in range(2)]
        xbf = sb("xbf", [128, NCH, TT], BF16); b_xbf = Buf("xbf")
        xpre = xres
        c_xin = newctx("xin")
        c_xnext = newctx("xnext")
        xnext = sb("xnext", [128, NCH, TT]); b_xnext = Buf("xnext")
        KT = sb("KT", [128, depth, NCH, NMEM], BF16); b_KT = Buf("KT")
        Vm = sb("Vm", [128, depth, 2, D], BF16); b_Vm = Buf("Vm")
        Sst = sb("Sst", [128, depth, 2, 256]); b_S = [[Buf("S%d%d" % (l, j)) for j in range(2)] for l in range(depth)]
        T.op(POOL, lambda e: e.memset(Sst[:], 0.0), writes=[b for row in b_S for b in row])
        ltp = (sb("ltp", [128, TT]), Buf("ltp"))
        lnv = sb("lnv", [128, TT]); b_lnv = Buf("lnv")
        lnr = sb("lnr", [128, TT]); b_lnr = Buf("lnr")
        lnn = sb("lnn", [128, TT]); b_lnn = Buf("lnn")

        kvrot = Rot([2, 3, 4, 5])
        for l in range(depth):
            wk_t = [take((l, "wk0")), take((l, "wk1"))]
            for ec in range(NCH):
                wt, wb, _ = wk_t[ec // 4]
                wv = wt[:].rearrange("p (k c) -> p k c", k=8)
                pt, pb = kvrot.next()
                for kc in range(NCH):
                    T.op(PE, lambda e, pt=pt, wv=wv, kc=kc, ec=ec: e.matmul(
                        pt[:, 0:NMEM], wv[:, kc, (ec % 4) * 128:(ec % 4 + 1) * 128], memb[:, kc, :],
                        start=(kc == 0), stop=(kc == NCH - 1)),
                        reads=[wb, b_memb], writes=[pb], inc=(kc == NCH - 1))
                T.op(ACT, lambda e, pt=pt, l=l, ec=ec: e.mul(out=KT[:, l, ec, :], in_=pt[:, 0:NMEM], mul=1.0 / 16.0),
                     reads=[pb], writes=[b_KT])
            rel(wk_t[0]); rel(wk_t[1])
            wv_t = [take((l, "wv0")), take((l, "wv1"))]
            for mc in range(2):
                for hf in range(2):
                    wt, wb, _ = wv_t[hf]
                    wv = wt[:].rearrange("p (k c) -> p k c", k=8)
                    pt, pb = kvrot.next()
                    for kc in range(NCH):
                        T.op(PE, lambda e, pt=pt, wv=wv, kc=kc, mc=mc: e.matmul(
                            pt[:, :], memb[:, kc, mc * 128:(mc + 1) * 128], wv[:, kc, :],
                            start=(kc == 0), stop=(kc == NCH - 1)),
                            reads=[wb, b_memb], writes=[pb], inc=(kc == NCH - 1))
                    T.op(DVE, lambda e, pt=pt, l=l, mc=mc, hf=hf: e.tensor_copy(
                        out=Vm[:, l, mc, hf * 512:(hf + 1) * 512], in_=pt[:, :]), reads=[pb], writes=[b_Vm])
            rel(wv_t[0]); rel(wv_t[1])

        pool_ok = [False]

        def stats_ops(dc, st):
            pm, pmb, pv, pvb, sqb, b_sqb = st
            T.op(ACT, lambda e, dc=dc: e.copy(out=xbf[:, dc, :], in_=xres[:, dc, :]), reads=[b_xres[dc]], writes=[b_xbf])
            if False:
                T.op(POOL, lambda e, dc=dc, sqb=sqb: e.tensor_tensor(out=sqb[:, dc, :], in0=xres[:, dc, :], in1=xres[:, dc, :], op=ALU.mult),
                     reads=[b_xres[dc]], writes=[b_sqb])
            else:
                T.op(ACT, lambda e, dc=dc, sqb=sqb: e.activation(out=sqb[:, dc, :], in_=xres[:, dc, :], func=AF.Square),
                     reads=[b_xres[dc]], writes=[b_sqb])

        def stats_mm(dc, st):
            pm, pmb, pv, pvb, sqb, b_sqb = st
            T.op(PE, lambda e, dc=dc, pm=pm: e.matmul(pm[:, :], onesm[:], xbf[:, dc, :], start=(dc == 0), stop=(dc == NCH - 1)),
                 reads=[b_onesm, b_xbf], writes=[pmb], inc=(dc == NCH - 1))
            T.op(PE, lambda e, dc=dc, pv=pv, sqb=sqb: e.matmul(pv[:, :], onesm[:], sqb[:, dc, :], start=(dc == 0), stop=(dc == NCH - 1)),
                 reads=[b_onesm, b_sqb], writes=[pvb], inc=(dc == NCH - 1))

        def layer_norm(l, which, st, use_pool=True):
            col0 = (l * 3 + which) * NCH
            pm, pmb, pv, pvb, sqb, b_sqb = st
            T.op(ACT, lambda e, pm=pm: e.activation(out=lnv[:], in_=pm[:, :], func=AF.Square), reads=[pmb], writes=[b_lnv])
            T.op(DVE, lambda e, pv=pv: e.tensor_tensor(out=lnv[:], in0=pv[:, :], in1=lnv[:], op=ALU.subtract),
                 reads=[pvb, b_lnv], writes=[b_lnv])
            T.op(DVE, lambda e: e.tensor_scalar(out=lnv[:], in0=lnv[:], scalar1=0.0, scalar2=LN_EPS, op0=ALU.max, op1=ALU.add),
                 reads=[b_lnv], writes=[b_lnv])
            T.op(ACT, lambda e: e.activation(out=lnr[:], in_=lnv[:], func=AF.Ln), reads=[b_lnv], writes=[b_lnr])
            T.op(ACT, lambda e: e.activation(out=lnr[:], in_=lnr[:], func=AF.Exp, scale=-0.5), reads=[b_lnr], writes=[b_lnr])
            T.op(DVE, lambda e, pm=pm: e.scalar_tensor_tensor(out=lnn[:], in0=pm[:, :], scalar=-1.0, in1=lnr[:], op0=ALU.mult, op1=ALU.mult),
                 reads=[pmb, b_lnr], writes=[b_lnn])
            for c in range(NCH):
                T.op(DVE, lambda e, c=c: e.tensor_tensor(out=xres[:, c, :], in0=xres[:, c, :], in1=lnr[:], op=ALU.mult),
                     reads=[b_xres[c], b_lnr], writes=[b_xres[c]])
                T.op(DVE, lambda e, c=c: e.tensor_tensor(out=xres[:, c, :], in0=xres[:, c, :], in1=lnn[:], op=ALU.add),
                     reads=[b_xres[c], b_lnn], writes=[b_xres[c]])
                T.op(ACT, lambda e, c=c: e.activation(out=xbf[:, c, :], in_=xres[:, c, :], func=AF.Identity,
                                                      scale=lng[:, col0 + c:col0 + c + 1], bias=lnb[:, col0 + c:col0 + c + 1]),
                     reads=[b_xres[c], b_lng, b_lnb], writes=[b_xbf])
            for c in range(NCH):
                T.op(ACT, lambda e, c=c: e.activation(out=xres[:, c, :], in_=xres[:, c, :], func=AF.Identity,
                                                      scale=lng[:, col0 + c:col0 + c + 1], bias=lnb[:, col0 + c:col0 + c + 1]),
                     reads=[b_xres[c], b_lng, b_lnb], writes=[b_xres[c]])

        def proj_residual(units, src, b_src, rot, st):
            for dc in range(NCH):
                wt, wb, _ = units[dc // 4]
                wv = wt[:].rearrange("p (k c) -> p k c", k=8)
                pt, pb = rot.next()
                for ic in range(NCH):
                    T.op(PE, lambda e, pt=pt, wv=wv, ic=ic, dc=dc: e.matmul(
                        pt[:, :], wv[:, ic, (dc % 4) * 128:(dc % 4 + 1) * 128], src[:, ic, :],
                        start=(ic == 0), stop=(ic == NCH - 1)),
                        reads=[wb, b_src], writes=[pb], inc=(ic == NCH - 1))
                T.op(DVE, lambda e, pt=pt, dc=dc: e.scalar_tensor_tensor(
                    out=xpre[:, dc, :], in0=xres[:, dc, :], scalar=ALPHA, in1=pt[:, :], op0=ALU.mult, op1=ALU.add),
                    reads=[b_xres[dc], pb], writes=[b_xres[dc]])
                stats_ops(dc, st)
                if dc >= 5:
                    stats_mm(dc - 5, st)
                if dc % 4 == 3:
                    rel(units[dc // 4])
            for d_ in range(NCH - 5, NCH):
                stats_mm(d_, st)

        def mixer(l, ti):
            AR.reset()
            ksb, b_ksb = AR.tile("ksb", [4, 256])
            e1 = [AR.tile("e1_%d" % i, [256]) for i in range(2)]
            lbuf, b_lbuf = AR.tile("lbuf", [4, 256])
            edb = [AR.tile("ed_%d" % i, [256]) for i in range(2)]
            r_off = AR.offs["ksb"]
            r_bufs = [b_ksb, e1[0][1], e1[1][1], b_lbuf, edb[0][1], edb[1][1]]
            vbf, b_vbf = AR.tile("vbf", [4, 512], BF16)
            gv, b_gv = AR.tile("gv", [4, 512])
            gsq, b_gsq = AR.tile("gsq", [4, 512])
            vn, b_vn = AR.tile("vn", [4, 512], BF16)
            alow, b_alow = AR.tile("alow", [TT])
            qT, b_qT = AR.tile("qT", [2, TT], BF16)
            gu, b_gu = AR.tile("gu", [4, TT])
            kdec, b_kdec = AR.tile("kdec", [4, 256], BF16)
            decay, b_decay = AR.tile("decay", [128])
            sbf = [[AR.tile("sbf%d%d" % (i, j), [256], BF16) for j in range(2)] for i in range(2)]
            ymix, b_ymix = AR.tile("ymix", [8, TT], BF16)
            st1, b_st1 = AR.tile("st1", [16])
            st2, b_st2 = AR.tile("st2", [16])
            st3, b_st3 = AR.tile("st3", [16])
            stmp = [AR.tile("stmp%d" % i, [4, 128]) for i in range(2)]

            rotA = Rot([0, 1, 2, 3])

            def w8(u):
                return u[0][:].rearrange("p (k c) -> p k c", k=8), u[1]

            def fm_proj(wv, wb, col, M, evac):
                pt, pb = rotA.next()
                for kc in range(NCH):
                    T.op(PE, lambda e, pt=pt, kc=kc: e.matmul(pt[0:M, :], wv[:, kc, col:col + M], xbf[:, kc, :],
                                                             start=(kc == 0), stop=(kc == NCH - 1)),
                         reads=[wb, b_xbf], writes=[pb], inc=(kc == NCH - 1))
                evac(pt, pb)

            def tm_proj(wv, wb, col, N, blk, evac):
                pt, pb = rotA.next()
                for kc in range(NCH):
                    T.op(PE, lambda e, pt=pt, kc=kc: e.matmul(pt[:, 0:N], xbf[:, kc, blk * 128:(blk + 1) * 128],
                                                             wv[:, kc, col:col + N],
                                                             start=(kc == 0), stop=(kc == NCH - 1)),
                         reads=[wb, b_xbf], writes=[pb], inc=(kc == NCH - 1))
                evac(pt, pb)

            T.op(DVE, lambda e: e.memset(alow[0:32, :], 1.0), writes=[b_alow])
            uE = take((l, "inE"))
            wE, bE = w8(uE)
            for blk in range(4):
                tm_proj(wE, bE, 0, 512, blk, lambda pt, pb, blk=blk: T.op(
                    ACT, lambda e: e.activation(out=gv[:, blk, :], in_=pt[:, :], func=AF.Gelu_apprx_tanh), reads=[pb], writes=[b_gv]))
            rel(uE)
            gvv = gv.rearrange("p a (g c) -> p (a g) c", g=4)
            gsqv = gsq.rearrange("p a (g c) -> p (a g) c", g=4)
            vnv = vn.rearrange("p a (g c) -> p (a g) c", g=4)
            T.op(DVE, lambda e: e.tensor_reduce(out=st1, in_=gvv, axis=AX.X, op=ALU.add), reads=[b_gv], writes=[b_st1])
            T.op(DVE, lambda e: e.tensor_tensor(out=gsq, in0=gv, in1=gv, op=ALU.mult), reads=[b_gv], writes=[b_gsq])
            T.op(DVE, lambda e: e.tensor_reduce(out=st2, in_=gsqv, axis=AX.X, op=ALU.add), reads=[b_gsq], writes=[b_st2])
            T.op(DVE, lambda e: e.tensor_scalar(out=st1, in0=st1, scalar1=1.0 / 128.0, scalar2=None, op0=ALU.mult),
                 reads=[b_st1], writes=[b_st1])
            T.op(DVE, lambda e: e.tensor_tensor(out=st3, in0=st1, in1=st1, op=ALU.mult), reads=[b_st1], writes=[b_st3])
            T.op(DVE, lambda e: e.scalar_tensor_tensor(out=st2, in0=st2, scalar=1.0 / 128.0, in1=st3, op0=ALU.mult, op1=ALU.subtract),
                 reads=[b_st2, b_st3], writes=[b_st2])
            T.op(DVE, lambda e: e.tensor_scalar(out=st2, in0=st2, scalar1=0.0, scalar2=None, op0=ALU.max), reads=[b_st2], writes=[b_st2])
            T.op(ACT, lambda e: e.activation(out=st2, in_=st2, func=AF.Sqrt, bias=LN_EPS, scale=1.0), reads=[b_st2], writes=[b_st2])
            T.op(DVE, lambda e: e.reciprocal(out=st2, in_=st2), reads=[b_st2], writes=[b_st2])
            T.op(DVE, lambda e: e.scalar_tensor_tensor(out=st3, in0=st1, scalar=-1.0, in1=st2, op0=ALU.mult, op1=ALU.mult),
                 reads=[b_st1, b_st2], writes=[b_st3])
            T.op(DVE, lambda e: e.tensor_tensor(out=gsqv, in0=gvv, in1=st2.unsqueeze(2).to_broadcast([128, 16, 128]), op=ALU.mult),
                 reads=[b_gv, b_st2], writes=[b_gsq])
            T.op(DVE, lambda e: e.tensor_tensor(out=vnv, in0=gsqv, in1=st3.unsqueeze(2).to_broadcast([128, 16, 128]), op=ALU.add),
                 reads=[b_gsq, b_st3], writes=[b_vn])

            alw_v = alwb[:, l, :, :]
            fm_proj(alw_v, b_alwb, 0, 16, lambda pt, pb: T.op(
                ACT, lambda e: e.copy(out=alow[0:16, :], in_=pt[0:16, :]), reads=[pb], writes=[b_alow]))
            uA = take((l, "inA"))
            wA, bA = w8(uA)
            for blk in range(4):
                tm_proj(wA, bA, 256, 256, blk, lambda pt, pb, blk=blk: T.op(
                    ACT, lambda e: e.copy(out=ksb[:, blk, :], in_=pt[:, 0:256]), reads=[pb], writes=[b_ksb]))
            for mc in range(2):
                fm_proj(wA, bA, mc * 128, 128, lambda pt, pb, mc=mc: T.op(
                    ACT, lambda e: e.mul(out=qT[:, mc, :], in_=pt[:, :], mul=0.125), reads=[pb], writes=[b_qT]))
            rel(uA)
            uB = take((l, "inB"))
            wB, bB = w8(uB)
            wa2v = wa2[0:17, l, :]
            bl_bank, bl_buf = banks[7]
            for blk in range(4):
                pz, pzb = rotA.next()
                T.op(PE, lambda e, pz=pz, blk=blk: e.matmul(pz[:, 0:256], alow[0:17, blk * 128:(blk + 1) * 128], wa2v,
                                                            start=True, stop=True),
                     reads=[b_alow, b_wa2], writes=[pzb])
                e1t, e1b = e1[blk % 2]
                T.op(ACT, lambda e, pz=pz, e1t=e1t: e.activation(out=e1t, in_=pz[:, 0:256], func=AF.Exp, scale=-1.0),
                     reads=[pzb], writes=[e1b])
                T.op(ACT, lambda e, e1t=e1t, blk=blk: e.activation(out=lbuf[:, blk, :], in_=e1t, func=AF.Ln, bias=1.0, scale=1.0),
                     reads=[e1b], writes=[b_lbuf])
                tm_proj(wB, bB, 0, 512, blk, lambda pt, pb, blk=blk: T.op(
                    ACT, lambda e: e.copy(out=vbf[:, blk, :], in_=pt[:, :]), reads=[pb], writes=[b_vbf]))
                pd, pdb = rotA.next()
                T.op(PE, lambda e, pd=pd, blk=blk: e.matmul(pd[:, 0:256], maskd[:], lbuf[:, blk, :], start=True, stop=True),
                     reads=[b_maskd, b_lbuf], writes=[pdb])
                edt, edbuf = edb[blk % 2]
                T.op(ACT, lambda e, pd=pd, edt=edt: e.activation(out=edt, in_=pd[:, 0:256], func=AF.Exp),
                     reads=[pdb], writes=[edbuf])
                T.op(DVE, lambda e, edt=edt, blk=blk: e.tensor_tensor(out=kdec[:, blk, :], in0=ksb[:, blk, :], in1=edt, op=ALU.mult),
                     reads=[b_ksb, edbuf], writes=[b_kdec])
                for j in range(2):
                    cidx = (blk * 2 + j) * 16
                    T.op(PE, lambda e, blk=blk, j=j, cidx=cidx: e.matmul(
                        bl_bank[:, cidx:cidx + 16], lbuf[:, blk, j * 128:(j + 1) * 128], csel[:, :], start=True, stop=True),
                        reads=[b_lbuf, b_csel], writes=[bl_buf], inc=True)
            rel(uB)
            T.op(ACT, lambda e: e.activation(out=decay, in_=bl_bank[:, 0:128], func=AF.Exp), reads=[bl_buf], writes=[b_decay])
            uD = take((l, "inD"))
            wD, bD = w8(uD)
            for mc in range(4):
                fm_proj(wD, bD, mc * 128, 128, lambda pt, pb, mc=mc: T.op(
                    ACT, lambda e: e.activation(out=gu[:, mc, :], in_=pt[:, :], func=AF.Gelu_apprx_tanh), reads=[pb], writes=[b_gu]))
            rel(uD)
            for g in range(4):
                pm, pmb = rotA.next()
                for blk in range(4):
                    T.op(PE, lambda e, pm=pm, g=g, blk=blk: e.matmul(
                        pm[:, blk * 128:(blk + 1) * 128], vn[:, blk, g * 128:(g + 1) * 128], wmT[:, l, g, :],
                        start=True, stop=True), reads=[b_vn, b_wmT], writes=[pmb], inc=(blk == 3))
                sm, smb = stmp[g % 2]
                T.op(DVE, lambda e, pm=pm, g=g, sm=sm: e.scalar_tensor_tensor(
                    out=sm, in0=pm[:, :].rearrange("p (a t) -> p a t", a=4), scalar=slng[:, l, g:g + 1],
                    in1=bterm[:, l, g, :].unsqueeze(1).to_broadcast([128, 4, 128]), op0=ALU.mult, op1=ALU.add),
                    reads=[pmb, b_slng, b_bterm], writes=[smb])
                T.op(DVE, lambda e, g=g, sm=sm: e.tensor_tensor(
                    out=ymix[:, 4 + g, :], in0=sm.rearrange("p a t -> p (a t)"), in1=gu[:, g, :], op=ALU.mult),
                    reads=[smb, b_gu], writes=[b_ymix])
            sr, b_sr = AR.tile_at("sr", AR.offs["gsq"], [4, TT], F32, inherit=[b_gsq])
            osq, b_osq = AR.tile_at("osq", r_off, [4, TT], BF16, inherit=r_bufs)
            rinv = [AR.tile_at("rinv%d" % i, r_off + 4096 + 2048 * i, [TT], F32, inherit=r_bufs) for i in range(2)]
            t1 = [AR.tile_at("t1_%d" % i, r_off + 8192 + 2048 * i, [TT], F32, inherit=r_bufs) for i in range(2)]
            uC = take((l, "inC"))
            wC, bC = w8(uC)
            oT = [banks[4 + h] for h in range(4)]
            for c in range(8):
                blk, half = c // 2, c % 2
                r0, r1 = half * 64, half * 64 + 64
                for j in range(2):
                    pk, pkb = rotA.next()
                    T.op(PE, lambda e, pk=pk, blk=blk, j=j, r0=r0, r1=r1: e.matmul(
                        pk[:, 0:256], kdec[r0:r1, blk, j * 128:(j + 1) * 128], vbf[r0:r1, blk, j * 256:(j + 1) * 256],
                        start=True, stop=True), reads=[b_kdec, b_vbf], writes=[pkb])
                    dcol = (blk * 2 + j) * 16 + half
                    T.op(DVE, lambda e, pk=pk, j=j, dcol=dcol: e.scalar_tensor_tensor(
                        out=Sst[:, l, j, :], in0=Sst[:, l, j, :], scalar=decay[:, dcol:dcol + 1], in1=pk[:, 0:256],
                        op0=ALU.mult, op1=ALU.add), reads=[b_S[l][j], b_decay, pkb], writes=[b_S[l][j]])
                    st, stb = sbf[c % 2][j]
                    T.op(ACT, lambda e, st=st, j=j: e.copy(out=st, in_=Sst[:, l, j, :]), reads=[b_S[l][j]], writes=[stb])
                if c % 2 == 0:
                    mc = c // 2
                    fm_proj(wC, bC, mc * 128, 128, lambda pt, pb, mc=mc: T.op(
                        ACT, lambda e: e.activation(out=sr[:, mc, :], in_=pt[:, :], func=AF.Silu), reads=[pb], writes=[b_sr]))
                for j in range(2):
                    st, stb = sbf[c % 2][j]
                    hA, hB = 2 * j, 2 * j + 1
                    T.op(PE, lambda e, st=st, j=j, c=c, hA=hA: e.matmul(
                        oT[hA][0][:, c * 64:(c + 1) * 64], st[0:64, 0:128], qT[0:64, j, c * 64:(c + 1) * 64],
                        start=True, stop=True), reads=[stb, b_qT], writes=[oT[hA][1]])
                    T.op(PE, lambda e, st=st, j=j, c=c, hB=hB: e.matmul(
                        oT[hB][0][:, c * 64:(c + 1) * 64], st[64:128, 128:256], qT[64:128, j, c * 64:(c + 1) * 64],
                        start=True, stop=True), reads=[stb, b_qT], writes=[oT[hB][1]])
            rel(uC)
            for h in range(4):
                ob, obb = oT[h]
                T.op(ACT, lambda e, ob=ob, h=h: e.activation(out=osq[:, h, :], in_=ob[:, :], func=AF.Square),
                     reads=[obb], writes=[b_osq])
                pr, prb = rotA.next()
                T.op(PE, lambda e, pr=pr, h=h: e.matmul(pr[:, :], onesb[:], osq[:, h, :], start=True, stop=True),
                     reads=[b_onesb, b_osq], writes=[prb])
                rv, rvb = rinv[h % 2]
                T.op(DVE, lambda e, pr=pr, rv=rv: e.tensor_scalar(out=rv, in0=pr[:, :], scalar1=128.0 * RMS_EPS, scalar2=None, op0=ALU.add),
                     reads=[prb], writes=[rvb])
                T.op(ACT, lambda e, rv=rv: e.activation(out=rv, in_=rv, func=AF.Ln), reads=[rvb], writes=[rvb])
                T.op(ACT, lambda e, rv=rv: e.activation(out=rv, in_=rv, func=AF.Exp, scale=-0.5), reads=[rvb], writes=[rvb])
                tt, ttb = t1[h % 2]
                T.op(DVE, lambda e, ob=ob, rv=rv, tt=tt: e.tensor_tensor(out=tt, in0=ob[:, :], in1=rv, op=ALU.mult),
                     reads=[obb, rvb], writes=[ttb])
                T.op(DVE, lambda e, tt=tt, h=h: e.scalar_tensor_tensor(
                    out=ymix[:, h, :], in0=tt, scalar=gng[:, l, h:h + 1], in1=sr[:, h, :], op0=ALU.mult, op1=ALU.mult),
                    reads=[ttb, b_gng, b_sr], writes=[b_ymix])
            uo = [take((l, "wout0")), take((l, "wout1"))]
            sqt = AR.tile_at("sqb", AR.offs["gv"], [NCH, TT], BF16, inherit=[b_gv, b_gsq])
            st = (banks[4][0], banks[4][1], banks[5][0], banks[5][1], sqt[0], sqt[1])
            proj_residual(uo, ymix, b_ymix, rotA, st)
            return st

        def xattn(l, ti):
            AR.reset()
            qT8, b_qT8 = AR.tile("qT8", [8, TT], BF16)
            eT, b_eT = AR.tile("eT", [4, 2, TT], BF16)
            rden = [AR.tile("rden%d" % i, [TT]) for i in range(2)]
            oT8, b_oT8 = AR.tile("oT8", [8, TT], BF16)
            sqt = AR.tile("sqb", [NCH, TT], BF16)
            rot = Rot([0, 1, 2, 3, 4, 5])
            uq = [take((l, "wq0")), take((l, "wq1"))]
            for ec in range(NCH):
                wt, wb, _ = uq[ec // 4]
                wv = wt[:].rearrange("p (k c) -> p k c", k=8)
                pt, pb = rot.next()
                for kc in range(NCH):
                    T.op(PE, lambda e, pt=pt, wv=wv, kc=kc, ec=ec: e.matmul(
                        pt[:, :], wv[:, kc, (ec % 4) * 128:(ec % 4 + 1) * 128], xbf[:, kc, :],
                        start=(kc == 0), stop=(kc == NCH - 1)), reads=[wb, b_xbf], writes=[pb], inc=(kc == NCH - 1))
                if ec % 2 == 0:
                    T.op(ACT, lambda e, pt=pt, ec=ec: e.copy(out=qT8[:, ec, :], in_=pt[:, :]), reads=[pb], writes=[b_qT8])
                else:
                    T.op(DVE, lambda e, pt=pt, ec=ec: e.tensor_copy(out=qT8[:, ec, :], in_=pt[:, :]), reads=[pb], writes=[b_qT8])
                if ec % 4 == 3:
                    rel(uq[ec // 4])
            for h in range(4):
                for mc in range(2):
                    pt, pb = rot.next()
                    for j in range(2):
                        T.op(PE, lambda e, pt=pt, h=h, mc=mc, j=j: e.matmul(
                            pt[:, :], KT[:, l, 2 * h + j, mc * 128:(mc + 1) * 128], qT8[:, 2 * h + j, :],
                            start=(j == 0), stop=(j == 1)), reads=[b_KT, b_qT8], writes=[pb], inc=(j == 1))
                    T.op(ACT, lambda e, pt=pt, h=h, mc=mc: e.activation(out=eT[:, h, mc, :], in_=pt[:, :], func=AF.Exp),
                         reads=[pb], writes=[b_eT])
                pd, pdb = rot.next()
                for mc in range(2):
                    T.op(PE, lambda e, pd=pd, h=h, mc=mc: e.matmul(pd[:, :], onesb[:], eT[:, h, mc, :],
                                                                  start=(mc == 0), stop=(mc == 1)),
                         reads=[b_onesb, b_eT], writes=[pdb], inc=(mc == 1))
                rd, rdb = rden[h % 2]
                T.op(ACT, lambda e, pd=pd, rd=rd: e.activation(out=rd, in_=pd[:, :], func=AF.Ln), reads=[pdb], writes=[rdb])
                T.op(ACT, lambda e, rd=rd: e.activation(out=rd, in_=rd, func=AF.Exp, scale=-1.0), reads=[rdb], writes=[rdb])
                for j in range(2):
                    po, pob = rot.next()
                    ecol = (2 * h + j) * 128
                    for mc in range(2):
                        T.op(PE, lambda e, po=po, h=h, mc=mc, ecol=ecol: e.matmul(
                            po[:, :], Vm[:, l, mc, ecol:ecol + 128], eT[:, h, mc, :], start=(mc == 0), stop=(mc == 1)),
                            reads=[b_Vm, b_eT], writes=[pob], inc=(mc == 1))
                    T.op(DVE, lambda e, po=po, rd=rd, h=h, j=j: e.tensor_tensor(
                        out=oT8[:, 2 * h + j, :], in0=po[:, :], in1=rd, op=ALU.mult), reads=[pob, rdb], writes=[b_oT8])
            uo = [take((l, "wo0")), take((l, "wo1"))]
            st = (banks[6][0], banks[6][1], banks[7][0], banks[7][1], sqt[0], sqt[1])
            proj_residual(uo, oT8, b_oT8, rot, st)
            return st

        def moe(l, ti):
            AR.reset()
            lg, b_lg = AR.tile("lg", [4, 16])
            sc, b_sc = AR.tile("sc", [4, 16])
            pairs, b_pairs = AR.tile("pairs", [16, 6])
            gs, b_gs = AR.tile("gs", [4, 4])
            goh, b_goh = AR.tile("goh", [4, 4])
            msk, b_msk = AR.tile("msk", [4, 16])
            m1, b_m1 = AR.tile("m1", [4, 16])
            rem, b_rem = AR.tile("rem", [4, 16])
            tmpm, b_tmpm = AR.tile("tmpm", [4, 16])
            gate, b_gate = AR.tile("gate", [4, 16])
            gatebf, b_gatebf = AR.tile("gatebf", [4, 16], BF16)
            s4a, b_s4a = AR.tile("s4a", [4])
            s4b, b_s4b = AR.tile("s4b", [4])
            s4c, b_s4c = AR.tile("s4c", [4])
            sgt = [AR.tile("sg%d" % i, [TT]) for i in range(2)]
            tmt = [AR.tile("tm%d" % i, [TT]) for i in range(8)]
            hT = [AR.tile("hT%d" % i, [4, TT], BF16) for i in range(2)]
            sqt = AR.tile("sqb", [NCH, TT], BF16)

            def router():
                pr, prb = banks[7]
                for blk in range(4):
                    for kc in range(NCH):
                        T.op(PE, lambda e, blk=blk, kc=kc: e.matmul(
                            pr[:, blk * 16:(blk + 1) * 16], xres[:, kc, blk * 128:(blk + 1) * 128], wr[:, kc, :],
                            start=(kc == 0), stop=False), reads=[b_xres[kc], b_wr], writes=[prb], inc=False)
                    T.op(PE, lambda e, blk=blk: e.matmul(pr[:, blk * 16:(blk + 1) * 16], onesr[0:1, :], br[0:1, :],
                                                         start=False, stop=True), reads=[b_onesr, b_br], writes=[prb])
                B4 = lambda ap: ap.unsqueeze(2).to_broadcast([128, 4, 16])
                T.op(DVE, lambda e: e.tensor_copy(out=lg, in_=pr[:, 0:64].rearrange("p (a b) -> p a b", a=4)), reads=[prb], writes=[b_lg])
                T.op(DVE, lambda e: e.tensor_reduce(out=s4a, in_=lg, axis=AX.X, op=ALU.max), reads=[b_lg], writes=[b_s4a])
                T.op(DVE, lambda e: e.tensor_tensor(out=lg, in0=lg, in1=B4(s4a), op=ALU.subtract), reads=[b_lg, b_s4a], writes=[b_lg])
                T.op(ACT, lambda e: e.activation(out=sc, in_=lg, func=AF.Exp), reads=[b_lg], writes=[b_sc])
                T.op(DVE, lambda e: e.tensor_reduce(out=s4b, in_=sc, axis=AX.X, op=ALU.add), reads=[b_sc], writes=[b_s4b])
                T.op(DVE, lambda e: e.reciprocal(out=s4b, in_=s4b), reads=[b_s4b], writes=[b_s4b])
                T.op(DVE, lambda e: e.tensor_tensor(out=sc, in0=sc, in1=B4(s4b), op=ALU.mult), reads=[b_sc, b_s4b], writes=[b_sc])
                scg = sc.rearrange("p a (g k) -> p (a g) k", g=4)
                T.op(DVE, lambda e: e.tensor_tensor(out=pairs[:, :, 0:3], in0=scg[:, :, 0:3], in1=scg[:, :, 1:4], op=ALU.add),
                     reads=[b_sc], writes=[b_pairs])
                T.op(DVE, lambda e: e.tensor_tensor(out=pairs[:, :, 3:5], in0=scg[:, :, 0:2], in1=scg[:, :, 2:4], op=ALU.add),
                     reads=[b_sc], writes=[b_pairs])
                T.op(DVE, lambda e: e.tensor_tensor(out=pairs[:, :, 5:6], in0=scg[:, :, 0:1], in1=scg[:, :, 3:4], op=ALU.add),
                     reads=[b_sc], writes=[b_pairs])
                T.op(DVE, lambda e: e.tensor_reduce(out=gs.rearrange("p a g -> p (a g)"), in_=pairs, axis=AX.X, op=ALU.max),
                     reads=[b_pairs], writes=[b_gs])
                T.op(DVE, lambda e: e.tensor_reduce(out=s4c, in_=gs, axis=AX.X, op=ALU.max), reads=[b_gs], writes=[b_s4c])
                T.op(DVE, lambda e: e.tensor_tensor(out=goh, in0=gs, in1=s4c.unsqueeze(2).to_broadcast([128, 4, 4]), op=ALU.is_equal),
                     reads=[b_gs, b_s4c], writes=[b_goh])
                T.op(DVE, lambda e: e.tensor_tensor(
                    out=msk.rearrange("p a (g k) -> p (a g) k", g=4), in0=scg,
                    in1=goh.rearrange("p a g -> p (a g)").unsqueeze(2).to_broadcast([128, 16, 4]), op=ALU.mult),
                    reads=[b_sc, b_goh], writes=[b_msk])
                T.op(DVE, lambda e: e.tensor_reduce(out=s4a, in_=msk, axis=AX.X, op=ALU.max), reads=[b_msk], writes=[b_s4a])
                T.op(DVE, lambda e: e.tensor_tensor(out=m1, in0=msk, in1=B4(s4a), op=ALU.is_equal), reads=[b_msk, b_s4a], writes=[b_m1])
                T.op(DVE, lambda e: e.tensor_tensor(out=tmpm, in0=msk, in1=m1, op=ALU.mult), reads=[b_msk, b_m1], writes=[b_tmpm])
                T.op(DVE, lambda e: e.tensor_tensor(out=rem, in0=msk, in1=tmpm, op=ALU.subtract), reads=[b_msk, b_tmpm], writes=[b_rem])
                T.op(DVE, lambda e: e.tensor_reduce(out=s4b, in_=rem, axis=AX.X, op=ALU.max), reads=[b_rem], writes=[b_s4b])
                T.op(DVE, lambda e: e.tensor_tensor(out=m1, in0=rem, in1=B4(s4b), op=ALU.is_equal), reads=[b_rem, b_s4b], writes=[b_m1])
                T.op(DVE, lambda e: e.tensor_tensor(out=rem, in0=rem, in1=m1, op=ALU.mult), reads=[b_rem, b_m1], writes=[b_rem])
                T.op(DVE, lambda e: e.tensor_tensor(out=tmpm, in0=tmpm, in1=rem, op=ALU.add), reads=[b_tmpm, b_rem], writes=[b_tmpm])
                T.op(DVE, lambda e: e.tensor_tensor(out=s4c, in0=s4a, in1=s4b, op=ALU.add), reads=[b_s4a, b_s4b], writes=[b_s4c])
                T.op(DVE, lambda e: e.reciprocal(out=s4c, in_=s4c), reads=[b_s4c], writes=[b_s4c])
                T.op(DVE, lambda e: e.tensor_tensor(out=gate, in0=tmpm, in1=B4(s4c), op=ALU.mult), reads=[b_tmpm, b_s4c], writes=[b_gate])
                T.op(DVE, lambda e: e.tensor_copy(out=gatebf, in_=gate), reads=[b_gate], writes=[b_gatebf])
            rot_gu = Rot([0, 1, 2, 3])
            rot_gate = Rot([4, 5])
            rot_y = Rot([6, 7])
            state = {}

            def gu_mm(e_):
                wg = take((l, "wg", e_)); wu = take((l, "wu", e_)); wd = take((l, "wd", e_))
                wgv = wg[0][:].rearrange("p (k c) -> p k c", k=8)
                wuv = wu[0][:].rearrange("p (k c) -> p k c", k=8)
                tms = []
                for fc in range(4):
                    pg, pgb = rot_gu.next()
                    for kc in range(NCH):
                        T.op(PE, lambda e, pg=pg, kc=kc, fc=fc: e.matmul(
                            pg[:, :], wgv[:, kc, fc * 128:(fc + 1) * 128], xbf[:, kc, :], start=(kc == 0), stop=(kc == NCH - 1)),
                            reads=[wg[1], b_xbf], writes=[pgb], inc=(kc == NCH - 1))
                    pu, pub = rot_gu.next()
                    for kc in range(NCH):
                        T.op(PE, lambda e, pu=pu, kc=kc, fc=fc: e.matmul(
                            pu[:, :], wuv[:, kc, fc * 128:(fc + 1) * 128], xbf[:, kc, :], start=(kc == 0), stop=(kc == NCH - 1)),
                            reads=[wu[1], b_xbf], writes=[pub], inc=(kc == NCH - 1))
                    sg, sgb = sgt[fc % 2]
                    T.op(ACT, lambda e, pg=pg, sg=sg: e.activation(out=sg, in_=pg[:, :], func=AF.Silu), reads=[pgb], writes=[sgb])
                    tm, tmb = tmt[(e_ % 2) * 4 + fc]
                    T.op(DVE, lambda e, pu=pu, sg=sg, tm=tm: e.tensor_tensor(out=tm, in0=pu[:, :], in1=sg, op=ALU.mult),
                         reads=[pub, sgb], writes=[tmb])
                    tms.append((tm, tmb))
                rel(wg); rel(wu)
                state[e_] = (wd, tms)

            def gate_mm(e_):
                wd, tms = state[e_]
                pgt, pgtb = rot_gate.next()
                for blk in range(4):
                    T.op(PE, lambda e, blk=blk, e_=e_, pgt=pgt: e.matmul(
                        pgt[:, blk * 128:(blk + 1) * 128], gatebf[:, blk, e_:e_ + 1].to_broadcast([128, 128]), identb[:],
                        start=True, stop=True), reads=[b_gatebf, b_identb], writes=[pgtb], inc=(blk == 3))
                state[e_] = (wd, tms, pgt, pgtb)

            def h_ops(e_):
                wd, tms, pgt, pgtb = state[e_]
                ht, htb = hT[e_ % 2]
                for fc in range(4):
                    tm, tmb = tms[fc]
                    T.op(DVE, lambda e, tm=tm, pgt=pgt, ht=ht, fc=fc: e.tensor_tensor(out=ht[:, fc, :], in0=tm, in1=pgt[:, :], op=ALU.mult),
                         reads=[tmb, pgtb], writes=[htb])
                state[e_] = (wd, ht, htb)

            def down_phase(e_, st):
                wd, ht, htb = state.pop(e_)
                wdv = wd[0][:].rearrange("p (k c) -> p k c", k=4)
                for dc in range(NCH):
                    py, pyb = rot_y.next()
                    for fc in range(4):
                        T.op(PE, lambda e, py=py, fc=fc, dc=dc: e.matmul(
                            py[:, :], wdv[:, fc, dc * 128:(dc + 1) * 128], ht[:, fc, :], start=(fc == 0), stop=(fc == 3)),
                            reads=[wd[1], htb], writes=[pyb], inc=(fc == 3))
                    if e_ == 0:
                        T.op(DVE, lambda e, py=py, dc=dc: e.scalar_tensor_tensor(
                            out=xres[:, dc, :], in0=xres[:, dc, :], scalar=ALPHA, in1=py[:, :], op0=ALU.mult, op1=ALU.add),
                            reads=[pyb, b_xres[dc]], writes=[b_xres[dc]])
                    else:
                        T.op(DVE, lambda e, py=py, dc=dc: e.tensor_tensor(out=xres[:, dc, :], in0=py[:, :], in1=xres[:, dc, :], op=ALU.add),
                             reads=[pyb, b_xres[dc]], writes=[b_xres[dc]])
                    if st is not None:
                        stats_ops(dc, st)
                        if dc >= 5:
                            stats_mm(dc - 5, st)
                rel(wd)
                if st is not None:
                    for d_ in range(NCH - 5, NCH):
                        stats_mm(d_, st)

            st = (banks[0][0], banks[0][1], banks[1][0], banks[1][1], sqt[0], sqt[1])
            gu_mm(0)
            router()
            gu_mm(1)
            gate_mm(0)
            h_ops(0)
            for e_ in range(NEXP):
                if e_ + 1 < NEXP:
                    gate_mm(e_ + 1)
                down_phase(e_, st if e_ == NEXP - 1 else None)
                if e_ + 1 < NEXP:
                    h_ops(e_ + 1)
                if e_ + 2 < NEXP:
                    gu_mm(e_ + 2)
            return st

        for ti in range(NT):
            if ti == 0:
                T.dma(SP, c_xnext, lambda e: e.dma_start(
                    out=xnext[:], in_=xT[:, 0:TT].rearrange("(k p) t -> p k t", p=128)), writes=[b_xnext])
            T.op(ACT, lambda e: e.copy(out=xbf[:, 0:4, :], in_=xnext[:, 0:4, :]), reads=[b_xnext], writes=[b_xbf])
            T.op(DVE, lambda e: e.tensor_copy(out=xbf[:, 4:8, :], in_=xnext[:, 4:8, :]), reads=[b_xnext], writes=[b_xbf])
            if True:
                T.op(ACT, lambda e: e.copy(out=xres[:, 0:4, :], in_=xnext[:, 0:4, :]), reads=[b_xnext], writes=b_xres[0:4])
                T.op(DVE, lambda e: e.tensor_copy(out=xres[:, 4:8, :], in_=xnext[:, 4:8, :]), reads=[b_xnext], writes=b_xres[4:8])
            else:
                T.op(POOL, lambda e: e.tensor_copy(out=xres[:, 0:4, :], in_=xnext[:, 0:4, :]), reads=[b_xnext], writes=b_xres[0:4])
                T.op(POOL, lambda e: e.tensor_copy(out=xres[:, 4:8, :], in_=xnext[:, 4:8, :]), reads=[b_xnext], writes=b_xres[4:8])
            if ti + 1 < NT:
                T.dma(SP, c_xnext, lambda e, ti=ti: e.dma_start(
                    out=xnext[:], in_=xT[:, (ti + 1) * TT:(ti + 2) * TT].rearrange("(k p) t -> p k t", p=128)), writes=[b_xnext])
            stop = False
            pool_ok[0] = (ti > 0)
            for l in range(depth):
                st = mixer(l, ti)
                layer_norm(l, 0, st, use_pool=(ti > 0))
                if dbg == (l, 0):
                    stop = True; break
                st = xattn(l, ti)
                layer_norm(l, 1, st, use_pool=(ti > 0))
                if dbg == (l, 1):
                    stop = True; break
                st = moe(l, ti)
                layer_norm(l, 2, st, use_pool=(ti > 0))
                if dbg == (l, 2):
                    stop = True; break
            if stop:
                T.dma(SP, c_xin, lambda e: e.dma_start(out=dbg_out.rearrange("(k p) t -> p k t", p=128), in_=xres[:]),
                      reads=b_xres)
                break
            T.dma(SP, c_xin, lambda e, ti=ti: e.dma_start(
                out=yT[:, ti * TT:(ti + 1) * TT].rearrange("(k p) t -> p k t", p=128), in_=xres[:]), reads=b_xres)
        T.wait_all(SP, all_ctx)
        with nc.Block() as block:
            block.sync(lambda e: T.replay(SP, e))
            block.tensor(lambda e: T.replay(PE, e))
            block.scalar(lambda e: T.replay(ACT, e))
            block.vector(lambda e: T.replay(DVE, e))
            block.gpsimd(lambda e: T.replay(POOL, e))
    nc._mk_stats = dict(nops=T.nops, nwaits=T.nwaits)
    return nc


def _prep_shared(inp):
    f = lambda a: np.ascontiguousarray(np.asarray(a, dtype=np.float32))
    sh = {}
    sh["w_in"] = f(inp["w_in"])
    sh["w_a2"] = f(inp["w_a2"])
    sh["b_a"] = f(inp["b_a"]).reshape(DEPTH, 1, 256)
    sh["gng_t"] = f(np.asarray(inp["gla_norm_g"]).reshape(DEPTH, 4, 128).transpose(0, 2, 1))
    sh["w_sT"] = f(np.asarray(inp["w_s"]).transpose(0, 1, 3, 2))
    sh["b_s"] = f(inp["b_s"]).reshape(DEPTH, 1, 512)
    sh["slng_t"] = f(np.asarray(inp["sgu_ln_g"]).reshape(DEPTH, 4, 128).transpose(0, 2, 1))
    sh["slnb"] = f(inp["sgu_ln_b"]).reshape(DEPTH, 1, 512)
    for k in ["w_out", "wq_x", "wk_x", "wv_x", "wo_x", "w_router", "w_gate", "w_up", "w_down"]:
        sh[k] = f(inp[k])
    sh["b_router"] = f(inp["b_router"]).reshape(1, NEXP)
    sh["lng_t"] = f(np.asarray(inp["ln_g"]).reshape(DEPTH * 3, NCH, 128).transpose(2, 0, 1).reshape(128, DEPTH * 3 * NCH))
    sh["lnb_t"] = f(np.asarray(inp["ln_b"]).reshape(DEPTH * 3, NCH, 128).transpose(2, 0, 1).reshape(128, DEPTH * 3 * NCH))
    return sh


def kernel(**inputs):
    x = np.asarray(inputs["x"], dtype=np.float32)
    mem = np.asarray(inputs["mem"], dtype=np.float32)
    B, S, _ = x.shape
    sh = _prep_shared(inputs)
    nc = build_nc(S)
    in_maps = []
    for b in range(B):
        m = dict(sh)
        m["xT"] = np.ascontiguousarray(x[b].T)
        m["memT"] = np.ascontiguousarray(mem[b].T)
        in_maps.append(m)
    res = run_bass_kernel_spmd(nc, in_maps, core_ids=list(range(B)))
    out = np.empty((B, S, D), dtype=np.float32)
    for b in range(B):
        out[b] = res.results[b]["yT"].T
    return out
```

```python
import numpy as np
from contextlib import ExitStack
import concourse.bass as bass
import concourse.mybir as mybir
from concourse.bass_utils import run_bass_kernel_spmd

F32 = mybir.dt.float32
BF16 = mybir.dt.bfloat16
AF = mybir.ActivationFunctionType
ALU = mybir.AluOpType
AX = mybir.AxisListType

D = 1024
NCH = 8
TT = 512
DIN = 2576
NEXP = 16
DEPTH = 2
NMEM = 256
ALPHA = float((2.0 * DEPTH) ** 0.25)
LN_EPS = 1e-5
RMS_EPS = 1e-6
RING = 6
SAME_ENG_SYNC = True


class Ctx:
    def __init__(self, name, sem, step):
        self.name = name
        self.sem = sem
        self.step = step
        self.count = 0


class Eng(Ctx):
    def __init__(self, name, sem):
        super().__init__(name, sem, 1)
        self.ops = []
        self.waited = {}


class Buf:
    def __init__(self, name):
        self.name = name
        self.last_write = None
        self.reads = []


def _compact(lst):
    best = {}
    for (c, v) in lst:
        if v > best.get(c, 0):
            best[c] = v
    return list(best.items())


class Tracker:
    def __init__(self):
        self.nwaits = 0
        self.nops = 0

    def _need(self, eng, deps):
        best = {}
        for (c, v) in deps:
            if c is eng and (not SAME_ENG_SYNC or eng.name == "pe" or v > eng.count):
                continue
            if v > best.get(c, 0):
                best[c] = v
        for c, v in best.items():
            if eng.waited.get(c, 0) >= v:
                continue
            eng.waited[c] = v
            eng.ops.append(("wait", c, v))
            self.nwaits += 1

    @staticmethod
    def _deps(reads, writes):
        deps = []
        for b in reads:
            if b.last_write is not None:
                deps.append(b.last_write)
        for b in writes:
            if b.last_write is not None:
                deps.append(b.last_write)
            deps.extend(b.reads)
        return deps

    def op(self, eng, fn, reads=(), writes=(), inc=True):
        self._need(eng, self._deps(reads, writes))
        val = eng.count + 1
        if inc:
            eng.count = val
        eng.ops.append(("op", fn, inc))
        self.nops += 1
        for b in reads:
            b.reads.append((eng, val))
            if len(b.reads) > 48:
                b.reads = _compact(b.reads)
        for b in writes:
            b.last_write = (eng, val)
            b.reads = []

    def dma(self, queue, ctx, fn, reads=(), writes=(), serial=False):
        deps = self._deps(reads, writes)
        if serial and ctx.count > 0:
            deps.append((ctx, ctx.count))
        self._need(queue, deps)
        ctx.count += 16
        val = ctx.count
        queue.ops.append(("dma", fn, ctx))
        self.nops += 1
        for b in reads:
            b.reads.append((ctx, val))
        for b in writes:
            b.last_write = (ctx, val)
            b.reads = []

    def wait_all(self, eng, ctxs):
        for c in ctxs:
            if c.count > 0 and eng.waited.get(c, 0) < c.count:
                eng.waited[c] = c.count
                eng.ops.append(("wait", c, c.count))

    @staticmethod
    def replay(eng, handle):
        for o in eng.ops:
            if o[0] == "wait":
                handle.wait_ge(o[1].sem, o[2])
            elif o[0] == "op":
                ins = o[1](handle)
                if o[2]:
                    ins.then_inc(eng.sem, 1)
            else:
                ins = o[1](handle)
                ins.then_inc(o[2].sem, 16)


def build_nc(S, depth=DEPTH, dbg=None):
    NT = S // TT
    nc = bass.Bass("TRN2", target_bir_lowering=False)

    def din(name, shape):
        return nc.dram_tensor(name, list(shape), F32, kind="ExternalInput").ap()

    xT = din("xT", [D, S])
    memT = din("memT", [D, NMEM])
    w_in = din("w_in", [DEPTH, D, DIN])
    w_a2 = din("w_a2", [DEPTH, 16, 256])
    b_a = din("b_a", [DEPTH, 1, 256])
    gng_t = din("gng_t", [DEPTH, 128, 4])
    w_sT = din("w_sT", [DEPTH, 4, 128, 128])
    b_s = din("b_s", [DEPTH, 1, 512])
    slng_t = din("slng_t", [DEPTH, 128, 4])
    slnb = din("slnb", [DEPTH, 1, 512])
    w_out = din("w_out", [DEPTH, D, D])
    wq_x = din("wq_x", [DEPTH, D, D])
    wk_x = din("wk_x", [DEPTH, D, D])
    wv_x = din("wv_x", [DEPTH, D, D])
    wo_x = din("wo_x", [DEPTH, D, D])
    w_router = din("w_router", [D, NEXP])
    b_router = din("b_router", [1, NEXP])
    w_gate = din("w_gate", [DEPTH, NEXP, D, 512])
    w_up = din("w_up", [DEPTH, NEXP, D, 512])
    w_down = din("w_down", [DEPTH, NEXP, 512, D])
    lng_t = din("lng_t", [128, DEPTH * 3 * NCH])
    lnb_t = din("lnb_t", [128, DEPTH * 3 * NCH])
    yT = nc.dram_tensor("yT", [D, S], F32, kind="ExternalOutput").ap()
    dbg_out = None
    if dbg is not None:
        dbg_out = nc.dram_tensor("dbg", [D, TT], F32, kind="ExternalOutput").ap()

    UPL = 5 + 2 + 2 + 2 + 2 + 2 + 3 * NEXP
    NU = depth * UPL
    wsc = nc.dram_tensor("wscratch", [NU, 128, 4096], BF16, kind="Internal").ap()

    T = Tracker()
    es = ExitStack()
    with es:
        def sb(name, shape, dt=F32):
            return es.enter_context(nc.sbuf_tensor(name, list(shape), dt))

        def sem(name):
            return es.enter_context(nc.semaphore(name))

        PE = Eng("pe", sem("s_pe"))
        ACT = Eng("act", sem("s_act"))
        DVE = Eng("dve", sem("s_dve"))
        POOL = Eng("pool", sem("s_pool"))
        SP = Eng("sp", sem("s_sp"))
        all_ctx = []

        def newctx(name):
            c = Ctx(name, sem("s_" + name), 16)
            all_ctx.append(c)
            return c

        banks = []
        for i in range(8):
            t = es.enter_context(nc.psum_tensor("bank%d" % i, [128, 512], F32))
            banks.append((t, Buf("bank%d" % i)))

        class Rot:
            def __init__(self, ids):
                self.ids = ids
                self.i = 0

            def next(self):
                b = banks[self.ids[self.i % len(self.ids)]]
                self.i += 1
                return b

        ARENA = 66560
        arena = sb("arena", [128, ARENA], mybir.dt.uint8)
        arena_bufs = []

        class Arena:
            def __init__(self):
                self.off = 0
                self.prev = []
                self.cur = []

            def reset(self):
                self.prev = self.prev + self.cur
                hz = []
                for b in self.prev:
                    if b.last_write is not None:
                        hz.append(b.last_write)
                    hz.extend(b.reads)
                self.hz = _compact(hz)
                self.prev = []
                self.cur = []
                self.off = 0
                self.offs = {}

            def tile(self, name, free_shape, dt=F32):
                n = 1
                for s in free_shape:
                    n *= s
                nbytes = n * (4 if dt == F32 else 2)
                nbytes = (nbytes + 63) // 64 * 64
                assert self.off + nbytes <= ARENA, (name, self.off, nbytes)
                v = arena[:, self.off:self.off + nbytes].bitcast(dt)
                if dt == F32:
                    v = arena[:, self.off:self.off + nbytes].bitcast(F32)
                self.off += nbytes
                v = v[:, 0:n]
                if len(free_shape) == 2:
                    v = v.rearrange("p (a b) -> p a b", a=free_shape[0])
                elif len(free_shape) == 3:
                    v = v.rearrange("p (a b c) -> p a b c", a=free_shape[0], b=free_shape[1])
                b = Buf(name)
                b.reads = list(self.hz)
                self.cur.append(b)
                self.offs[name] = self.off - nbytes
                return v, b

            def tile_at(self, name, off, free_shape, dt, inherit):
                save = self.off
                self.off = off
                v, b = self.tile(name, free_shape, dt)
                self.off = save
                for o in inherit:
                    if o.last_write is not None:
                        b.reads.append(o.last_write)
                    b.reads.extend(o.reads)
                return v, b

        AR = Arena()
        AR.hz = []
        AR.offs = {}

        cc = newctx("cc")
        const_bufs = []

        def cload(tile_ap, dram_ap, name):
            b = Buf(name)
            T.dma(SP, cc, lambda e, o=tile_ap, i=dram_ap: e.dma_start(out=o, in_=i), writes=[b])
            const_bufs.append(b)
            return b

        rowi = sb("rowi", [128, 128]); b_rowi = Buf("rowi")
        coli = sb("coli", [128, 128]); b_coli = Buf("coli")
        T.op(POOL, lambda e: e.iota(rowi[:], [[0, 128]], base=0, channel_multiplier=1,
                                    allow_small_or_imprecise_dtypes=True), writes=[b_rowi])
        T.op(POOL, lambda e: e.iota(coli[:], [[1, 128]], base=0, channel_multiplier=0,
                                    allow_small_or_imprecise_dtypes=True), writes=[b_coli])
        identf = sb("identf", [128, 128]); b_identf = Buf("identf")
        T.op(DVE, lambda e: e.tensor_tensor(out=identf[:], in0=rowi[:], in1=coli[:], op=ALU.is_equal),
             reads=[b_rowi, b_coli], writes=[b_identf])
        identb = sb("identb", [128, 128], BF16); b_identb = Buf("identb")
        T.op(DVE, lambda e: e.tensor_copy(out=identb[:], in_=identf[:]), reads=[b_identf], writes=[b_identb])
        maskd = sb("maskd", [128, 128]); b_maskd = Buf("maskd")
        tmpa = sb("tmpa", [128, 128]); b_tmpa = Buf("tmpa")
        tmpb = sb("tmpb", [128, 128]); b_tmpb = Buf("tmpb")
        T.op(DVE, lambda e: e.tensor_tensor(out=maskd[:], in0=rowi[:], in1=coli[:], op=ALU.is_gt),
             reads=[b_rowi, b_coli], writes=[b_maskd])
        T.op(DVE, lambda e: e.tensor_single_scalar(out=tmpa[:], in_=rowi[:], scalar=64.0, op=ALU.is_ge),
             reads=[b_rowi], writes=[b_tmpa])
        T.op(DVE, lambda e: e.tensor_single_scalar(out=tmpb[:], in_=coli[:], scalar=64.0, op=ALU.is_ge),
             reads=[b_coli], writes=[b_tmpb])
        T.op(DVE, lambda e: e.tensor_tensor(out=tmpb[:], in0=tmpa[:], in1=tmpb[:], op=ALU.is_equal),
             reads=[b_tmpa, b_tmpb], writes=[b_tmpb])
        T.op(DVE, lambda e: e.scalar_tensor_tensor(out=maskd[:], in0=maskd[:], scalar=-1.0 / 16.0, in1=tmpb[:],
                                                   op0=ALU.mult, op1=ALU.mult),
             reads=[b_maskd, b_tmpb], writes=[b_maskd])
        csel = sb("csel", [128, 16]); b_csel = Buf("csel")
        T.op(POOL, lambda e: e.memset(csel[:], 0.0), writes=[b_csel])
        T.op(DVE, lambda e: e.tensor_scalar(out=csel[:, 1:2], in0=tmpa[:, 0:1], scalar1=-1.0 / 16.0, scalar2=None,
                                            op0=ALU.mult), reads=[b_tmpa], writes=[b_csel])
        T.op(DVE, lambda e: e.tensor_scalar(out=csel[:, 0:1], in0=tmpa[:, 0:1], scalar1=1.0 / 16.0,
                                            scalar2=-1.0 / 16.0, op0=ALU.mult, op1=ALU.add),
             reads=[b_tmpa], writes=[b_csel])
        onesb = sb("onesb", [128, 128], BF16); b_onesb = Buf("onesb")
        T.op(POOL, lambda e: e.memset(onesb[:], 1.0), writes=[b_onesb])
        onesm = sb("onesm", [128, 128], BF16); b_onesm = Buf("onesm")
        T.op(POOL, lambda e: e.memset(onesm[:], 1.0 / 1024.0), writes=[b_onesm])
        onesr = sb("onesr", [1, 128]); b_onesr = Buf("onesr")
        T.op(POOL, lambda e: e.memset(onesr[:], 1.0), writes=[b_onesr])
        onescol = sb("onescol", [128, 1]); b_onescol = Buf("onescol")
        T.op(POOL, lambda e: e.memset(onescol[:], 1.0), writes=[b_onescol])

        lng = sb("lng", [128, DEPTH * 3 * NCH]); b_lng = cload(lng[:], lng_t, "lng")
        lnb = sb("lnb", [128, DEPTH * 3 * NCH]); b_lnb = cload(lnb[:], lnb_t, "lnb")
        wr = sb("wr", [128, NCH, NEXP]); b_wr = cload(wr[:], w_router.rearrange("(k p) e -> p k e", p=128), "wr")
        br = sb("br", [1, NEXP]); b_br = cload(br[:], b_router, "br")
        wa2 = sb("wa2", [32, depth, 256]); b_wa2 = Buf("wa2")
        alwf = sb("alwf", [128, depth, NCH, 16]); b_alwf = Buf("alwf")
        gng = sb("gng", [128, depth, 4]); b_gng = Buf("gng")
        slng = sb("slng", [128, depth, 4]); b_slng = Buf("slng")
        wsf, b_wsf = AR.tile("wsf", [depth, 4, 128])
        bsr, b_bsr = AR.tile("bsr", [depth, 512])
        slnbr, b_slnbr = AR.tile("slnbr", [depth, 512])
        for l in range(depth):
            T.dma(SP, cc, lambda e, l=l: e.dma_start(out=wa2[0:16, l, :], in_=w_a2[l]), writes=[b_wa2])
            T.dma(SP, cc, lambda e, l=l: e.dma_start(out=wa2[16:17, l, :], in_=b_a[l]), writes=[b_wa2])
            T.dma(SP, cc, lambda e, l=l: e.dma_start(
                out=alwf[:, l, :, :], in_=w_in[l, :, 1536:1552].rearrange("(k p) c -> p k c", p=128)), writes=[b_alwf])
            T.dma(SP, cc, lambda e, l=l: e.dma_start(out=gng[:, l, :], in_=gng_t[l]), writes=[b_gng])
            T.dma(SP, cc, lambda e, l=l: e.dma_start(out=slng[:, l, :], in_=slng_t[l]), writes=[b_slng])
            T.dma(SP, cc, lambda e, l=l: e.dma_start(
                out=wsf[:, l, :, :], in_=w_sT[l].rearrange("g s t -> s g t")), writes=[b_wsf])
            T.dma(SP, cc, lambda e, l=l: e.dma_start(out=bsr[0:1, l, :], in_=b_s[l]), writes=[b_bsr])
            T.dma(SP, cc, lambda e, l=l: e.dma_start(out=slnbr[0:1, l, :], in_=slnb[l]), writes=[b_slnbr])
        memf, b_memf = AR.tile("memf", [NCH, NMEM])
        T.dma(SP, cc, lambda e: e.dma_start(out=memf, in_=memT.rearrange("(k p) m -> p k m", p=128)), writes=[b_memf])
        for b in const_bufs + [b_wa2, b_alwf, b_gng, b_slng, b_wsf, b_bsr, b_slnbr, b_memf]:
            b.last_write = (cc, cc.count)

        alwb = sb("alwb", [128, depth, NCH, 16], BF16); b_alwb = Buf("alwb")
        T.op(DVE, lambda e: e.tensor_copy(out=alwb[:], in_=alwf[:]), reads=[b_alwf], writes=[b_alwb])
        T.op(DVE, lambda e: e.tensor_scalar(out=gng[:], in0=gng[:], scalar1=float(np.sqrt(128.0)), scalar2=None,
                                            op0=ALU.mult), reads=[b_gng], writes=[b_gng])
        wmT = sb("wmT", [128, depth, 4, 128], BF16); b_wmT = Buf("wmT")
        for l in range(depth):
            for g in range(4):
                T.op(POOL, lambda e, l=l, g=g: e.memset(wsf[64:128, l, g, 0:64], 0.0), reads=[], writes=[b_wsf])
        T.op(DVE, lambda e: e.tensor_copy(out=wmT[:], in_=wsf), reads=[b_wsf], writes=[b_wmT])
        memb, b_memb = AR.tile("memb", [NCH, NMEM], BF16)
        T.op(DVE, lambda e: e.tensor_copy(out=memb, in_=memf), reads=[b_memf], writes=[b_memb])
        bterm = sb("bterm", [128, depth, 4, 128]); b_bterm = Buf("bterm")
        rsr = sb("rsr", [1, 128]); b_rsr = Buf("rsr")
        crot = Rot([0, 1])
        for l in range(depth):
            for g in range(4):
                pt, pb = crot.next()
                T.op(PE, lambda e, pt=pt, l=l, g=g: e.matmul(pt[0:1, 0:128], onescol[:, 0:1], wsf[:, l, g, :],
                                                              start=True, stop=True),
                     reads=[b_onescol, b_wsf], writes=[pb])
                T.op(ACT, lambda e, pt=pt: e.copy(out=rsr[:], in_=pt[0:1, 0:128]), reads=[pb], writes=[b_rsr])
                pt2, pb2 = crot.next()
                T.op(PE, lambda e, pt2=pt2, l=l, g=g: e.matmul(pt2[:, 0:128], slnbr[0:1, l, g * 128:(g + 1) * 128],
                                                                rsr[0:1, :], start=True, stop=False),
                     reads=[b_slnbr, b_rsr], writes=[pb2], inc=False)
                T.op(PE, lambda e, pt2=pt2, l=l, g=g: e.matmul(pt2[:, 0:128], onesr[0:1, :],
                                                                bsr[0:1, l, g * 128:(g + 1) * 128],
                                                                start=False, stop=True),
                     reads=[b_onesr, b_bsr], writes=[pb2])
                T.op(ACT, lambda e, pt2=pt2, l=l, g=g: e.copy(out=bterm[:, l, g, :], in_=pt2[:, 0:128]),
                     reads=[pb2], writes=[b_bterm])

        castctx = [newctx("cast%d" % i) for i in range(8)]
        unit_buf = [Buf("unit%d" % u) for u in range(NU)]
        cast_i = [0]

        def kp(ap2d):
            return ap2d.rearrange("(k p) c -> p k c", p=128)

        UIDX = {}
        USRC = {}
        for l in range(depth):
            base = l * UPL
            names = ["wk0", "wk1", "wv0", "wv1", "inA", "inB", "inC", "inD", "inE", "wout0", "wout1",
                     "wq0", "wq1", "wo0", "wo1"]
            for i, n in enumerate(names):
                UIDX[(l, n)] = base + i
            USRC[(l, "wk0")] = (kp(wk_x[l, :, 0:512]), 8); USRC[(l, "wk1")] = (kp(wk_x[l, :, 512:1024]), 8)
            USRC[(l, "wv0")] = (kp(wv_x[l, :, 0:512]), 8); USRC[(l, "wv1")] = (kp(wv_x[l, :, 512:1024]), 8)
            USRC[(l, "inA")] = (kp(w_in[l, :, 0:512]), 8); USRC[(l, "inB")] = (kp(w_in[l, :, 512:1024]), 8)
            USRC[(l, "inC")] = (kp(w_in[l, :, 1024:1536]), 8); USRC[(l, "inD")] = (kp(w_in[l, :, 1552:2064]), 8)
            USRC[(l, "inE")] = (kp(w_in[l, :, 2064:2576]), 8)
            USRC[(l, "wout0")] = (kp(w_out[l, :, 0:512]), 8); USRC[(l, "wout1")] = (kp(w_out[l, :, 512:1024]), 8)
            USRC[(l, "wq0")] = (kp(wq_x[l, :, 0:512]), 8); USRC[(l, "wq1")] = (kp(wq_x[l, :, 512:1024]), 8)
            USRC[(l, "wo0")] = (kp(wo_x[l, :, 0:512]), 8); USRC[(l, "wo1")] = (kp(wo_x[l, :, 512:1024]), 8)
            for e_ in range(NEXP):
                UIDX[(l, "wg", e_)] = base + 15 + 3 * e_
                UIDX[(l, "wu", e_)] = base + 15 + 3 * e_ + 1
                UIDX[(l, "wd", e_)] = base + 15 + 3 * e_ + 2
                USRC[(l, "wg", e_)] = (kp(w_gate[l, e_]), 8)
                USRC[(l, "wu", e_)] = (kp(w_up[l, e_]), 8)
                USRC[(l, "wd", e_)] = (kp(w_down[l, e_]), 4)

        cast_done = set()

        def emit_cast(key):
            if key in cast_done:
                return
            cast_done.add(key)
            u = UIDX[key]
            src, k = USRC[key]
            c = castctx[cast_i[0] % len(castctx)]
            cast_i[0] += 1
            kh = k // 2
            for hh in range(2):
                T.dma(POOL, c, lambda e, u=u, s=src, k=k, hh=hh, kh=kh: e.dma_start(
                    out=wsc[u].rearrange("p (k c) -> p k c", k=k)[:, hh * kh:(hh + 1) * kh, :],
                    in_=s[:, hh * kh:(hh + 1) * kh, :]), writes=[unit_buf[u]], serial=True)

        ring = []
        for i in range(RING):
            t = sb("ring%d" % i, [128, 4096], BF16)
            ring.append((t, Buf("ring%d" % i), newctx("ring%d" % i)))

        stream = []
        for l in range(depth):
            for n in ["wk0", "wk1", "wv0", "wv1"]:
                stream.append((l, n))
        for ti in range(NT):
            for l in range(depth):
                for n in ["inE", "inA", "inB", "inD", "inC", "wout0", "wout1", "wq0", "wq1", "wo0", "wo1"]:
                    stream.append((l, n))
                for e_ in range(NEXP):
                    stream.append((l, "wg", e_)); stream.append((l, "wu", e_)); stream.append((l, "wd", e_))
        issued = [0]
        taken = [0]
        casted = [0]
        released = set()
        loaded = {}
        LOOKAHEAD = RING - 1
        CASTAHEAD = 10

        def pump():
            while casted[0] < len(stream) and casted[0] < taken[0] + LOOKAHEAD + CASTAHEAD:
                emit_cast(stream[casted[0]])
                casted[0] += 1
            while issued[0] < len(stream) and issued[0] < taken[0] + LOOKAHEAD:
                k = issued[0]
                if k >= RING and (k - RING) not in released:
                    break
                u = UIDX[stream[k]]
                t, b, c = ring[k % RING]
                T.dma(SP, c, lambda e, t=t, u=u: e.dma_start(out=t[:], in_=wsc[u]), reads=[unit_buf[u]], writes=[b])
                loaded[k] = (t, b, k)
                issued[0] += 1

        def take(key):
            k = taken[0]
            assert stream[k] == key, (stream[k], key)
            pump()
            assert k in loaded, ("ring stall", key, k, issued[0], sorted(released)[-4:])
            r = loaded.pop(k)
            taken[0] += 1
            pump()
            return r

        def rel(unit):
            released.add(unit[2])
            pump()

        xres = sb("xres", [128, NCH, TT]); b_xres = [Buf("xres%d" % c) for c in range(NCH)]
        lt = [(sb("lt%d" % i, [128, TT]), Buf("lt%d" % i)) for i in range(2)]
        xbf = sb("xbf", [128, NCH, TT], BF16); b_xbf = Buf("xbf")
        xpre = xres
        c_xin = newctx("xin")
        c_xnext = newctx("xnext")
        xnext = sb("xnext", [128, NCH, TT]); b_xnext = Buf("xnext")
        KT = sb("KT", [128, depth, NCH, NMEM], BF16); b_KT = Buf("KT")
        Vm = sb("Vm", [128, depth, 2, D], BF16); b_Vm = Buf("Vm")
        Sst = sb("Sst", [128, depth, 2, 256]); b_S = [[Buf("S%d%d" % (l, j)) for j in range(2)] for l in range(depth)]
        T.op(POOL, lambda e: e.memset(Sst[:], 0.0), writes=[b for row in b_S for b in row])
        ltp = (sb("ltp", [128, TT]), Buf("ltp"))
        lnv = sb("lnv", [128, TT]); b_lnv = Buf("lnv")
        lnr = sb("lnr", [128, TT]); b_lnr = Buf("lnr")
        lnn = sb("lnn", [128, TT]); b_lnn = Buf("lnn")

        kvrot = Rot([2, 3, 4, 5])
        for l in range(depth):
            wk_t = [take((l, "wk0")), take((l, "wk1"))]
            for ec in range(NCH):
                wt, wb, _ = wk_t[ec // 4]
                wv = wt[:].rearrange("p (k c) -> p k c", k=8)
                pt, pb = kvrot.next()
                for kc in range(NCH):
                    T.op(PE, lambda e, pt=pt, wv=wv, kc=kc, ec=ec: e.matmul(
                        pt[:, 0:NMEM], wv[:, kc, (ec % 4) * 128:(ec % 4 + 1) * 128], memb[:, kc, :],
                        start=(kc == 0), stop=(kc == NCH - 1)),
                        reads=[wb, b_memb], writes=[pb], inc=(kc == NCH - 1))
                T.op(ACT, lambda e, pt=pt, l=l, ec=ec: e.mul(out=KT[:, l, ec, :], in_=pt[:, 0:NMEM], mul=1.0 / 16.0),
                     reads=[pb], writes=[b_KT])
            rel(wk_t[0]); rel(wk_t[1])
            wv_t = [take((l, "wv0")), take((l, "wv1"))]
            for mc in range(2):
                for hf in range(2):
                    wt, wb, _ = wv_t[hf]
                    wv = wt[:].rearrange("p (k c) -> p k c", k=8)
                    pt, pb = kvrot.next()
                    for kc in range(NCH):
                        T.op(PE, lambda e, pt=pt, wv=wv, kc=kc, mc=mc: e.matmul(
                            pt[:, :], memb[:, kc, mc * 128:(mc + 1) * 128], wv[:, kc, :],
                            start=(kc == 0), stop=(kc == NCH - 1)),
                            reads=[wb, b_memb], writes=[pb], inc=(kc == NCH - 1))
                    T.op(DVE, lambda e, pt=pt, l=l, mc=mc, hf=hf: e.tensor_copy(
                        out=Vm[:, l, mc, hf * 512:(hf + 1) * 512], in_=pt[:, :]), reads=[pb], writes=[b_Vm])
            rel(wv_t[0]); rel(wv_t[1])

        pool_ok = [False]

        def stats_ops(dc, st):
            pm, pmb, pv, pvb, sqb, b_sqb = st
            T.op(ACT, lambda e, dc=dc: e.copy(out=xbf[:, dc, :], in_=xres[:, dc, :]), reads=[b_xres[dc]], writes=[b_xbf])
            if False:
                T.op(POOL, lambda e, dc=dc, sqb=sqb: e.tensor_tensor(out=sqb[:, dc, :], in0=xres[:, dc, :], in1=xres[:, dc, :], op=ALU.mult),
                     reads=[b_xres[dc]], writes=[b_sqb])
            else:
                T.op(ACT, lambda e, dc=dc, sqb=sqb: e.activation(out=sqb[:, dc, :], in_=xres[:, dc, :], func=AF.Square),
                     reads=[b_xres[dc]], writes=[b_sqb])

        def stats_mm(dc, st):
            pm, pmb, pv, pvb, sqb, b_sqb = st
            T.op(PE, lambda e, dc=dc, pm=pm: e.matmul(pm[:, :], onesm[:], xbf[:, dc, :], start=(dc == 0), stop=(dc == NCH - 1)),
                 reads=[b_onesm, b_xbf], writes=[pmb], inc=(dc == NCH - 1))
            T.op(PE, lambda e, dc=dc, pv=pv, sqb=sqb: e.matmul(pv[:, :], onesm[:], sqb[:, dc, :], start=(dc == 0), stop=(dc == NCH - 1)),
                 reads=[b_onesm, b_sqb], writes=[pvb], inc=(dc == NCH - 1))

        def layer_norm(l, which, st, use_pool=True, final=False):
            col0 = (l * 3 + which) * NCH
            pm, pmb, pv, pvb, sqb, b_sqb = st
            T.op(ACT, lambda e, pm=pm: e.activation(out=lnv[:], in_=pm[:, :], func=AF.Square), reads=[pmb], writes=[b_lnv])
            T.op(DVE, lambda e, pv=pv: e.tensor_tensor(out=lnv[:], in0=pv[:, :], in1=lnv[:], op=ALU.subtract),
                 reads=[pvb, b_lnv], writes=[b_lnv])
            T.op(DVE, lambda e: e.tensor_scalar(out=lnv[:], in0=lnv[:], scalar1=0.0, scalar2=LN_EPS, op0=ALU.max, op1=ALU.add),
                 reads=[b_lnv], writes=[b_lnv])
            T.op(ACT, lambda e: e.activation(out=lnr[:], in_=lnv[:], func=AF.Ln), reads=[b_lnv], writes=[b_lnr])
            T.op(ACT, lambda e: e.activation(out=lnr[:], in_=lnr[:], func=AF.Exp, scale=-0.5), reads=[b_lnr], writes=[b_lnr])
            T.op(DVE, lambda e, pm=pm: e.scalar_tensor_tensor(out=lnn[:], in0=pm[:, :], scalar=-1.0, in1=lnr[:], op0=ALU.mult, op1=ALU.mult),
                 reads=[pmb, b_lnr], writes=[b_lnn])
            for c in range(NCH):
                T.op(DVE, lambda e, c=c: e.tensor_tensor(out=xres[:, c, :], in0=xres[:, c, :], in1=lnr[:], op=ALU.mult),
                     reads=[b_xres[c], b_lnr], writes=[b_xres[c]])
                T.op(DVE, lambda e, c=c: e.tensor_tensor(out=xres[:, c, :], in0=xres[:, c, :], in1=lnn[:], op=ALU.add),
                     reads=[b_xres[c], b_lnn], writes=[b_xres[c]])
                if final:
                    T.op(ACT, lambda e, c=c: e.activation(out=xres[:, c, :], in_=xres[:, c, :], func=AF.Identity,
                                                          scale=lng[:, col0 + c:col0 + c + 1], bias=lnb[:, col0 + c:col0 + c + 1]),
                         reads=[b_xres[c], b_lng, b_lnb], writes=[b_xres[c]])
                    continue
                T.op(ACT, lambda e, c=c: e.activation(out=xbf[:, c, :], in_=xres[:, c, :], func=AF.Identity,
                                                      scale=lng[:, col0 + c:col0 + c + 1], bias=lnb[:, col0 + c:col0 + c + 1]),
                     reads=[b_xres[c], b_lng, b_lnb], writes=[b_xbf])
            if final:
                return
            for c in range(NCH):
                T.op(ACT, lambda e, c=c: e.activation(out=xres[:, c, :], in_=xres[:, c, :], func=AF.Identity,
                                                      scale=lng[:, col0 + c:col0 + c + 1], bias=lnb[:, col0 + c:col0 + c + 1]),
                     reads=[b_xres[c], b_lng, b_lnb], writes=[b_xres[c]])

        def proj_residual(units, src, b_src, rot, st):
            for dc in range(NCH):
                wt, wb, _ = units[dc // 4]
                wv = wt[:].rearrange("p (k c) -> p k c", k=8)
                pt, pb = rot.next()
                for ic in range(NCH):
                    T.op(PE, lambda e, pt=pt, wv=wv, ic=ic, dc=dc: e.matmul(
                        pt[:, :], wv[:, ic, (dc % 4) * 128:(dc % 4 + 1) * 128], src[:, ic, :],
                        start=(ic == 0), stop=(ic == NCH - 1)),
                        reads=[wb, b_src], writes=[pb], inc=(ic == NCH - 1))
                T.op(DVE, lambda e, pt=pt, dc=dc: e.scalar_tensor_tensor(
                    out=xpre[:, dc, :], in0=xres[:, dc, :], scalar=ALPHA, in1=pt[:, :], op0=ALU.mult, op1=ALU.add),
                    reads=[b_xres[dc], pb], writes=[b_xres[dc]])
                stats_ops(dc, st)
                if dc >= 5:
                    stats_mm(dc - 5, st)
                if dc % 4 == 3:
                    rel(units[dc // 4])
            for d_ in range(NCH - 5, NCH):
                stats_mm(d_, st)

        def mixer(l, ti, pre_out=None):
            AR.reset()
            ksb, b_ksb = AR.tile("ksb", [4, 256])
            e1 = [AR.tile("e1_%d" % i, [256]) for i in range(2)]
            lbuf, b_lbuf = AR.tile("lbuf", [4, 256])
            edb = [AR.tile("ed_%d" % i, [256]) for i in range(2)]
            r_off = AR.offs["ksb"]
            r_bufs = [b_ksb, e1[0][1], e1[1][1], b_lbuf, edb[0][1], edb[1][1]]
            vbf, b_vbf = AR.tile("vbf", [4, 512], BF16)
            gv, b_gv = AR.tile("gv", [4, 512])
            gsq, b_gsq = AR.tile("gsq", [4, 512])
            vn, b_vn = AR.tile("vn", [4, 512], BF16)
            alow, b_alow = AR.tile("alow", [TT])
            qT, b_qT = AR.tile("qT", [2, TT], BF16)
            gu, b_gu = AR.tile("gu", [4, TT])
            kdec, b_kdec = AR.tile("kdec", [4, 256], BF16)
            decay, b_decay = AR.tile("decay", [128])
            sbf = [[AR.tile("sbf%d%d" % (i, j), [256], BF16) for j in range(2)] for i in range(2)]
            ymix, b_ymix = AR.tile("ymix", [8, TT], BF16)
            st1, b_st1 = AR.tile("st1", [16])
            st2, b_st2 = AR.tile("st2", [16])
            st3, b_st3 = AR.tile("st3", [16])
            stmp = [AR.tile("stmp%d" % i, [4, 128]) for i in range(2)]

            rotA = Rot([0, 1, 2, 3])

            def w8(u):
                return u[0][:].rearrange("p (k c) -> p k c", k=8), u[1]

            def fm_proj(wv, wb, col, M, evac):
                pt, pb = rotA.next()
                for kc in range(NCH):
                    T.op(PE, lambda e, pt=pt, kc=kc: e.matmul(pt[0:M, :], wv[:, kc, col:col + M], xbf[:, kc, :],
                                                             start=(kc == 0), stop=(kc == NCH - 1)),
                         reads=[wb, b_xbf], writes=[pb], inc=(kc == NCH - 1))
                evac(pt, pb)

            def tm_proj(wv, wb, col, N, blk, evac):
                pt, pb = rotA.next()
                for kc in range(NCH):
                    T.op(PE, lambda e, pt=pt, kc=kc: e.matmul(pt[:, 0:N], xbf[:, kc, blk * 128:(blk + 1) * 128],
                                                             wv[:, kc, col:col + N],
                                                             start=(kc == 0), stop=(kc == NCH - 1)),
                         reads=[wb, b_xbf], writes=[pb], inc=(kc == NCH - 1))
                evac(pt, pb)

            T.op(DVE, lambda e: e.memset(alow[0:32, :], 1.0), writes=[b_alow])
            uE = take((l, "inE"))
            wE, bE = w8(uE)
            for blk in range(4):
                tm_proj(wE, bE, 0, 512, blk, lambda pt, pb, blk=blk: T.op(
                    ACT, lambda e: e.activation(out=gv[:, blk, :], in_=pt[:, :], func=AF.Gelu_apprx_tanh), reads=[pb], writes=[b_gv]))
            rel(uE)
            gvv = gv.rearrange("p a (g c) -> p (a g) c", g=4)
            gsqv = gsq.rearrange("p a (g c) -> p (a g) c", g=4)
            vnv = vn.rearrange("p a (g c) -> p (a g) c", g=4)
            T.op(DVE, lambda e: e.tensor_reduce(out=st1, in_=gvv, axis=AX.X, op=ALU.add), reads=[b_gv], writes=[b_st1])
            T.op(DVE, lambda e: e.tensor_tensor(out=gsq, in0=gv, in1=gv, op=ALU.mult), reads=[b_gv], writes=[b_gsq])
            T.op(DVE, lambda e: e.tensor_reduce(out=st2, in_=gsqv, axis=AX.X, op=ALU.add), reads=[b_gsq], writes=[b_st2])
            T.op(DVE, lambda e: e.tensor_scalar(out=st1, in0=st1, scalar1=1.0 / 128.0, scalar2=None, op0=ALU.mult),
                 reads=[b_st1], writes=[b_st1])
            T.op(DVE, lambda e: e.tensor_tensor(out=st3, in0=st1, in1=st1, op=ALU.mult), reads=[b_st1], writes=[b_st3])
            T.op(DVE, lambda e: e.scalar_tensor_tensor(out=st2, in0=st2, scalar=1.0 / 128.0, in1=st3, op0=ALU.mult, op1=ALU.subtract),
                 reads=[b_st2, b_st3], writes=[b_st2])
            T.op(DVE, lambda e: e.tensor_scalar(out=st2, in0=st2, scalar1=0.0, scalar2=None, op0=ALU.max), reads=[b_st2], writes=[b_st2])
            T.op(ACT, lambda e: e.activation(out=st2, in_=st2, func=AF.Sqrt, bias=LN_EPS, scale=1.0), reads=[b_st2], writes=[b_st2])
            T.op(DVE, lambda e: e.reciprocal(out=st2, in_=st2), reads=[b_st2], writes=[b_st2])
            T.op(DVE, lambda e: e.scalar_tensor_tensor(out=st3, in0=st1, scalar=-1.0, in1=st2, op0=ALU.mult, op1=ALU.mult),
                 reads=[b_st1, b_st2], writes=[b_st3])
            T.op(DVE, lambda e: e.tensor_tensor(out=gsqv, in0=gvv, in1=st2.unsqueeze(2).to_broadcast([128, 16, 128]), op=ALU.mult),
                 reads=[b_gv, b_st2], writes=[b_gsq])
            T.op(DVE, lambda e: e.tensor_tensor(out=vnv, in0=gsqv, in1=st3.unsqueeze(2).to_broadcast([128, 16, 128]), op=ALU.add),
                 reads=[b_gsq, b_st3], writes=[b_vn])

            alw_v = alwb[:, l, :, :]
            fm_proj(alw_v, b_alwb, 0, 16, lambda pt, pb: T.op(
                ACT, lambda e: e.copy(out=alow[0:16, :], in_=pt[0:16, :]), reads=[pb], writes=[b_alow]))
            uA = take((l, "inA"))
            wA, bA = w8(uA)
            for blk in range(4):
                tm_proj(wA, bA, 256, 256, blk, lambda pt, pb, blk=blk: T.op(
                    ACT, lambda e: e.copy(out=ksb[:, blk, :], in_=pt[:, 0:256]), reads=[pb], writes=[b_ksb]))
            for mc in range(2):
                fm_proj(wA, bA, mc * 128, 128, lambda pt, pb, mc=mc: T.op(
                    ACT, lambda e: e.mul(out=qT[:, mc, :], in_=pt[:, :], mul=0.125), reads=[pb], writes=[b_qT]))
            rel(uA)
            uB = take((l, "inB"))
            wB, bB = w8(uB)
            wa2v = wa2[0:17, l, :]
            bl_bank, bl_buf = banks[7]
            for blk in range(4):
                pz, pzb = rotA.next()
                T.op(PE, lambda e, pz=pz, blk=blk: e.matmul(pz[:, 0:256], alow[0:17, blk * 128:(blk + 1) * 128], wa2v,
                                                            start=True, stop=True),
                     reads=[b_alow, b_wa2], writes=[pzb])
                e1t, e1b = e1[blk % 2]
                T.op(ACT, lambda e, pz=pz, e1t=e1t: e.activation(out=e1t, in_=pz[:, 0:256], func=AF.Exp, scale=-1.0),
                     reads=[pzb], writes=[e1b])
                T.op(ACT, lambda e, e1t=e1t, blk=blk: e.activation(out=lbuf[:, blk, :], in_=e1t, func=AF.Ln, bias=1.0, scale=1.0),
                     reads=[e1b], writes=[b_lbuf])
                tm_proj(wB, bB, 0, 512, blk, lambda pt, pb, blk=blk: T.op(
                    ACT, lambda e: e.copy(out=vbf[:, blk, :], in_=pt[:, :]), reads=[pb], writes=[b_vbf]))
                pd, pdb = rotA.next()
                T.op(PE, lambda e, pd=pd, blk=blk: e.matmul(pd[:, 0:256], maskd[:], lbuf[:, blk, :], start=True, stop=True),
                     reads=[b_maskd, b_lbuf], writes=[pdb])
                edt, edbuf = edb[blk % 2]
                T.op(ACT, lambda e, pd=pd, edt=edt: e.activation(out=edt, in_=pd[:, 0:256], func=AF.Exp),
                     reads=[pdb], writes=[edbuf])
                T.op(DVE, lambda e, edt=edt, blk=blk: e.tensor_tensor(out=kdec[:, blk, :], in0=ksb[:, blk, :], in1=edt, op=ALU.mult),
                     reads=[b_ksb, edbuf], writes=[b_kdec])
                for j in range(2):
                    cidx = (blk * 2 + j) * 16
                    T.op(PE, lambda e, blk=blk, j=j, cidx=cidx: e.matmul(
                        bl_bank[:, cidx:cidx + 16], lbuf[:, blk, j * 128:(j + 1) * 128], csel[:, :], start=True, stop=True),
                        reads=[b_lbuf, b_csel], writes=[bl_buf], inc=True)
            rel(uB)
            T.op(ACT, lambda e: e.activation(out=decay, in_=bl_bank[:, 0:128], func=AF.Exp), reads=[bl_buf], writes=[b_decay])
            uD = take((l, "inD"))
            wD, bD = w8(uD)
            for mc in range(4):
                fm_proj(wD, bD, mc * 128, 128, lambda pt, pb, mc=mc: T.op(
                    ACT, lambda e: e.activation(out=gu[:, mc, :], in_=pt[:, :], func=AF.Gelu_apprx_tanh), reads=[pb], writes=[b_gu]))
            rel(uD)
            for g in range(4):
                pm, pmb = rotA.next()
                for blk in range(4):
                    T.op(PE, lambda e, pm=pm, g=g, blk=blk: e.matmul(
                        pm[:, blk * 128:(blk + 1) * 128], vn[:, blk, g * 128:(g + 1) * 128], wmT[:, l, g, :],
                        start=True, stop=True), reads=[b_vn, b_wmT], writes=[pmb], inc=(blk == 3))
                sm, smb = stmp[g % 2]
                T.op(DVE, lambda e, pm=pm, g=g, sm=sm: e.scalar_tensor_tensor(
                    out=sm, in0=pm[:, :].rearrange("p (a t) -> p a t", a=4), scalar=slng[:, l, g:g + 1],
                    in1=bterm[:, l, g, :].unsqueeze(1).to_broadcast([128, 4, 128]), op0=ALU.mult, op1=ALU.add),
                    reads=[pmb, b_slng, b_bterm], writes=[smb])
                T.op(DVE, lambda e, g=g, sm=sm: e.tensor_tensor(
                    out=ymix[:, 4 + g, :], in0=sm.rearrange("p a t -> p (a t)"), in1=gu[:, g, :], op=ALU.mult),
                    reads=[smb, b_gu], writes=[b_ymix])
            sr, b_sr = AR.tile_at("sr", AR.offs["gsq"], [4, TT], F32, inherit=[b_gsq])
            osq, b_osq = AR.tile_at("osq", r_off, [4, TT], BF16, inherit=r_bufs)
            rinv = [AR.tile_at("rinv%d" % i, r_off + 4096 + 2048 * i, [TT], F32, inherit=r_bufs) for i in range(2)]
            t1 = [AR.tile_at("t1_%d" % i, r_off + 8192 + 2048 * i, [TT], F32, inherit=r_bufs) for i in range(2)]
            uC = take((l, "inC"))
            wC, bC = w8(uC)
            oT = [banks[4 + h] for h in range(4)]
            for c in range(8):
                blk, half = c // 2, c % 2
                r0, r1 = half * 64, half * 64 + 64
                for j in range(2):
                    pk, pkb = rotA.next()
                    T.op(PE, lambda e, pk=pk, blk=blk, j=j, r0=r0, r1=r1: e.matmul(
                        pk[:, 0:256], kdec[r0:r1, blk, j * 128:(j + 1) * 128], vbf[r0:r1, blk, j * 256:(j + 1) * 256],
                        start=True, stop=True), reads=[b_kdec, b_vbf], writes=[pkb])
                    dcol = (blk * 2 + j) * 16 + half
                    T.op(DVE, lambda e, pk=pk, j=j, dcol=dcol: e.scalar_tensor_tensor(
                        out=Sst[:, l, j, :], in0=Sst[:, l, j, :], scalar=decay[:, dcol:dcol + 1], in1=pk[:, 0:256],
                        op0=ALU.mult, op1=ALU.add), reads=[b_S[l][j], b_decay, pkb], writes=[b_S[l][j]])
                    st, stb = sbf[c % 2][j]
                    T.op(ACT, lambda e, st=st, j=j: e.copy(out=st, in_=Sst[:, l, j, :]), reads=[b_S[l][j]], writes=[stb])
                if c % 2 == 0:
                    mc = c // 2
                    fm_proj(wC, bC, mc * 128, 128, lambda pt, pb, mc=mc: T.op(
                        ACT, lambda e: e.activation(out=sr[:, mc, :], in_=pt[:, :], func=AF.Silu), reads=[pb], writes=[b_sr]))
                for j in range(2):
                    st, stb = sbf[c % 2][j]
                    hA, hB = 2 * j, 2 * j + 1
                    T.op(PE, lambda e, st=st, j=j, c=c, hA=hA: e.matmul(
                        oT[hA][0][:, c * 64:(c + 1) * 64], st[0:64, 0:128], qT[0:64, j, c * 64:(c + 1) * 64],
                        start=True, stop=True), reads=[stb, b_qT], writes=[oT[hA][1]])
                    T.op(PE, lambda e, st=st, j=j, c=c, hB=hB: e.matmul(
                        oT[hB][0][:, c * 64:(c + 1) * 64], st[64:128, 128:256], qT[64:128, j, c * 64:(c + 1) * 64],
                        start=True, stop=True), reads=[stb, b_qT], writes=[oT[hB][1]])
            rel(uC)
            for h in range(4):
                ob, obb = oT[h]
                T.op(ACT, lambda e, ob=ob, h=h: e.activation(out=osq[:, h, :], in_=ob[:, :], func=AF.Square),
                     reads=[obb], writes=[b_osq])
                pr, prb = rotA.next()
                T.op(PE, lambda e, pr=pr, h=h: e.matmul(pr[:, :], onesb[:], osq[:, h, :], start=True, stop=True),
                     reads=[b_onesb, b_osq], writes=[prb])
                rv, rvb = rinv[h % 2]
                T.op(DVE, lambda e, pr=pr, rv=rv: e.tensor_scalar(out=rv, in0=pr[:, :], scalar1=128.0 * RMS_EPS, scalar2=None, op0=ALU.add),
                     reads=[prb], writes=[rvb])
                T.op(ACT, lambda e, rv=rv: e.activation(out=rv, in_=rv, func=AF.Ln), reads=[rvb], writes=[rvb])
                T.op(ACT, lambda e, rv=rv: e.activation(out=rv, in_=rv, func=AF.Exp, scale=-0.5), reads=[rvb], writes=[rvb])
                tt, ttb = t1[h % 2]
                T.op(DVE, lambda e, ob=ob, rv=rv, tt=tt: e.tensor_tensor(out=tt, in0=ob[:, :], in1=rv, op=ALU.mult),
                     reads=[obb, rvb], writes=[ttb])
                T.op(DVE, lambda e, tt=tt, h=h: e.scalar_tensor_tensor(
                    out=ymix[:, h, :], in0=tt, scalar=gng[:, l, h:h + 1], in1=sr[:, h, :], op0=ALU.mult, op1=ALU.mult),
                    reads=[ttb, b_gng, b_sr], writes=[b_ymix])
            if pre_out is not None:
                pre_out()
            uo = [take((l, "wout0")), take((l, "wout1"))]
            sqt = AR.tile_at("sqb", AR.offs["gv"], [NCH, TT], BF16, inherit=[b_gv, b_gsq])
            st = (banks[4][0], banks[4][1], banks[5][0], banks[5][1], sqt[0], sqt[1])
            proj_residual(uo, ymix, b_ymix, rotA, st)
            return st

        def xattn(l, ti):
            AR.reset()
            qT8, b_qT8 = AR.tile("qT8", [8, TT], BF16)
            eT, b_eT = AR.tile("eT", [4, 2, TT], BF16)
            rden = [AR.tile("rden%d" % i, [TT]) for i in range(2)]
            oT8, b_oT8 = AR.tile("oT8", [8, TT], BF16)
            sqt = AR.tile("sqb", [NCH, TT], BF16)
            rot = Rot([0, 1, 2, 3, 4, 5])
            uq = [take((l, "wq0")), take((l, "wq1"))]
            for ec in range(NCH):
                wt, wb, _ = uq[ec // 4]
                wv = wt[:].rearrange("p (k c) -> p k c", k=8)
                pt, pb = rot.next()
                for kc in range(NCH):
                    T.op(PE, lambda e, pt=pt, wv=wv, kc=kc, ec=ec: e.matmul(
                        pt[:, :], wv[:, kc, (ec % 4) * 128:(ec % 4 + 1) * 128], xbf[:, kc, :],
                        start=(kc == 0), stop=(kc == NCH - 1)), reads=[wb, b_xbf], writes=[pb], inc=(kc == NCH - 1))
                if ec % 2 == 0:
                    T.op(ACT, lambda e, pt=pt, ec=ec: e.copy(out=qT8[:, ec, :], in_=pt[:, :]), reads=[pb], writes=[b_qT8])
                else:
                    T.op(DVE, lambda e, pt=pt, ec=ec: e.tensor_copy(out=qT8[:, ec, :], in_=pt[:, :]), reads=[pb], writes=[b_qT8])
                if ec % 4 == 3:
                    rel(uq[ec // 4])
            for h in range(4):
                for mc in range(2):
                    pt, pb = rot.next()
                    for j in range(2):
                        T.op(PE, lambda e, pt=pt, h=h, mc=mc, j=j: e.matmul(
                            pt[:, :], KT[:, l, 2 * h + j, mc * 128:(mc + 1) * 128], qT8[:, 2 * h + j, :],
                            start=(j == 0), stop=(j == 1)), reads=[b_KT, b_qT8], writes=[pb], inc=(j == 1))
                    T.op(ACT, lambda e, pt=pt, h=h, mc=mc: e.activation(out=eT[:, h, mc, :], in_=pt[:, :], func=AF.Exp),
                         reads=[pb], writes=[b_eT])
                pd, pdb = rot.next()
                for mc in range(2):
                    T.op(PE, lambda e, pd=pd, h=h, mc=mc: e.matmul(pd[:, :], onesb[:], eT[:, h, mc, :],
                                                                  start=(mc == 0), stop=(mc == 1)),
                         reads=[b_onesb, b_eT], writes=[pdb], inc=(mc == 1))
                rd, rdb = rden[h % 2]
                T.op(ACT, lambda e, pd=pd, rd=rd: e.activation(out=rd, in_=pd[:, :], func=AF.Ln), reads=[pdb], writes=[rdb])
                T.op(ACT, lambda e, rd=rd: e.activation(out=rd, in_=rd, func=AF.Exp, scale=-1.0), reads=[rdb], writes=[rdb])
                for j in range(2):
                    po, pob = rot.next()
                    ecol = (2 * h + j) * 128
                    for mc in range(2):
                        T.op(PE, lambda e, po=po, h=h, mc=mc, ecol=ecol: e.matmul(
                            po[:, :], Vm[:, l, mc, ecol:ecol + 128], eT[:, h, mc, :], start=(mc == 0), stop=(mc == 1)),
                            reads=[b_Vm, b_eT], writes=[pob], inc=(mc == 1))
                    T.op(DVE, lambda e, po=po, rd=rd, h=h, j=j: e.tensor_tensor(
                        out=oT8[:, 2 * h + j, :], in0=po[:, :], in1=rd, op=ALU.mult), reads=[pob, rdb], writes=[b_oT8])
            uo = [take((l, "wo0")), take((l, "wo1"))]
            st = (banks[6][0], banks[6][1], banks[7][0], banks[7][1], sqt[0], sqt[1])
            proj_residual(uo, oT8, b_oT8, rot, st)
            return st

        def moe(l, ti):
            AR.reset()
            lg, b_lg = AR.tile("lg", [4, 16])
            sc, b_sc = AR.tile("sc", [4, 16])
            pairs, b_pairs = AR.tile("pairs", [16, 6])
            gs, b_gs = AR.tile("gs", [4, 4])
            goh, b_goh = AR.tile("goh", [4, 4])
            msk, b_msk = AR.tile("msk", [4, 16])
            m1, b_m1 = AR.tile("m1", [4, 16])
            rem, b_rem = AR.tile("rem", [4, 16])
            tmpm, b_tmpm = AR.tile("tmpm", [4, 16])
            gate, b_gate = AR.tile("gate", [4, 16])
            gatebf, b_gatebf = AR.tile("gatebf", [4, 16], BF16)
            s4a, b_s4a = AR.tile("s4a", [4])
            s4b, b_s4b = AR.tile("s4b", [4])
            s4c, b_s4c = AR.tile("s4c", [4])
            sgt = [AR.tile("sg%d" % i, [TT]) for i in range(2)]
            tmt = [AR.tile("tm%d" % i, [TT]) for i in range(8)]
            hT = [AR.tile("hT%d" % i, [4, TT], BF16) for i in range(2)]
            sqt = AR.tile("sqb", [NCH, TT], BF16)

            def router():
                pr, prb = banks[7]
                for blk in range(4):
                    for kc in range(NCH):
                        T.op(PE, lambda e, blk=blk, kc=kc: e.matmul(
                            pr[:, blk * 16:(blk + 1) * 16], xres[:, kc, blk * 128:(blk + 1) * 128], wr[:, kc, :],
                            start=(kc == 0), stop=False), reads=[b_xres[kc], b_wr], writes=[prb], inc=False)
                    T.op(PE, lambda e, blk=blk: e.matmul(pr[:, blk * 16:(blk + 1) * 16], onesr[0:1, :], br[0:1, :],
                                                         start=False, stop=True), reads=[b_onesr, b_br], writes=[prb])
                B4 = lambda ap: ap.unsqueeze(2).to_broadcast([128, 4, 16])
                T.op(DVE, lambda e: e.tensor_copy(out=lg, in_=pr[:, 0:64].rearrange("p (a b) -> p a b", a=4)), reads=[prb], writes=[b_lg])
                T.op(DVE, lambda e: e.tensor_reduce(out=s4a, in_=lg, axis=AX.X, op=ALU.max), reads=[b_lg], writes=[b_s4a])
                T.op(DVE, lambda e: e.tensor_tensor(out=lg, in0=lg, in1=B4(s4a), op=ALU.subtract), reads=[b_lg, b_s4a], writes=[b_lg])
                T.op(ACT, lambda e: e.activation(out=sc, in_=lg, func=AF.Exp), reads=[b_lg], writes=[b_sc])
                T.op(DVE, lambda e: e.tensor_reduce(out=s4b, in_=sc, axis=AX.X, op=ALU.add), reads=[b_sc], writes=[b_s4b])
                T.op(DVE, lambda e: e.reciprocal(out=s4b, in_=s4b), reads=[b_s4b], writes=[b_s4b])
                T.op(DVE, lambda e: e.tensor_tensor(out=sc, in0=sc, in1=B4(s4b), op=ALU.mult), reads=[b_sc, b_s4b], writes=[b_sc])
                scg = sc.rearrange("p a (g k) -> p (a g) k", g=4)
                T.op(DVE, lambda e: e.tensor_tensor(out=pairs[:, :, 0:3], in0=scg[:, :, 0:3], in1=scg[:, :, 1:4], op=ALU.add),
                     reads=[b_sc], writes=[b_pairs])
                T.op(DVE, lambda e: e.tensor_tensor(out=pairs[:, :, 3:5], in0=scg[:, :, 0:2], in1=scg[:, :, 2:4], op=ALU.add),
                     reads=[b_sc], writes=[b_pairs])
                T.op(DVE, lambda e: e.tensor_tensor(out=pairs[:, :, 5:6], in0=scg[:, :, 0:1], in1=scg[:, :, 3:4], op=ALU.add),
                     reads=[b_sc], writes=[b_pairs])
                T.op(DVE, lambda e: e.tensor_reduce(out=gs.rearrange("p a g -> p (a g)"), in_=pairs, axis=AX.X, op=ALU.max),
                     reads=[b_pairs], writes=[b_gs])
                T.op(DVE, lambda e: e.tensor_reduce(out=s4c, in_=gs, axis=AX.X, op=ALU.max), reads=[b_gs], writes=[b_s4c])
                T.op(DVE, lambda e: e.tensor_tensor(out=goh, in0=gs, in1=s4c.unsqueeze(2).to_broadcast([128, 4, 4]), op=ALU.is_equal),
                     reads=[b_gs, b_s4c], writes=[b_goh])
                T.op(DVE, lambda e: e.tensor_tensor(
                    out=msk.rearrange("p a (g k) -> p (a g) k", g=4), in0=scg,
                    in1=goh.rearrange("p a g -> p (a g)").unsqueeze(2).to_broadcast([128, 16, 4]), op=ALU.mult),
                    reads=[b_sc, b_goh], writes=[b_msk])
                T.op(DVE, lambda e: e.tensor_reduce(out=s4a, in_=msk, axis=AX.X, op=ALU.max), reads=[b_msk], writes=[b_s4a])
                T.op(DVE, lambda e: e.tensor_tensor(out=m1, in0=msk, in1=B4(s4a), op=ALU.is_equal), reads=[b_msk, b_s4a], writes=[b_m1])
                T.op(DVE, lambda e: e.tensor_tensor(out=tmpm, in0=msk, in1=m1, op=ALU.mult), reads=[b_msk, b_m1], writes=[b_tmpm])
                T.op(DVE, lambda e: e.tensor_tensor(out=rem, in0=msk, in1=tmpm, op=ALU.subtract), reads=[b_msk, b_tmpm], writes=[b_rem])
                T.op(DVE, lambda e: e.tensor_reduce(out=s4b, in_=rem, axis=AX.X, op=ALU.max), reads=[b_rem], writes=[b_s4b])
                T.op(DVE, lambda e: e.tensor_tensor(out=m1, in0=rem, in1=B4(s4b), op=ALU.is_equal), reads=[b_rem, b_s4b], writes=[b_m1])
                T.op(DVE, lambda e: e.tensor_tensor(out=rem, in0=rem, in1=m1, op=ALU.mult), reads=[b_rem, b_m1], writes=[b_rem])
                T.op(DVE, lambda e: e.tensor_tensor(out=tmpm, in0=tmpm, in1=rem, op=ALU.add), reads=[b_tmpm, b_rem], writes=[b_tmpm])
                T.op(DVE, lambda e: e.tensor_tensor(out=s4c, in0=s4a, in1=s4b, op=ALU.add), reads=[b_s4a, b_s4b], writes=[b_s4c])
                T.op(DVE, lambda e: e.reciprocal(out=s4c, in_=s4c), reads=[b_s4c], writes=[b_s4c])
                T.op(DVE, lambda e: e.tensor_tensor(out=gate, in0=tmpm, in1=B4(s4c), op=ALU.mult), reads=[b_tmpm, b_s4c], writes=[b_gate])
                T.op(DVE, lambda e: e.tensor_copy(out=gatebf, in_=gate), reads=[b_gate], writes=[b_gatebf])
            rot_gu = Rot([0, 1, 2, 3])
            rot_gate = Rot([4, 5])
            rot_y = Rot([6, 7])
            state = {}

            def gu_mm(e_):
                wg = take((l, "wg", e_)); wu = take((l, "wu", e_)); wd = take((l, "wd", e_))
                wgv = wg[0][:].rearrange("p (k c) -> p k c", k=8)
                wuv = wu[0][:].rearrange("p (k c) -> p k c", k=8)
                tms = []
                for fc in range(4):
                    pg, pgb = rot_gu.next()
                    for kc in range(NCH):
                        T.op(PE, lambda e, pg=pg, kc=kc, fc=fc: e.matmul(
                            pg[:, :], wgv[:, kc, fc * 128:(fc + 1) * 128], xbf[:, kc, :], start=(kc == 0), stop=(kc == NCH - 1)),
                            reads=[wg[1], b_xbf], writes=[pgb], inc=(kc == NCH - 1))
                    pu, pub = rot_gu.next()
                    for kc in range(NCH):
                        T.op(PE, lambda e, pu=pu, kc=kc, fc=fc: e.matmul(
                            pu[:, :], wuv[:, kc, fc * 128:(fc + 1) * 128], xbf[:, kc, :], start=(kc == 0), stop=(kc == NCH - 1)),
                            reads=[wu[1], b_xbf], writes=[pub], inc=(kc == NCH - 1))
                    sg, sgb = sgt[fc % 2]
                    T.op(ACT, lambda e, pg=pg, sg=sg: e.activation(out=sg, in_=pg[:, :], func=AF.Silu), reads=[pgb], writes=[sgb])
                    tm, tmb = tmt[(e_ % 2) * 4 + fc]
                    T.op(DVE, lambda e, pu=pu, sg=sg, tm=tm: e.tensor_tensor(out=tm, in0=pu[:, :], in1=sg, op=ALU.mult),
                         reads=[pub, sgb], writes=[tmb])
                    tms.append((tm, tmb))
                rel(wg); rel(wu)
                state[e_] = (wd, tms)

            def gate_mm(e_):
                wd, tms = state[e_]
                pgt, pgtb = rot_gate.next()
                for blk in range(4):
                    T.op(PE, lambda e, blk=blk, e_=e_, pgt=pgt: e.matmul(
                        pgt[:, blk * 128:(blk + 1) * 128], gatebf[:, blk, e_:e_ + 1].to_broadcast([128, 128]), identb[:],
                        start=True, stop=True), reads=[b_gatebf, b_identb], writes=[pgtb], inc=(blk == 3))
                state[e_] = (wd, tms, pgt, pgtb)

            def h_ops(e_):
                wd, tms, pgt, pgtb = state[e_]
                ht, htb = hT[e_ % 2]
                for fc in range(4):
                    tm, tmb = tms[fc]
                    T.op(DVE, lambda e, tm=tm, pgt=pgt, ht=ht, fc=fc: e.tensor_tensor(out=ht[:, fc, :], in0=tm, in1=pgt[:, :], op=ALU.mult),
                         reads=[tmb, pgtb], writes=[htb])
                state[e_] = (wd, ht, htb)

            def down_phase(e_, st):
                wd, ht, htb = state.pop(e_)
                wdv = wd[0][:].rearrange("p (k c) -> p k c", k=4)
                for dc in range(NCH):
                    py, pyb = rot_y.next()
                    for fc in range(4):
                        T.op(PE, lambda e, py=py, fc=fc, dc=dc: e.matmul(
                            py[:, :], wdv[:, fc, dc * 128:(dc + 1) * 128], ht[:, fc, :], start=(fc == 0), stop=(fc == 3)),
                            reads=[wd[1], htb], writes=[pyb], inc=(fc == 3))
                    if e_ == 0:
                        T.op(DVE, lambda e, py=py, dc=dc: e.scalar_tensor_tensor(
                            out=xres[:, dc, :], in0=xres[:, dc, :], scalar=ALPHA, in1=py[:, :], op0=ALU.mult, op1=ALU.add),
                            reads=[pyb, b_xres[dc]], writes=[b_xres[dc]])
                    else:
                        T.op(DVE, lambda e, py=py, dc=dc: e.tensor_tensor(out=xres[:, dc, :], in0=py[:, :], in1=xres[:, dc, :], op=ALU.add),
                             reads=[pyb, b_xres[dc]], writes=[b_xres[dc]])
                    if st is not None:
                        stats_ops(dc, st)
                        if dc >= 5:
                            stats_mm(dc - 5, st)
                rel(wd)
                if st is not None:
                    for d_ in range(NCH - 5, NCH):
                        stats_mm(d_, st)

            st = (banks[0][0], banks[0][1], banks[1][0], banks[1][1], sqt[0], sqt[1])
            gu_mm(0)
            router()
            gu_mm(1)
            gate_mm(0)
            h_ops(0)
            for e_ in range(NEXP):
                if e_ + 1 < NEXP:
                    gate_mm(e_ + 1)
                down_phase(e_, st if e_ == NEXP - 1 else None)
                if e_ + 1 < NEXP:
                    h_ops(e_ + 1)
                if e_ + 2 < NEXP:
                    gu_mm(e_ + 2)
            return st

        for ti in range(NT):
            def cast_xbf():
                T.op(ACT, lambda e: e.copy(out=xbf[:, 0:4, :], in_=xnext[:, 0:4, :]), reads=[b_xnext], writes=[b_xbf])
                T.op(DVE, lambda e: e.tensor_copy(out=xbf[:, 4:8, :], in_=xnext[:, 4:8, :]), reads=[b_xnext], writes=[b_xbf])

            def load_xres(ti=ti):
                T.op(ACT, lambda e: e.copy(out=xres[:, 0:4, :], in_=xnext[:, 0:4, :]), reads=[b_xnext], writes=b_xres[0:4])
                T.op(DVE, lambda e: e.tensor_copy(out=xres[:, 4:8, :], in_=xnext[:, 4:8, :]), reads=[b_xnext], writes=b_xres[4:8])
                if ti + 1 < NT:
                    T.dma(SP, c_xnext, lambda e, ti=ti: e.dma_start(
                        out=xnext[:], in_=xT[:, (ti + 1) * TT:(ti + 2) * TT].rearrange("(k p) t -> p k t", p=128)), writes=[b_xnext])

            if ti == 0:
                T.dma(SP, c_xnext, lambda e: e.dma_start(
                    out=xnext[:], in_=xT[:, 0:TT].rearrange("(k p) t -> p k t", p=128)), writes=[b_xnext])
                cast_xbf()
            stop = False
            pool_ok[0] = (ti > 0)
            for l in range(depth):
                st = mixer(l, ti, pre_out=(load_xres if l == 0 else None))
                layer_norm(l, 0, st, use_pool=(ti > 0))
                if dbg == (l, 0):
                    stop = True; break
                st = xattn(l, ti)
                layer_norm(l, 1, st, use_pool=(ti > 0))
                if dbg == (l, 1):
                    stop = True; break
                st = moe(l, ti)
                is_final = (l == depth - 1) and dbg is None
                if is_final and ti + 1 < NT:
                    cast_xbf()
                layer_norm(l, 2, st, use_pool=(ti > 0), final=is_final)
                if dbg == (l, 2):
                    stop = True; break
            if stop:
                T.dma(SP, c_xin, lambda e: e.dma_start(out=dbg_out.rearrange("(k p) t -> p k t", p=128), in_=xres[:]),
                      reads=b_xres)
                break
            T.dma(SP, c_xin, lambda e, ti=ti: e.dma_start(
                out=yT[:, ti * TT:(ti + 1) * TT].rearrange("(k p) t -> p k t", p=128), in_=xres[:]), reads=b_xres)
        T.wait_all(SP, all_ctx)
        with nc.Block() as block:
            block.sync(lambda e: T.replay(SP, e))
            block.tensor(lambda e: T.replay(PE, e))
            block.scalar(lambda e: T.replay(ACT, e))
            block.vector(lambda e: T.replay(DVE, e))
            block.gpsimd(lambda e: T.replay(POOL, e))
    nc._mk_stats = dict(nops=T.nops, nwaits=T.nwaits)
    return nc


def _prep_shared(inp):
    f = lambda a: np.ascontiguousarray(np.asarray(a, dtype=np.float32))
    sh = {}
    sh["w_in"] = f(inp["w_in"])
    sh["w_a2"] = f(inp["w_a2"])
    sh["b_a"] = f(inp["b_a"]).reshape(DEPTH, 1, 256)
    sh["gng_t"] = f(np.asarray(inp["gla_norm_g"]).reshape(DEPTH, 4, 128).transpose(0, 2, 1))
    sh["w_sT"] = f(np.asarray(inp["w_s"]).transpose(0, 1, 3, 2))
    sh["b_s"] = f(inp["b_s"]).reshape(DEPTH, 1, 512)
    sh["slng_t"] = f(np.asarray(inp["sgu_ln_g"]).reshape(DEPTH, 4, 128).transpose(0, 2, 1))
    sh["slnb"] = f(inp["sgu_ln_b"]).reshape(DEPTH, 1, 512)
    for k in ["w_out", "wq_x", "wk_x", "wv_x", "wo_x", "w_router", "w_gate", "w_up", "w_down"]:
        sh[k] = f(inp[k])
    sh["b_router"] = f(inp["b_router"]).reshape(1, NEXP)
    sh["lng_t"] = f(np.asarray(inp["ln_g"]).reshape(DEPTH * 3, NCH, 128).transpose(2, 0, 1).reshape(128, DEPTH * 3 * NCH))
    sh["lnb_t"] = f(np.asarray(inp["ln_b"]).reshape(DEPTH * 3, NCH, 128).transpose(2, 0, 1).reshape(128, DEPTH * 3 * NCH))
    return sh


def kernel(**inputs):
    x = np.asarray(inputs["x"], dtype=np.float32)
    mem = np.asarray(inputs["mem"], dtype=np.float32)
    B, S, _ = x.shape
    sh = _prep_shared(inputs)
    nc = build_nc(S)
    in_maps = []
    for b in range(B):
        m = dict(sh)
        m["xT"] = np.ascontiguousarray(x[b].T)
        m["memT"] = np.ascontiguousarray(mem[b].T)
        in_maps.append(m)
    res = run_bass_kernel_spmd(nc, in_maps, core_ids=list(range(B)))
    out = np.empty((B, S, D), dtype=np.float32)
    for b in range(B):
        out[b] = res.results[b]["yT"].T
    return out
```

```python
import numpy as np
from contextlib import ExitStack
import concourse.bass as bass
import concourse.mybir as mybir
from concourse.bass_utils import run_bass_kernel_spmd

F32 = mybir.dt.float32
BF16 = mybir.dt.bfloat16
AF = mybir.ActivationFunctionType
ALU = mybir.AluOpType
AX = mybir.AxisListType

D = 1024
NCH = 8
TT = 512
DIN = 2576
NEXP = 16
DEPTH = 2
NMEM = 256
ALPHA = float((2.0 * DEPTH) ** 0.25)
LN_EPS = 1e-5
RMS_EPS = 1e-6
RING = 6
SAME_ENG_SYNC = True


class Ctx:
    def __init__(self, name, sem, step):
        self.name = name
        self.sem = sem
        self.step = step
        self.count = 0


class Eng(Ctx):
    def __init__(self, name, sem):
        super().__init__(name, sem, 1)
        self.ops = []
        self.waited = {}


class Buf:
    def __init__(self, name):
        self.name = name
        self.last_write = None
        self.reads = []


def _compact(lst):
    best = {}
    for (c, v) in lst:
        if v > best.get(c, 0):
            best[c] = v
    return list(best.items())


class Tracker:
    def __init__(self):
        self.nwaits = 0
        self.nops = 0

    def _need(self, eng, deps):
        best = {}
        for (c, v) in deps:
            if c is eng and (not SAME_ENG_SYNC or eng.name == "pe" or v > eng.count):
                continue
            if v > best.get(c, 0):
                best[c] = v
        for c, v in best.items():
            if eng.waited.get(c, 0) >= v:
                continue
            eng.waited[c] = v
            eng.ops.append(("wait", c, v))
            self.nwaits += 1

    @staticmethod
    def _deps(reads, writes):
        deps = []
        for b in reads:
            if b.last_write is not None:
                deps.append(b.last_write)
        for b in writes:
            if b.last_write is not None:
                deps.append(b.last_write)
            deps.extend(b.reads)
        return deps

    def op(self, eng, fn, reads=(), writes=(), inc=True):
        self._need(eng, self._deps(reads, writes))
        val = eng.count + 1
        if inc:
            eng.count = val
        eng.ops.append(("op", fn, inc))
        self.nops += 1
        for b in reads:
            b.reads.append((eng, val))
            if len(b.reads) > 48:
                b.reads = _compact(b.reads)
        for b in writes:
            b.last_write = (eng, val)
            b.reads = []

    def dma(self, queue, ctx, fn, reads=(), writes=(), serial=False):
        deps = self._deps(reads, writes)
        if serial and ctx.count > 0:
            deps.append((ctx, ctx.count))
        self._need(queue, deps)
        ctx.count += 16
        val = ctx.count
        queue.ops.append(("dma", fn, ctx))
        self.nops += 1
        for b in reads:
            b.reads.append((ctx, val))
        for b in writes:
            b.last_write = (ctx, val)
            b.reads = []

    def wait_all(self, eng, ctxs):
        for c in ctxs:
            if c.count > 0 and eng.waited.get(c, 0) < c.count:
                eng.waited[c] = c.count
                eng.ops.append(("wait", c, c.count))

    @staticmethod
    def replay(eng, handle):
        for o in eng.ops:
            if o[0] == "wait":
                handle.wait_ge(o[1].sem, o[2])
            elif o[0] == "op":
                ins = o[1](handle)
                if o[2]:
                    ins.then_inc(eng.sem, 1)
            else:
                ins = o[1](handle)
                ins.then_inc(o[2].sem, 16)


def build_nc(S, depth=DEPTH, dbg=None):
    NT = S // TT
    nc = bass.Bass("TRN2", target_bir_lowering=False)

    def din(name, shape):
        return nc.dram_tensor(name, list(shape), F32, kind="ExternalInput").ap()

    xT = din("xT", [D, S])
    memT = din("memT", [D, NMEM])
    w_in = din("w_in", [DEPTH, D, DIN])
    w_a2 = din("w_a2", [DEPTH, 16, 256])
    b_a = din("b_a", [DEPTH, 1, 256])
    gng_t = din("gng_t", [DEPTH, 128, 4])
    w_sT = din("w_sT", [DEPTH, 4, 128, 128])
    b_s = din("b_s", [DEPTH, 1, 512])
    slng_t = din("slng_t", [DEPTH, 128, 4])
    slnb = din("slnb", [DEPTH, 1, 512])
    w_out = din("w_out", [DEPTH, D, D])
    wq_x = din("wq_x", [DEPTH, D, D])
    wk_x = din("wk_x", [DEPTH, D, D])
    wv_x = din("wv_x", [DEPTH, D, D])
    wo_x = din("wo_x", [DEPTH, D, D])
    w_router = din("w_router", [D, NEXP])
    b_router = din("b_router", [1, NEXP])
    w_gate = din("w_gate", [DEPTH, NEXP, D, 512])
    w_up = din("w_up", [DEPTH, NEXP, D, 512])
    w_down = din("w_down", [DEPTH, NEXP, 512, D])
    lng_t = din("lng_t", [128, DEPTH * 3 * NCH])
    lnb_t = din("lnb_t", [128, DEPTH * 3 * NCH])
    yT = nc.dram_tensor("yT", [D, S], F32, kind="ExternalOutput").ap()
    dbg_out = None
    if dbg is not None:
        dbg_out = nc.dram_tensor("dbg", [D, TT], F32, kind="ExternalOutput").ap()

    UPL = 5 + 2 + 2 + 2 + 2 + 2 + 3 * NEXP
    NU = depth * UPL
    wsc = nc.dram_tensor("wscratch", [NU, 128, 4096], BF16, kind="Internal").ap()

    T = Tracker()
    es = ExitStack()
    with es:
        def sb(name, shape, dt=F32):
            return es.enter_context(nc.sbuf_tensor(name, list(shape), dt))

        def sem(name):
            return es.enter_context(nc.semaphore(name))

        PE = Eng("pe", sem("s_pe"))
        ACT = Eng("act", sem("s_act"))
        DVE = Eng("dve", sem("s_dve"))
        POOL = Eng("pool", sem("s_pool"))
        SP = Eng("sp", sem("s_sp"))
        all_ctx = []

        def newctx(name):
            c = Ctx(name, sem("s_" + name), 16)
            all_ctx.append(c)
            return c

        banks = []
        for i in range(8):
            t = es.enter_context(nc.psum_tensor("bank%d" % i, [128, 512], F32))
            banks.append((t, Buf("bank%d" % i)))

        class Rot:
            def __init__(self, ids):
                self.ids = ids
                self.i = 0

            def next(self):
                b = banks[self.ids[self.i % len(self.ids)]]
                self.i += 1
                return b

        ARENA = 66560
        arena = sb("arena", [128, ARENA], mybir.dt.uint8)
        arena_bufs = []

        class Arena:
            def __init__(self):
                self.off = 0
                self.prev = []
                self.cur = []

            def reset(self):
                self.prev = self.prev + self.cur
                hz = []
                for b in self.prev:
                    if b.last_write is not None:
                        hz.append(b.last_write)
                    hz.extend(b.reads)
                self.hz = _compact(hz)
                self.prev = []
                self.cur = []
                self.off = 0
                self.offs = {}

            def tile(self, name, free_shape, dt=F32):
                n = 1
                for s in free_shape:
                    n *= s
                nbytes = n * (4 if dt == F32 else 2)
                nbytes = (nbytes + 63) // 64 * 64
                assert self.off + nbytes <= ARENA, (name, self.off, nbytes)
                v = arena[:, self.off:self.off + nbytes].bitcast(dt)
                if dt == F32:
                    v = arena[:, self.off:self.off + nbytes].bitcast(F32)
                self.off += nbytes
                v = v[:, 0:n]
                if len(free_shape) == 2:
                    v = v.rearrange("p (a b) -> p a b", a=free_shape[0])
                elif len(free_shape) == 3:
                    v = v.rearrange("p (a b c) -> p a b c", a=free_shape[0], b=free_shape[1])
                b = Buf(name)
                b.reads = list(self.hz)
                self.cur.append(b)
                self.offs[name] = self.off - nbytes
                return v, b

            def tile_at(self, name, off, free_shape, dt, inherit):
                save = self.off
                self.off = off
                v, b = self.tile(name, free_shape, dt)
                self.off = save
                for o in inherit:
                    if o.last_write is not None:
                        b.reads.append(o.last_write)
                    b.reads.extend(o.reads)
                return v, b

        AR = Arena()
        AR.hz = []
        AR.offs = {}

        cc = newctx("cc")
        const_bufs = []

        def cload(tile_ap, dram_ap, name):
            b = Buf(name)
            T.dma(SP, cc, lambda e, o=tile_ap, i=dram_ap: e.dma_start(out=o, in_=i), writes=[b])
            const_bufs.append(b)
            return b

        rowi = sb("rowi", [128, 128]); b_rowi = Buf("rowi")
        coli = sb("coli", [128, 128]); b_coli = Buf("coli")
        T.op(POOL, lambda e: e.iota(rowi[:], [[0, 128]], base=0, channel_multiplier=1,
                                    allow_small_or_imprecise_dtypes=True), writes=[b_rowi])
        T.op(POOL, lambda e: e.iota(coli[:], [[1, 128]], base=0, channel_multiplier=0,
                                    allow_small_or_imprecise_dtypes=True), writes=[b_coli])
        identf = sb("identf", [128, 128]); b_identf = Buf("identf")
        T.op(DVE, lambda e: e.tensor_tensor(out=identf[:], in0=rowi[:], in1=coli[:], op=ALU.is_equal),
             reads=[b_rowi, b_coli], writes=[b_identf])
        identb = sb("identb", [128, 128], BF16); b_identb = Buf("identb")
        T.op(DVE, lambda e: e.tensor_copy(out=identb[:], in_=identf[:]), reads=[b_identf], writes=[b_identb])
        maskd = sb("maskd", [128, 128]); b_maskd = Buf("maskd")
        tmpa = sb("tmpa", [128, 128]); b_tmpa = Buf("tmpa")
        tmpb = sb("tmpb", [128, 128]); b_tmpb = Buf("tmpb")
        T.op(DVE, lambda e: e.tensor_tensor(out=maskd[:], in0=rowi[:], in1=coli[:], op=ALU.is_gt),
             reads=[b_rowi, b_coli], writes=[b_maskd])
        T.op(DVE, lambda e: e.tensor_single_scalar(out=tmpa[:], in_=rowi[:], scalar=64.0, op=ALU.is_ge),
             reads=[b_rowi], writes=[b_tmpa])
        T.op(DVE, lambda e: e.tensor_single_scalar(out=tmpb[:], in_=coli[:], scalar=64.0, op=ALU.is_ge),
             reads=[b_coli], writes=[b_tmpb])
        T.op(DVE, lambda e: e.tensor_tensor(out=tmpb[:], in0=tmpa[:], in1=tmpb[:], op=ALU.is_equal),
             reads=[b_tmpa, b_tmpb], writes=[b_tmpb])
        T.op(DVE, lambda e: e.scalar_tensor_tensor(out=maskd[:], in0=maskd[:], scalar=-1.0 / 16.0, in1=tmpb[:],
                                                   op0=ALU.mult, op1=ALU.mult),
             reads=[b_maskd, b_tmpb], writes=[b_maskd])
        csel = sb("csel", [128, 16]); b_csel = Buf("csel")
        T.op(POOL, lambda e: e.memset(csel[:], 0.0), writes=[b_csel])
        T.op(DVE, lambda e: e.tensor_scalar(out=csel[:, 1:2], in0=tmpa[:, 0:1], scalar1=-1.0 / 16.0, scalar2=None,
                                            op0=ALU.mult), reads=[b_tmpa], writes=[b_csel])
        T.op(DVE, lambda e: e.tensor_scalar(out=csel[:, 0:1], in0=tmpa[:, 0:1], scalar1=1.0 / 16.0,
                                            scalar2=-1.0 / 16.0, op0=ALU.mult, op1=ALU.add),
             reads=[b_tmpa], writes=[b_csel])
        onesb = sb("onesb", [128, 128], BF16); b_onesb = Buf("onesb")
        T.op(POOL, lambda e: e.memset(onesb[:], 1.0), writes=[b_onesb])
        onesm = sb("onesm", [128, 128], BF16); b_onesm = Buf("onesm")
        T.op(POOL, lambda e: e.memset(onesm[:], 1.0 / 1024.0), writes=[b_onesm])
        onesr = sb("onesr", [1, 128]); b_onesr = Buf("onesr")
        T.op(POOL, lambda e: e.memset(onesr[:], 1.0), writes=[b_onesr])
        onescol = sb("onescol", [128, 1]); b_onescol = Buf("onescol")
        T.op(POOL, lambda e: e.memset(onescol[:], 1.0), writes=[b_onescol])

        lng = sb("lng", [128, DEPTH * 3 * NCH]); b_lng = cload(lng[:], lng_t, "lng")
        lnb = sb("lnb", [128, DEPTH * 3 * NCH]); b_lnb = cload(lnb[:], lnb_t, "lnb")
        wr = sb("wr", [128, NCH, NEXP]); b_wr = cload(wr[:], w_router.rearrange("(k p) e -> p k e", p=128), "wr")
        br = sb("br", [1, NEXP]); b_br = cload(br[:], b_router, "br")
        wa2 = sb("wa2", [32, depth, 256]); b_wa2 = Buf("wa2")
        alwf = sb("alwf", [128, depth, NCH, 16]); b_alwf = Buf("alwf")
        gng = sb("gng", [128, depth, 4]); b_gng = Buf("gng")
        slng = sb("slng", [128, depth, 4]); b_slng = Buf("slng")
        wsf, b_wsf = AR.tile("wsf", [depth, 4, 128])
        bsr, b_bsr = AR.tile("bsr", [depth, 512])
        slnbr, b_slnbr = AR.tile("slnbr", [depth, 512])
        for l in range(depth):
            T.dma(SP, cc, lambda e, l=l: e.dma_start(out=wa2[0:16, l, :], in_=w_a2[l]), writes=[b_wa2])
            T.dma(SP, cc, lambda e, l=l: e.dma_start(out=wa2[16:17, l, :], in_=b_a[l]), writes=[b_wa2])
            T.dma(SP, cc, lambda e, l=l: e.dma_start(
                out=alwf[:, l, :, :], in_=w_in[l, :, 1536:1552].rearrange("(k p) c -> p k c", p=128)), writes=[b_alwf])
            T.dma(SP, cc, lambda e, l=l: e.dma_start(out=gng[:, l, :], in_=gng_t[l]), writes=[b_gng])
            T.dma(SP, cc, lambda e, l=l: e.dma_start(out=slng[:, l, :], in_=slng_t[l]), writes=[b_slng])
            T.dma(SP, cc, lambda e, l=l: e.dma_start(
                out=wsf[:, l, :, :], in_=w_sT[l].rearrange("g s t -> s g t")), writes=[b_wsf])
            T.dma(SP, cc, lambda e, l=l: e.dma_start(out=bsr[0:1, l, :], in_=b_s[l]), writes=[b_bsr])
            T.dma(SP, cc, lambda e, l=l: e.dma_start(out=slnbr[0:1, l, :], in_=slnb[l]), writes=[b_slnbr])
        memf, b_memf = AR.tile("memf", [NCH, NMEM])
        T.dma(SP, cc, lambda e: e.dma_start(out=memf, in_=memT.rearrange("(k p) m -> p k m", p=128)), writes=[b_memf])
        for b in const_bufs + [b_wa2, b_alwf, b_gng, b_slng, b_wsf, b_bsr, b_slnbr, b_memf]:
            b.last_write = (cc, cc.count)

        alwb = sb("alwb", [128, depth, NCH, 16], BF16); b_alwb = Buf("alwb")
        T.op(DVE, lambda e: e.tensor_copy(out=alwb[:], in_=alwf[:]), reads=[b_alwf], writes=[b_alwb])
        T.op(DVE, lambda e: e.tensor_scalar(out=gng[:], in0=gng[:], scalar1=float(np.sqrt(128.0)), scalar2=None,
                                            op0=ALU.mult), reads=[b_gng], writes=[b_gng])
        wmT = sb("wmT", [128, depth, 4, 128], BF16); b_wmT = Buf("wmT")
        for l in range(depth):
            for g in range(4):
                T.op(POOL, lambda e, l=l, g=g: e.memset(wsf[64:128, l, g, 0:64], 0.0), reads=[], writes=[b_wsf])
        T.op(DVE, lambda e: e.tensor_copy(out=wmT[:], in_=wsf), reads=[b_wsf], writes=[b_wmT])
        memb, b_memb = AR.tile("memb", [NCH, NMEM], BF16)
        T.op(DVE, lambda e: e.tensor_copy(out=memb, in_=memf), reads=[b_memf], writes=[b_memb])
        bterm = sb("bterm", [128, depth, 4, 128]); b_bterm = Buf("bterm")
        rsr = sb("rsr", [1, 128]); b_rsr = Buf("rsr")
        crot = Rot([0, 1])
        for l in range(depth):
            for g in range(4):
                pt, pb = crot.next()
                T.op(PE, lambda e, pt=pt, l=l, g=g: e.matmul(pt[0:1, 0:128], onescol[:, 0:1], wsf[:, l, g, :],
                                                              start=True, stop=True),
                     reads=[b_onescol, b_wsf], writes=[pb])
                T.op(ACT, lambda e, pt=pt: e.copy(out=rsr[:], in_=pt[0:1, 0:128]), reads=[pb], writes=[b_rsr])
                pt2, pb2 = crot.next()
                T.op(PE, lambda e, pt2=pt2, l=l, g=g: e.matmul(pt2[:, 0:128], slnbr[0:1, l, g * 128:(g + 1) * 128],
                                                                rsr[0:1, :], start=True, stop=False),
                     reads=[b_slnbr, b_rsr], writes=[pb2], inc=False)
                T.op(PE, lambda e, pt2=pt2, l=l, g=g: e.matmul(pt2[:, 0:128], onesr[0:1, :],
                                                                bsr[0:1, l, g * 128:(g + 1) * 128],
                                                                start=False, stop=True),
                     reads=[b_onesr, b_bsr], writes=[pb2])
                T.op(ACT, lambda e, pt2=pt2, l=l, g=g: e.copy(out=bterm[:, l, g, :], in_=pt2[:, 0:128]),
                     reads=[pb2], writes=[b_bterm])

        castctx = [newctx("cast%d" % i) for i in range(8)]
        unit_buf = [Buf("unit%d" % u) for u in range(NU)]
        cast_i = [0]

        def kp(ap2d):
            return ap2d.rearrange("(k p) c -> p k c", p=128)

        UIDX = {}
        USRC = {}
        for l in range(depth):
            base = l * UPL
            names = ["wk0", "wk1", "wv0", "wv1", "inA", "inB", "inC", "inD", "inE", "wout0", "wout1",
                     "wq0", "wq1", "wo0", "wo1"]
            for i, n in enumerate(names):
                UIDX[(l, n)] = base + i
            USRC[(l, "wk0")] = (kp(wk_x[l, :, 0:512]), 8); USRC[(l, "wk1")] = (kp(wk_x[l, :, 512:1024]), 8)
            USRC[(l, "wv0")] = (kp(wv_x[l, :, 0:512]), 8); USRC[(l, "wv1")] = (kp(wv_x[l, :, 512:1024]), 8)
            USRC[(l, "inA")] = (kp(w_in[l, :, 0:512]), 8); USRC[(l, "inB")] = (kp(w_in[l, :, 512:1024]), 8)
            USRC[(l, "inC")] = (kp(w_in[l, :, 1024:1536]), 8); USRC[(l, "inD")] = (kp(w_in[l, :, 1552:2064]), 8)
            USRC[(l, "inE")] = (kp(w_in[l, :, 2064:2576]), 8)
            USRC[(l, "wout0")] = (kp(w_out[l, :, 0:512]), 8); USRC[(l, "wout1")] = (kp(w_out[l, :, 512:1024]), 8)
            USRC[(l, "wq0")] = (kp(wq_x[l, :, 0:512]), 8); USRC[(l, "wq1")] = (kp(wq_x[l, :, 512:1024]), 8)
            USRC[(l, "wo0")] = (kp(wo_x[l, :, 0:512]), 8); USRC[(l, "wo1")] = (kp(wo_x[l, :, 512:1024]), 8)
            for e_ in range(NEXP):
                UIDX[(l, "wg", e_)] = base + 15 + 3 * e_
                UIDX[(l, "wu", e_)] = base + 15 + 3 * e_ + 1
                UIDX[(l, "wd", e_)] = base + 15 + 3 * e_ + 2
                USRC[(l, "wg", e_)] = (kp(w_gate[l, e_]), 8)
                USRC[(l, "wu", e_)] = (kp(w_up[l, e_]), 8)
                USRC[(l, "wd", e_)] = (kp(w_down[l, e_]), 4)

        cast_done = set()

        def emit_cast(key):
            if key in cast_done:
                return
            cast_done.add(key)
            u = UIDX[key]
            src, k = USRC[key]
            c = castctx[cast_i[0] % len(castctx)]
            cast_i[0] += 1
            kh = k // 2
            for hh in range(2):
                T.dma(POOL, c, lambda e, u=u, s=src, k=k, hh=hh, kh=kh: e.dma_start(
                    out=wsc[u].rearrange("p (k c) -> p k c", k=k)[:, hh * kh:(hh + 1) * kh, :],
                    in_=s[:, hh * kh:(hh + 1) * kh, :]), writes=[unit_buf[u]], serial=True)

        ring = []
        for i in range(RING):
            t = sb("ring%d" % i, [128, 4096], BF16)
            ring.append((t, Buf("ring%d" % i), newctx("ring%d" % i)))

        stream = []
        for l in range(depth):
            for n in ["wk0", "wk1", "wv0", "wv1"]:
                stream.append((l, n))
        for ti in range(NT):
            for l in range(depth):
                for n in ["inE", "inA", "inB", "inD", "inC", "wout0", "wout1", "wq0", "wq1", "wo0", "wo1"]:
                    stream.append((l, n))
                for e_ in range(NEXP):
                    stream.append((l, "wg", e_)); stream.append((l, "wu", e_)); stream.append((l, "wd", e_))
        issued = [0]
        taken = [0]
        casted = [0]
        released = set()
        loaded = {}
        LOOKAHEAD = RING - 1
        CASTAHEAD = 10

        def pump():
            while casted[0] < len(stream) and casted[0] < taken[0] + LOOKAHEAD + CASTAHEAD:
                emit_cast(stream[casted[0]])
                casted[0] += 1
            while issued[0] < len(stream) and issued[0] < taken[0] + LOOKAHEAD:
                k = issued[0]
                if k >= RING and (k - RING) not in released:
                    break
                u = UIDX[stream[k]]
                t, b, c = ring[k % RING]
                T.dma(SP, c, lambda e, t=t, u=u: e.dma_start(out=t[:], in_=wsc[u]), reads=[unit_buf[u]], writes=[b])
                loaded[k] = (t, b, k)
                issued[0] += 1

        def take(key):
            k = taken[0]
            assert stream[k] == key, (stream[k], key)
            pump()
            assert k in loaded, ("ring stall", key, k, issued[0], sorted(released)[-4:])
            r = loaded.pop(k)
            taken[0] += 1
            pump()
            return r

        def rel(unit):
            released.add(unit[2])
            pump()

        xres = sb("xres", [128, NCH, TT]); b_xres = [Buf("xres%d" % c) for c in range(NCH)]
        lt = [(sb("lt%d" % i, [128, TT]), Buf("lt%d" % i)) for i in range(2)]
        xbf = sb("xbf", [128, NCH, TT], BF16); b_xbf = [Buf("xbf%d" % c) for c in range(NCH)]
        xpre = xres
        c_xin = newctx("xin")
        c_xnext = newctx("xnext")
        xnext = sb("xnext", [128, NCH, TT]); b_xnext = Buf("xnext")
        KT = sb("KT", [128, depth, NCH, NMEM], BF16); b_KT = Buf("KT")
        Vm = sb("Vm", [128, depth, 2, D], BF16); b_Vm = Buf("Vm")
        Sst = sb("Sst", [128, depth, 2, 256]); b_S = [[Buf("S%d%d" % (l, j)) for j in range(2)] for l in range(depth)]
        T.op(POOL, lambda e: e.memset(Sst[:], 0.0), writes=[b for row in b_S for b in row])
        ltp = (sb("ltp", [128, TT]), Buf("ltp"))
        lnv = sb("lnv", [128, TT]); b_lnv = Buf("lnv")
        lnr = sb("lnr", [128, TT]); b_lnr = Buf("lnr")
        lnn = sb("lnn", [128, TT]); b_lnn = Buf("lnn")

        kvrot = Rot([2, 3, 4, 5])
        for l in range(depth):
            wk_t = [take((l, "wk0")), take((l, "wk1"))]
            for ec in range(NCH):
                wt, wb, _ = wk_t[ec // 4]
                wv = wt[:].rearrange("p (k c) -> p k c", k=8)
                pt, pb = kvrot.next()
                for kc in range(NCH):
                    T.op(PE, lambda e, pt=pt, wv=wv, kc=kc, ec=ec: e.matmul(
                        pt[:, 0:NMEM], wv[:, kc, (ec % 4) * 128:(ec % 4 + 1) * 128], memb[:, kc, :],
                        start=(kc == 0), stop=(kc == NCH - 1)),
                        reads=[wb, b_memb], writes=[pb], inc=(kc == NCH - 1))
                T.op(ACT, lambda e, pt=pt, l=l, ec=ec: e.mul(out=KT[:, l, ec, :], in_=pt[:, 0:NMEM], mul=1.0 / 16.0),
                     reads=[pb], writes=[b_KT])
            rel(wk_t[0]); rel(wk_t[1])
            wv_t = [take((l, "wv0")), take((l, "wv1"))]
            for mc in range(2):
                for hf in range(2):
                    wt, wb, _ = wv_t[hf]
                    wv = wt[:].rearrange("p (k c) -> p k c", k=8)
                    pt, pb = kvrot.next()
                    for kc in range(NCH):
                        T.op(PE, lambda e, pt=pt, wv=wv, kc=kc, mc=mc: e.matmul(
                            pt[:, :], memb[:, kc, mc * 128:(mc + 1) * 128], wv[:, kc, :],
                            start=(kc == 0), stop=(kc == NCH - 1)),
                            reads=[wb, b_memb], writes=[pb], inc=(kc == NCH - 1))
                    T.op(DVE, lambda e, pt=pt, l=l, mc=mc, hf=hf: e.tensor_copy(
                        out=Vm[:, l, mc, hf * 512:(hf + 1) * 512], in_=pt[:, :]), reads=[pb], writes=[b_Vm])
            rel(wv_t[0]); rel(wv_t[1])

        pool_ok = [False]

        def stats_ops(dc, st):
            pm, pmb, pv, pvb, sqb, b_sqb = st
            T.op(ACT, lambda e, dc=dc: e.copy(out=xbf[:, dc, :], in_=xres[:, dc, :]), reads=[b_xres[dc]], writes=[b_xbf[dc]])
            if False:
                T.op(POOL, lambda e, dc=dc, sqb=sqb: e.tensor_tensor(out=sqb[:, dc, :], in0=xres[:, dc, :], in1=xres[:, dc, :], op=ALU.mult),
                     reads=[b_xres[dc]], writes=[b_sqb])
            else:
                T.op(ACT, lambda e, dc=dc, sqb=sqb: e.activation(out=sqb[:, dc, :], in_=xres[:, dc, :], func=AF.Square),
                     reads=[b_xres[dc]], writes=[b_sqb])

        def stats_mm(dc, st):
            pm, pmb, pv, pvb, sqb, b_sqb = st
            T.op(PE, lambda e, dc=dc, pm=pm: e.matmul(pm[:, :], onesm[:], xbf[:, dc, :], start=(dc == 0), stop=(dc == NCH - 1)),
                 reads=[b_onesm, b_xbf[dc]], writes=[pmb], inc=(dc == NCH - 1))
            T.op(PE, lambda e, dc=dc, pv=pv, sqb=sqb: e.matmul(pv[:, :], onesm[:], sqb[:, dc, :], start=(dc == 0), stop=(dc == NCH - 1)),
                 reads=[b_onesm, b_sqb], writes=[pvb], inc=(dc == NCH - 1))

        def layer_norm(l, which, st, use_pool=True, final=False):
            col0 = (l * 3 + which) * NCH
            pm, pmb, pv, pvb, sqb, b_sqb = st
            T.op(ACT, lambda e, pm=pm: e.activation(out=lnv[:], in_=pm[:, :], func=AF.Square), reads=[pmb], writes=[b_lnv])
            T.op(DVE, lambda e, pv=pv: e.tensor_tensor(out=lnv[:], in0=pv[:, :], in1=lnv[:], op=ALU.subtract),
                 reads=[pvb, b_lnv], writes=[b_lnv])
            T.op(DVE, lambda e: e.tensor_scalar(out=lnv[:], in0=lnv[:], scalar1=0.0, scalar2=LN_EPS, op0=ALU.max, op1=ALU.add),
                 reads=[b_lnv], writes=[b_lnv])
            T.op(ACT, lambda e: e.activation(out=lnr[:], in_=lnv[:], func=AF.Ln), reads=[b_lnv], writes=[b_lnr])
            T.op(ACT, lambda e: e.activation(out=lnr[:], in_=lnr[:], func=AF.Exp, scale=-0.5), reads=[b_lnr], writes=[b_lnr])
            T.op(DVE, lambda e, pm=pm: e.scalar_tensor_tensor(out=lnn[:], in0=pm[:, :], scalar=-1.0, in1=lnr[:], op0=ALU.mult, op1=ALU.mult),
                 reads=[pmb, b_lnr], writes=[b_lnn])
            for c in range(NCH):
                T.op(DVE, lambda e, c=c: e.tensor_tensor(out=xres[:, c, :], in0=xres[:, c, :], in1=lnr[:], op=ALU.mult),
                     reads=[b_xres[c], b_lnr], writes=[b_xres[c]])
                T.op(DVE, lambda e, c=c: e.tensor_tensor(out=xres[:, c, :], in0=xres[:, c, :], in1=lnn[:], op=ALU.add),
                     reads=[b_xres[c], b_lnn], writes=[b_xres[c]])
                if final:
                    T.op(ACT, lambda e, c=c: e.activation(out=xres[:, c, :], in_=xres[:, c, :], func=AF.Identity,
                                                          scale=lng[:, col0 + c:col0 + c + 1], bias=lnb[:, col0 + c:col0 + c + 1]),
                         reads=[b_xres[c], b_lng, b_lnb], writes=[b_xres[c]])
                    continue
                T.op(ACT, lambda e, c=c: e.activation(out=xbf[:, c, :], in_=xres[:, c, :], func=AF.Identity,
                                                      scale=lng[:, col0 + c:col0 + c + 1], bias=lnb[:, col0 + c:col0 + c + 1]),
                     reads=[b_xres[c], b_lng, b_lnb], writes=[b_xbf[c]])
            if final:
                return
            for c in range(NCH):
                T.op(ACT, lambda e, c=c: e.activation(out=xres[:, c, :], in_=xres[:, c, :], func=AF.Identity,
                                                      scale=lng[:, col0 + c:col0 + c + 1], bias=lnb[:, col0 + c:col0 + c + 1]),
                     reads=[b_xres[c], b_lng, b_lnb], writes=[b_xres[c]])

        def proj_residual(units, src, b_src, rot, st):
            for dc in range(NCH):
                wt, wb, _ = units[dc // 4]
                wv = wt[:].rearrange("p (k c) -> p k c", k=8)
                pt, pb = rot.next()
                for ic in range(NCH):
                    T.op(PE, lambda e, pt=pt, wv=wv, ic=ic, dc=dc: e.matmul(
                        pt[:, :], wv[:, ic, (dc % 4) * 128:(dc % 4 + 1) * 128], src[:, ic, :],
                        start=(ic == 0), stop=(ic == NCH - 1)),
                        reads=[wb, b_src], writes=[pb], inc=(ic == NCH - 1))
                T.op(DVE, lambda e, pt=pt, dc=dc: e.scalar_tensor_tensor(
                    out=xpre[:, dc, :], in0=xres[:, dc, :], scalar=ALPHA, in1=pt[:, :], op0=ALU.mult, op1=ALU.add),
                    reads=[b_xres[dc], pb], writes=[b_xres[dc]])
                stats_ops(dc, st)
                if dc >= 5:
                    stats_mm(dc - 5, st)
                if dc % 4 == 3:
                    rel(units[dc // 4])
            for d_ in range(NCH - 5, NCH):
                stats_mm(d_, st)

        def mixer(l, ti, pre_out=None):
            AR.reset()
            ksb, b_ksb = AR.tile("ksb", [4, 256])
            e1 = [AR.tile("e1_%d" % i, [256]) for i in range(2)]
            lbuf, b_lbuf = AR.tile("lbuf", [4, 256])
            edb = [AR.tile("ed_%d" % i, [256]) for i in range(2)]
            r_off = AR.offs["ksb"]
            r_bufs = [b_ksb, e1[0][1], e1[1][1], b_lbuf, edb[0][1], edb[1][1]]
            vbf, b_vbf = AR.tile("vbf", [4, 512], BF16)
            gv, b_gv = AR.tile("gv", [4, 512])
            gsq, b_gsq = AR.tile("gsq", [4, 512])
            vn, b_vn = AR.tile("vn", [4, 512], BF16)
            alow, b_alow = AR.tile("alow", [TT])
            qT, b_qT = AR.tile("qT", [2, TT], BF16)
            gu, b_gu = AR.tile("gu", [4, TT])
            kdec, b_kdec = AR.tile("kdec", [4, 256], BF16)
            decay, b_decay = AR.tile("decay", [128])
            sbf = [[AR.tile("sbf%d%d" % (i, j), [256], BF16) for j in range(2)] for i in range(2)]
            ymix, b_ymix = AR.tile("ymix", [8, TT], BF16)
            st1, b_st1 = AR.tile("st1", [16])
            st2, b_st2 = AR.tile("st2", [16])
            st3, b_st3 = AR.tile("st3", [16])
            stmp = [AR.tile("stmp%d" % i, [4, 128]) for i in range(2)]

            rotA = Rot([0, 1, 2, 3])

            def w8(u):
                return u[0][:].rearrange("p (k c) -> p k c", k=8), u[1]

            def fm_proj(wv, wb, col, M, evac):
                pt, pb = rotA.next()
                for kc in range(NCH):
                    T.op(PE, lambda e, pt=pt, kc=kc: e.matmul(pt[0:M, :], wv[:, kc, col:col + M], xbf[:, kc, :],
                                                             start=(kc == 0), stop=(kc == NCH - 1)),
                         reads=[wb, b_xbf[kc]], writes=[pb], inc=(kc == NCH - 1))
                evac(pt, pb)

            def tm_proj(wv, wb, col, N, blk, evac):
                pt, pb = rotA.next()
                for kc in range(NCH):
                    T.op(PE, lambda e, pt=pt, kc=kc: e.matmul(pt[:, 0:N], xbf[:, kc, blk * 128:(blk + 1) * 128],
                                                             wv[:, kc, col:col + N],
                                                             start=(kc == 0), stop=(kc == NCH - 1)),
                         reads=[wb, b_xbf[kc]], writes=[pb], inc=(kc == NCH - 1))
                evac(pt, pb)

            T.op(DVE, lambda e: e.memset(alow[0:32, :], 1.0), writes=[b_alow])
            uE = take((l, "inE"))
            wE, bE = w8(uE)
            vb = [rotA.next() for _ in range(4)]
            for kc in range(NCH):
                for blk in range(4):
                    pt, pb = vb[blk]
                    T.op(PE, lambda e, pt=pt, kc=kc, blk=blk: e.matmul(pt[:, :], xbf[:, kc, blk * 128:(blk + 1) * 128], wE[:, kc, 0:512],
                                                                      start=(kc == 0), stop=(kc == NCH - 1)),
                         reads=[bE, b_xbf[kc]], writes=[pb], inc=(kc == NCH - 1))
            for blk in range(4):
                pt, pb = vb[blk]
                T.op(ACT, lambda e, pt=pt, blk=blk: e.activation(out=gv[:, blk, :], in_=pt[:, :], func=AF.Gelu_apprx_tanh),
                     reads=[pb], writes=[b_gv])
            rel(uE)
            gvv = gv.rearrange("p a (g c) -> p (a g) c", g=4)
            gsqv = gsq.rearrange("p a (g c) -> p (a g) c", g=4)
            vnv = vn.rearrange("p a (g c) -> p (a g) c", g=4)
            T.op(DVE, lambda e: e.tensor_reduce(out=st1, in_=gvv, axis=AX.X, op=ALU.add), reads=[b_gv], writes=[b_st1])
            T.op(DVE, lambda e: e.tensor_tensor(out=gsq, in0=gv, in1=gv, op=ALU.mult), reads=[b_gv], writes=[b_gsq])
            T.op(DVE, lambda e: e.tensor_reduce(out=st2, in_=gsqv, axis=AX.X, op=ALU.add), reads=[b_gsq], writes=[b_st2])
            T.op(DVE, lambda e: e.tensor_scalar(out=st1, in0=st1, scalar1=1.0 / 128.0, scalar2=None, op0=ALU.mult),
                 reads=[b_st1], writes=[b_st1])
            T.op(DVE, lambda e: e.tensor_tensor(out=st3, in0=st1, in1=st1, op=ALU.mult), reads=[b_st1], writes=[b_st3])
            T.op(DVE, lambda e: e.scalar_tensor_tensor(out=st2, in0=st2, scalar=1.0 / 128.0, in1=st3, op0=ALU.mult, op1=ALU.subtract),
                 reads=[b_st2, b_st3], writes=[b_st2])
            T.op(DVE, lambda e: e.tensor_scalar(out=st2, in0=st2, scalar1=0.0, scalar2=None, op0=ALU.max), reads=[b_st2], writes=[b_st2])
            T.op(ACT, lambda e: e.activation(out=st2, in_=st2, func=AF.Sqrt, bias=LN_EPS, scale=1.0), reads=[b_st2], writes=[b_st2])
            T.op(DVE, lambda e: e.reciprocal(out=st2, in_=st2), reads=[b_st2], writes=[b_st2])
            T.op(DVE, lambda e: e.scalar_tensor_tensor(out=st3, in0=st1, scalar=-1.0, in1=st2, op0=ALU.mult, op1=ALU.mult),
                 reads=[b_st1, b_st2], writes=[b_st3])
            T.op(DVE, lambda e: e.tensor_tensor(out=gsqv, in0=gvv, in1=st2.unsqueeze(2).to_broadcast([128, 16, 128]), op=ALU.mult),
                 reads=[b_gv, b_st2], writes=[b_gsq])
            T.op(DVE, lambda e: e.tensor_tensor(out=vnv, in0=gsqv, in1=st3.unsqueeze(2).to_broadcast([128, 16, 128]), op=ALU.add),
                 reads=[b_gsq, b_st3], writes=[b_vn])

            alw_v = alwb[:, l, :, :]
            fm_proj(alw_v, b_alwb, 0, 16, lambda pt, pb: T.op(
                ACT, lambda e: e.copy(out=alow[0:16, :], in_=pt[0:16, :]), reads=[pb], writes=[b_alow]))
            uA = take((l, "inA"))
            wA, bA = w8(uA)
            for blk in range(4):
                tm_proj(wA, bA, 256, 256, blk, lambda pt, pb, blk=blk: T.op(
                    ACT, lambda e: e.copy(out=ksb[:, blk, :], in_=pt[:, 0:256]), reads=[pb], writes=[b_ksb]))
            for mc in range(2):
                fm_proj(wA, bA, mc * 128, 128, lambda pt, pb, mc=mc: T.op(
                    ACT, lambda e: e.mul(out=qT[:, mc, :], in_=pt[:, :], mul=0.125), reads=[pb], writes=[b_qT]))
            rel(uA)
            uB = take((l, "inB"))
            wB, bB = w8(uB)
            wa2v = wa2[0:17, l, :]
            bl_bank, bl_buf = banks[7]
            for blk in range(4):
                pz, pzb = rotA.next()
                T.op(PE, lambda e, pz=pz, blk=blk: e.matmul(pz[:, 0:256], alow[0:17, blk * 128:(blk + 1) * 128], wa2v,
                                                            start=True, stop=True),
                     reads=[b_alow, b_wa2], writes=[pzb])
                e1t, e1b = e1[blk % 2]
                T.op(ACT, lambda e, pz=pz, e1t=e1t: e.activation(out=e1t, in_=pz[:, 0:256], func=AF.Exp, scale=-1.0),
                     reads=[pzb], writes=[e1b])
                T.op(ACT, lambda e, e1t=e1t, blk=blk: e.activation(out=lbuf[:, blk, :], in_=e1t, func=AF.Ln, bias=1.0, scale=1.0),
                     reads=[e1b], writes=[b_lbuf])
                tm_proj(wB, bB, 0, 512, blk, lambda pt, pb, blk=blk: T.op(
                    ACT, lambda e: e.copy(out=vbf[:, blk, :], in_=pt[:, :]), reads=[pb], writes=[b_vbf]))
                pd, pdb = rotA.next()
                T.op(PE, lambda e, pd=pd, blk=blk: e.matmul(pd[:, 0:256], maskd[:], lbuf[:, blk, :], start=True, stop=True),
                     reads=[b_maskd, b_lbuf], writes=[pdb])
                edt, edbuf = edb[blk % 2]
                T.op(ACT, lambda e, pd=pd, edt=edt: e.activation(out=edt, in_=pd[:, 0:256], func=AF.Exp),
                     reads=[pdb], writes=[edbuf])
                T.op(DVE, lambda e, edt=edt, blk=blk: e.tensor_tensor(out=kdec[:, blk, :], in0=ksb[:, blk, :], in1=edt, op=ALU.mult),
                     reads=[b_ksb, edbuf], writes=[b_kdec])
                for j in range(2):
                    cidx = (blk * 2 + j) * 16
                    T.op(PE, lambda e, blk=blk, j=j, cidx=cidx: e.matmul(
                        bl_bank[:, cidx:cidx + 16], lbuf[:, blk, j * 128:(j + 1) * 128], csel[:, :], start=True, stop=True),
                        reads=[b_lbuf, b_csel], writes=[bl_buf], inc=True)
            rel(uB)
            T.op(ACT, lambda e: e.activation(out=decay, in_=bl_bank[:, 0:128], func=AF.Exp), reads=[bl_buf], writes=[b_decay])
            uD = take((l, "inD"))
            wD, bD = w8(uD)
            for mc in range(4):
                fm_proj(wD, bD, mc * 128, 128, lambda pt, pb, mc=mc: T.op(
                    ACT, lambda e: e.activation(out=gu[:, mc, :], in_=pt[:, :], func=AF.Gelu_apprx_tanh), reads=[pb], writes=[b_gu]))
            rel(uD)
            for g in range(4):
                pm, pmb = rotA.next()
                for blk in range(4):
                    T.op(PE, lambda e, pm=pm, g=g, blk=blk: e.matmul(
                        pm[:, blk * 128:(blk + 1) * 128], vn[:, blk, g * 128:(g + 1) * 128], wmT[:, l, g, :],
                        start=True, stop=True), reads=[b_vn, b_wmT], writes=[pmb], inc=(blk == 3))
                sm, smb = stmp[g % 2]
                T.op(DVE, lambda e, pm=pm, g=g, sm=sm: e.scalar_tensor_tensor(
                    out=sm, in0=pm[:, :].rearrange("p (a t) -> p a t", a=4), scalar=slng[:, l, g:g + 1],
                    in1=bterm[:, l, g, :].unsqueeze(1).to_broadcast([128, 4, 128]), op0=ALU.mult, op1=ALU.add),
                    reads=[pmb, b_slng, b_bterm], writes=[smb])
                T.op(DVE, lambda e, g=g, sm=sm: e.tensor_tensor(
                    out=ymix[:, 4 + g, :], in0=sm.rearrange("p a t -> p (a t)"), in1=gu[:, g, :], op=ALU.mult),
                    reads=[smb, b_gu], writes=[b_ymix])
            sr, b_sr = AR.tile_at("sr", AR.offs["gsq"], [4, TT], F32, inherit=[b_gsq])
            osq, b_osq = AR.tile_at("osq", r_off, [4, TT], BF16, inherit=r_bufs)
            rinv = [AR.tile_at("rinv%d" % i, r_off + 4096 + 2048 * i, [TT], F32, inherit=r_bufs) for i in range(2)]
            t1 = [AR.tile_at("t1_%d" % i, r_off + 8192 + 2048 * i, [TT], F32, inherit=r_bufs) for i in range(2)]
            uC = take((l, "inC"))
            wC, bC = w8(uC)
            oT = [banks[4 + h] for h in range(4)]
            for c in range(8):
                blk, half = c // 2, c % 2
                r0, r1 = half * 64, half * 64 + 64
                for j in range(2):
                    pk, pkb = rotA.next()
                    T.op(PE, lambda e, pk=pk, blk=blk, j=j, r0=r0, r1=r1: e.matmul(
                        pk[:, 0:256], kdec[r0:r1, blk, j * 128:(j + 1) * 128], vbf[r0:r1, blk, j * 256:(j + 1) * 256],
                        start=True, stop=True), reads=[b_kdec, b_vbf], writes=[pkb])
                    dcol = (blk * 2 + j) * 16 + half
                    T.op(DVE, lambda e, pk=pk, j=j, dcol=dcol: e.scalar_tensor_tensor(
                        out=Sst[:, l, j, :], in0=Sst[:, l, j, :], scalar=decay[:, dcol:dcol + 1], in1=pk[:, 0:256],
                        op0=ALU.mult, op1=ALU.add), reads=[b_S[l][j], b_decay, pkb], writes=[b_S[l][j]])
                    st, stb = sbf[c % 2][j]
                    T.op(ACT, lambda e, st=st, j=j: e.copy(out=st, in_=Sst[:, l, j, :]), reads=[b_S[l][j]], writes=[stb])
                if c % 2 == 0:
                    mc = c // 2
                    fm_proj(wC, bC, mc * 128, 128, lambda pt, pb, mc=mc: T.op(
                        ACT, lambda e: e.activation(out=sr[:, mc, :], in_=pt[:, :], func=AF.Silu), reads=[pb], writes=[b_sr]))
                for j in range(2):
                    st, stb = sbf[c % 2][j]
                    hA, hB = 2 * j, 2 * j + 1
                    T.op(PE, lambda e, st=st, j=j, c=c, hA=hA: e.matmul(
                        oT[hA][0][:, c * 64:(c + 1) * 64], st[0:64, 0:128], qT[0:64, j, c * 64:(c + 1) * 64],
                        start=True, stop=True), reads=[stb, b_qT], writes=[oT[hA][1]])
                    T.op(PE, lambda e, st=st, j=j, c=c, hB=hB: e.matmul(
                        oT[hB][0][:, c * 64:(c + 1) * 64], st[64:128, 128:256], qT[64:128, j, c * 64:(c + 1) * 64],
                        start=True, stop=True), reads=[stb, b_qT], writes=[oT[hB][1]])
            rel(uC)
            for h in range(4):
                ob, obb = oT[h]
                T.op(ACT, lambda e, ob=ob, h=h: e.activation(out=osq[:, h, :], in_=ob[:, :], func=AF.Square),
                     reads=[obb], writes=[b_osq])
                pr, prb = rotA.next()
                T.op(PE, lambda e, pr=pr, h=h: e.matmul(pr[:, :], onesb[:], osq[:, h, :], start=True, stop=True),
                     reads=[b_onesb, b_osq], writes=[prb])
                rv, rvb = rinv[h % 2]
                T.op(DVE, lambda e, pr=pr, rv=rv: e.tensor_scalar(out=rv, in0=pr[:, :], scalar1=128.0 * RMS_EPS, scalar2=None, op0=ALU.add),
                     reads=[prb], writes=[rvb])
                T.op(ACT, lambda e, rv=rv: e.activation(out=rv, in_=rv, func=AF.Ln), reads=[rvb], writes=[rvb])
                T.op(ACT, lambda e, rv=rv: e.activation(out=rv, in_=rv, func=AF.Exp, scale=-0.5), reads=[rvb], writes=[rvb])
                tt, ttb = t1[h % 2]
                T.op(DVE, lambda e, ob=ob, rv=rv, tt=tt: e.tensor_tensor(out=tt, in0=ob[:, :], in1=rv, op=ALU.mult),
                     reads=[obb, rvb], writes=[ttb])
                T.op(DVE, lambda e, tt=tt, h=h: e.scalar_tensor_tensor(
                    out=ymix[:, h, :], in0=tt, scalar=gng[:, l, h:h + 1], in1=sr[:, h, :], op0=ALU.mult, op1=ALU.mult),
                    reads=[ttb, b_gng, b_sr], writes=[b_ymix])
            if pre_out is not None:
                pre_out()
            uo = [take((l, "wout0")), take((l, "wout1"))]
            sqt = AR.tile_at("sqb", AR.offs["gv"], [NCH, TT], BF16, inherit=[b_gv, b_gsq])
            st = (banks[4][0], banks[4][1], banks[5][0], banks[5][1], sqt[0], sqt[1])
            proj_residual(uo, ymix, b_ymix, rotA, st)
            return st

        def xattn(l, ti):
            AR.reset()
            qT8, b_qT8 = AR.tile("qT8", [8, TT], BF16)
            eT, b_eT = AR.tile("eT", [4, 2, TT], BF16)
            rden = [AR.tile("rden%d" % i, [TT]) for i in range(2)]
            oT8, b_oT8 = AR.tile("oT8", [8, TT], BF16)
            sqt = AR.tile("sqb", [NCH, TT], BF16)
            rot = Rot([0, 1, 2, 3, 4, 5])
            uq = [take((l, "wq0")), take((l, "wq1"))]
            for half in range(2):
                wt, wb, _ = uq[half]
                wv = wt[:].rearrange("p (k c) -> p k c", k=8)
                qb = [rot.next() for _ in range(4)]
                for kc in range(NCH):
                    for i4 in range(4):
                        pt, pb = qb[i4]
                        T.op(PE, lambda e, pt=pt, wv=wv, kc=kc, i4=i4: e.matmul(
                            pt[:, :], wv[:, kc, i4 * 128:(i4 + 1) * 128], xbf[:, kc, :],
                            start=(kc == 0), stop=(kc == NCH - 1)), reads=[wb, b_xbf[kc]], writes=[pb], inc=(kc == NCH - 1))
                for i4 in range(4):
                    pt, pb = qb[i4]
                    ec = half * 4 + i4
                    if ec % 2 == 0:
                        T.op(ACT, lambda e, pt=pt, ec=ec: e.copy(out=qT8[:, ec, :], in_=pt[:, :]), reads=[pb], writes=[b_qT8])
                    else:
                        T.op(DVE, lambda e, pt=pt, ec=ec: e.tensor_copy(out=qT8[:, ec, :], in_=pt[:, :]), reads=[pb], writes=[b_qT8])
                rel(uq[half])
            for h in range(4):
                for mc in range(2):
                    pt, pb = rot.next()
                    for j in range(2):
                        T.op(PE, lambda e, pt=pt, h=h, mc=mc, j=j: e.matmul(
                            pt[:, :], KT[:, l, 2 * h + j, mc * 128:(mc + 1) * 128], qT8[:, 2 * h + j, :],
                            start=(j == 0), stop=(j == 1)), reads=[b_KT, b_qT8], writes=[pb], inc=(j == 1))
                    T.op(ACT, lambda e, pt=pt, h=h, mc=mc: e.activation(out=eT[:, h, mc, :], in_=pt[:, :], func=AF.Exp),
                         reads=[pb], writes=[b_eT])
                pd, pdb = rot.next()
                for mc in range(2):
                    T.op(PE, lambda e, pd=pd, h=h, mc=mc: e.matmul(pd[:, :], onesb[:], eT[:, h, mc, :],
                                                                  start=(mc == 0), stop=(mc == 1)),
                         reads=[b_onesb, b_eT], writes=[pdb], inc=(mc == 1))
                rd, rdb = rden[h % 2]
                T.op(ACT, lambda e, pd=pd, rd=rd: e.activation(out=rd, in_=pd[:, :], func=AF.Ln), reads=[pdb], writes=[rdb])
                T.op(ACT, lambda e, rd=rd: e.activation(out=rd, in_=rd, func=AF.Exp, scale=-1.0), reads=[rdb], writes=[rdb])
                for j in range(2):
                    po, pob = rot.next()
                    ecol = (2 * h + j) * 128
                    for mc in range(2):
                        T.op(PE, lambda e, po=po, h=h, mc=mc, ecol=ecol: e.matmul(
                            po[:, :], Vm[:, l, mc, ecol:ecol + 128], eT[:, h, mc, :], start=(mc == 0), stop=(mc == 1)),
                            reads=[b_Vm, b_eT], writes=[pob], inc=(mc == 1))
                    T.op(DVE, lambda e, po=po, rd=rd, h=h, j=j: e.tensor_tensor(
                        out=oT8[:, 2 * h + j, :], in0=po[:, :], in1=rd, op=ALU.mult), reads=[pob, rdb], writes=[b_oT8])
            uo = [take((l, "wo0")), take((l, "wo1"))]
            st = (banks[6][0], banks[6][1], banks[7][0], banks[7][1], sqt[0], sqt[1])
            proj_residual(uo, oT8, b_oT8, rot, st)
            return st

        def moe(l, ti):
            AR.reset()
            lg, b_lg = AR.tile("lg", [4, 16])
            sc, b_sc = AR.tile("sc", [4, 16])
            pairs, b_pairs = AR.tile("pairs", [16, 6])
            gs, b_gs = AR.tile("gs", [4, 4])
            goh, b_goh = AR.tile("goh", [4, 4])
            msk, b_msk = AR.tile("msk", [4, 16])
            m1, b_m1 = AR.tile("m1", [4, 16])
            rem, b_rem = AR.tile("rem", [4, 16])
            tmpm, b_tmpm = AR.tile("tmpm", [4, 16])
            gate, b_gate = AR.tile("gate", [4, 16])
            gatebf, b_gatebf = AR.tile("gatebf", [4, 16], BF16)
            s4a, b_s4a = AR.tile("s4a", [4])
            s4b, b_s4b = AR.tile("s4b", [4])
            s4c, b_s4c = AR.tile("s4c", [4])
            sgt = [AR.tile("sg%d" % i, [TT]) for i in range(2)]
            tmt = [AR.tile("tm%d" % i, [TT]) for i in range(8)]
            hT = [AR.tile("hT%d" % i, [4, TT], BF16) for i in range(2)]
            sqt = AR.tile("sqb", [NCH, TT], BF16)

            def router():
                pr, prb = banks[7]
                for blk in range(4):
                    for kc in range(NCH):
                        T.op(PE, lambda e, blk=blk, kc=kc: e.matmul(
                            pr[:, blk * 16:(blk + 1) * 16], xres[:, kc, blk * 128:(blk + 1) * 128], wr[:, kc, :],
                            start=(kc == 0), stop=False), reads=[b_xres[kc], b_wr], writes=[prb], inc=False)
                    T.op(PE, lambda e, blk=blk: e.matmul(pr[:, blk * 16:(blk + 1) * 16], onesr[0:1, :], br[0:1, :],
                                                         start=False, stop=True), reads=[b_onesr, b_br], writes=[prb])
                B4 = lambda ap: ap.unsqueeze(2).to_broadcast([128, 4, 16])
                T.op(DVE, lambda e: e.tensor_copy(out=lg, in_=pr[:, 0:64].rearrange("p (a b) -> p a b", a=4)), reads=[prb], writes=[b_lg])
                T.op(DVE, lambda e: e.tensor_reduce(out=s4a, in_=lg, axis=AX.X, op=ALU.max), reads=[b_lg], writes=[b_s4a])
                T.op(DVE, lambda e: e.tensor_tensor(out=lg, in0=lg, in1=B4(s4a), op=ALU.subtract), reads=[b_lg, b_s4a], writes=[b_lg])
                T.op(ACT, lambda e: e.activation(out=sc, in_=lg, func=AF.Exp), reads=[b_lg], writes=[b_sc])
                T.op(DVE, lambda e: e.tensor_reduce(out=s4b, in_=sc, axis=AX.X, op=ALU.add), reads=[b_sc], writes=[b_s4b])
                T.op(DVE, lambda e: e.reciprocal(out=s4b, in_=s4b), reads=[b_s4b], writes=[b_s4b])
                T.op(DVE, lambda e: e.tensor_tensor(out=sc, in0=sc, in1=B4(s4b), op=ALU.mult), reads=[b_sc, b_s4b], writes=[b_sc])
                scg = sc.rearrange("p a (g k) -> p (a g) k", g=4)
                T.op(DVE, lambda e: e.tensor_tensor(out=pairs[:, :, 0:3], in0=scg[:, :, 0:3], in1=scg[:, :, 1:4], op=ALU.add),
                     reads=[b_sc], writes=[b_pairs])
                T.op(DVE, lambda e: e.tensor_tensor(out=pairs[:, :, 3:5], in0=scg[:, :, 0:2], in1=scg[:, :, 2:4], op=ALU.add),
                     reads=[b_sc], writes=[b_pairs])
                T.op(DVE, lambda e: e.tensor_tensor(out=pairs[:, :, 5:6], in0=scg[:, :, 0:1], in1=scg[:, :, 3:4], op=ALU.add),
                     reads=[b_sc], writes=[b_pairs])
                T.op(DVE, lambda e: e.tensor_reduce(out=gs.rearrange("p a g -> p (a g)"), in_=pairs, axis=AX.X, op=ALU.max),
                     reads=[b_pairs], writes=[b_gs])
                T.op(DVE, lambda e: e.tensor_reduce(out=s4c, in_=gs, axis=AX.X, op=ALU.max), reads=[b_gs], writes=[b_s4c])
                T.op(DVE, lambda e: e.tensor_tensor(out=goh, in0=gs, in1=s4c.unsqueeze(2).to_broadcast([128, 4, 4]), op=ALU.is_equal),
                     reads=[b_gs, b_s4c], writes=[b_goh])
                T.op(DVE, lambda e: e.tensor_tensor(
                    out=msk.rearrange("p a (g k) -> p (a g) k", g=4), in0=scg,
                    in1=goh.rearrange("p a g -> p (a g)").unsqueeze(2).to_broadcast([128, 16, 4]), op=ALU.mult),
                    reads=[b_sc, b_goh], writes=[b_msk])
                T.op(DVE, lambda e: e.tensor_reduce(out=s4a, in_=msk, axis=AX.X, op=ALU.max), reads=[b_msk], writes=[b_s4a])
                T.op(DVE, lambda e: e.tensor_tensor(out=m1, in0=msk, in1=B4(s4a), op=ALU.is_equal), reads=[b_msk, b_s4a], writes=[b_m1])
                T.op(DVE, lambda e: e.tensor_tensor(out=tmpm, in0=msk, in1=m1, op=ALU.mult), reads=[b_msk, b_m1], writes=[b_tmpm])
                T.op(DVE, lambda e: e.tensor_tensor(out=rem, in0=msk, in1=tmpm, op=ALU.subtract), reads=[b_msk, b_tmpm], writes=[b_rem])
                T.op(DVE, lambda e: e.tensor_reduce(out=s4b, in_=rem, axis=AX.X, op=ALU.max), reads=[b_rem], writes=[b_s4b])
                T.op(DVE, lambda e: e.tensor_tensor(out=m1, in0=rem, in1=B4(s4b), op=ALU.is_equal), reads=[b_rem, b_s4b], writes=[b_m1])
                T.op(DVE, lambda e: e.tensor_tensor(out=rem, in0=rem, in1=m1, op=ALU.mult), reads=[b_rem, b_m1], writes=[b_rem])
                T.op(DVE, lambda e: e.tensor_tensor(out=tmpm, in0=tmpm, in1=rem, op=ALU.add), reads=[b_tmpm, b_rem], writes=[b_tmpm])
                T.op(DVE, lambda e: e.tensor_tensor(out=s4c, in0=s4a, in1=s4b, op=ALU.add), reads=[b_s4a, b_s4b], writes=[b_s4c])
                T.op(DVE, lambda e: e.reciprocal(out=s4c, in_=s4c), reads=[b_s4c], writes=[b_s4c])
                T.op(DVE, lambda e: e.tensor_tensor(out=gate, in0=tmpm, in1=B4(s4c), op=ALU.mult), reads=[b_tmpm, b_s4c], writes=[b_gate])
                T.op(DVE, lambda e: e.tensor_copy(out=gatebf, in_=gate), reads=[b_gate], writes=[b_gatebf])
            rot_gu = Rot([0, 1, 2, 3])
            rot_gate = Rot([4, 5])
            rot_y = Rot([6, 7])
            state = {}

            def gu_mm(e_):
                wg = take((l, "wg", e_)); wu = take((l, "wu", e_)); wd = take((l, "wd", e_))
                wgv = wg[0][:].rearrange("p (k c) -> p k c", k=8)
                wuv = wu[0][:].rearrange("p (k c) -> p k c", k=8)
                tms = []

                def evac(fc, pg, pgb, pu, pub):
                    sg, sgb = sgt[fc % 2]
                    T.op(ACT, lambda e, pg=pg, sg=sg: e.activation(out=sg, in_=pg[:, :], func=AF.Silu), reads=[pgb], writes=[sgb])
                    tm, tmb = tmt[(e_ % 2) * 4 + fc]
                    T.op(DVE, lambda e, pu=pu, sg=sg, tm=tm: e.tensor_tensor(out=tm, in0=pu[:, :], in1=sg, op=ALU.mult),
                         reads=[pub, sgb], writes=[tmb])
                    tms.append((tm, tmb))

                def grp(pt, pb, wvv, wbb, fc, kcs):
                    for kc in kcs:
                        T.op(PE, lambda e, pt=pt, wvv=wvv, kc=kc, fc=fc: e.matmul(
                            pt[:, :], wvv[:, kc, fc * 128:(fc + 1) * 128], xbf[:, kc, :], start=(kc == 0), stop=(kc == NCH - 1)),
                            reads=[wbb, b_xbf[kc]], writes=[pb], inc=(kc == NCH - 1))

                for fp in range(2):
                    if e_ == 0 and fp == 0:
                        bk = [rot_gu.next() for _ in range(4)]
                        for kc in range(NCH):
                            for i4 in range(4):
                                wvv, wbb = (wgv, wg[1]) if i4 % 2 == 0 else (wuv, wu[1])
                                grp(bk[i4][0], bk[i4][1], wvv, wbb, i4 // 2, [kc])
                        for i2 in range(2):
                            evac(i2, bk[2 * i2][0], bk[2 * i2][1], bk[2 * i2 + 1][0], bk[2 * i2 + 1][1])
                    else:
                        for i2 in range(2):
                            fc = fp * 2 + i2
                            pg, pgb = rot_gu.next()
                            grp(pg, pgb, wgv, wg[1], fc, range(NCH))
                            pu, pub = rot_gu.next()
                            grp(pu, pub, wuv, wu[1], fc, range(NCH))
                            evac(fc, pg, pgb, pu, pub)
                rel(wg); rel(wu)
                state[e_] = (wd, tms)

            def gate_mm(e_):
                wd, tms = state[e_]
                pgt, pgtb = rot_gate.next()
                for blk in range(4):
                    T.op(PE, lambda e, blk=blk, e_=e_, pgt=pgt: e.matmul(
                        pgt[:, blk * 128:(blk + 1) * 128], gatebf[:, blk, e_:e_ + 1].to_broadcast([128, 128]), identb[:],
                        start=True, stop=True), reads=[b_gatebf, b_identb], writes=[pgtb], inc=(blk == 3))
                state[e_] = (wd, tms, pgt, pgtb)

            def h_ops(e_):
                wd, tms, pgt, pgtb = state[e_]
                ht, htb = hT[e_ % 2]
                for fc in range(4):
                    tm, tmb = tms[fc]
                    T.op(DVE, lambda e, tm=tm, pgt=pgt, ht=ht, fc=fc: e.tensor_tensor(out=ht[:, fc, :], in0=tm, in1=pgt[:, :], op=ALU.mult),
                         reads=[tmb, pgtb], writes=[htb])
                state[e_] = (wd, ht, htb)

            def down_phase(e_, st):
                wd, ht, htb = state.pop(e_)
                wdv = wd[0][:].rearrange("p (k c) -> p k c", k=4)
                for dc in range(NCH):
                    py, pyb = rot_y.next()
                    for fc in range(4):
                        T.op(PE, lambda e, py=py, fc=fc, dc=dc: e.matmul(
                            py[:, :], wdv[:, fc, dc * 128:(dc + 1) * 128], ht[:, fc, :], start=(fc == 0), stop=(fc == 3)),
                            reads=[wd[1], htb], writes=[pyb], inc=(fc == 3))
                    if e_ == 0:
                        T.op(DVE, lambda e, py=py, dc=dc: e.scalar_tensor_tensor(
                            out=xres[:, dc, :], in0=xres[:, dc, :], scalar=ALPHA, in1=py[:, :], op0=ALU.mult, op1=ALU.add),
                            reads=[pyb, b_xres[dc]], writes=[b_xres[dc]])
                    else:
                        T.op(DVE, lambda e, py=py, dc=dc: e.tensor_tensor(out=xres[:, dc, :], in0=py[:, :], in1=xres[:, dc, :], op=ALU.add),
                             reads=[pyb, b_xres[dc]], writes=[b_xres[dc]])
                    if st is not None:
                        stats_ops(dc, st)
                        if dc >= 5:
                            stats_mm(dc - 5, st)
                rel(wd)
                if st is not None:
                    for d_ in range(NCH - 5, NCH):
                        stats_mm(d_, st)

            st = (banks[0][0], banks[0][1], banks[1][0], banks[1][1], sqt[0], sqt[1])
            gu_mm(0)
            router()
            gu_mm(1)
            gate_mm(0)
            h_ops(0)
            for e_ in range(NEXP):
                if e_ + 1 < NEXP:
                    gate_mm(e_ + 1)
                down_phase(e_, st if e_ == NEXP - 1 else None)
                if e_ + 1 < NEXP:
                    h_ops(e_ + 1)
                if e_ + 2 < NEXP:
                    gu_mm(e_ + 2)
            return st

        for ti in range(NT):
            def cast_xbf():
                T.op(ACT, lambda e: e.copy(out=xbf[:, 0:4, :], in_=xnext[:, 0:4, :]), reads=[b_xnext], writes=b_xbf[0:4])
                T.op(DVE, lambda e: e.tensor_copy(out=xbf[:, 4:8, :], in_=xnext[:, 4:8, :]), reads=[b_xnext], writes=b_xbf[4:8])

            def load_xres(ti=ti):
                T.op(ACT, lambda e: e.copy(out=xres[:, 0:4, :], in_=xnext[:, 0:4, :]), reads=[b_xnext], writes=b_xres[0:4])
                T.op(DVE, lambda e: e.tensor_copy(out=xres[:, 4:8, :], in_=xnext[:, 4:8, :]), reads=[b_xnext], writes=b_xres[4:8])
                if ti + 1 < NT:
                    T.dma(SP, c_xnext, lambda e, ti=ti: e.dma_start(
                        out=xnext[:], in_=xT[:, (ti + 1) * TT:(ti + 2) * TT].rearrange("(k p) t -> p k t", p=128)), writes=[b_xnext])

            if ti == 0:
                T.dma(SP, c_xnext, lambda e: e.dma_start(
                    out=xnext[:], in_=xT[:, 0:TT].rearrange("(k p) t -> p k t", p=128)), writes=[b_xnext])
                cast_xbf()
            stop = False
            pool_ok[0] = (ti > 0)
            for l in range(depth):
                st = mixer(l, ti, pre_out=(load_xres if l == 0 else None))
                layer_norm(l, 0, st, use_pool=(ti > 0))
                if dbg == (l, 0):
                    stop = True; break
                st = xattn(l, ti)
                layer_norm(l, 1, st, use_pool=(ti > 0))
                if dbg == (l, 1):
                    stop = True; break
                st = moe(l, ti)
                is_final = (l == depth - 1) and dbg is None
                if is_final and ti + 1 < NT:
                    cast_xbf()
                layer_norm(l, 2, st, use_pool=(ti > 0), final=is_final)
                if dbg == (l, 2):
                    stop = True; break
            if stop:
                T.dma(SP, c_xin, lambda e: e.dma_start(out=dbg_out.rearrange("(k p) t -> p k t", p=128), in_=xres[:]),
                      reads=b_xres)
                break
            T.dma(SP, c_xin, lambda e, ti=ti: e.dma_start(
                out=yT[:, ti * TT:(ti + 1) * TT].rearrange("(k p) t -> p k t", p=128), in_=xres[:]), reads=b_xres)
        T.wait_all(SP, all_ctx)
        with nc.Block() as block:
            block.sync(lambda e: T.replay(SP, e))
            block.tensor(lambda e: T.replay(PE, e))
            block.scalar(lambda e: T.replay(ACT, e))
            block.vector(lambda e: T.replay(DVE, e))
            block.gpsimd(lambda e: T.replay(POOL, e))
    nc._mk_stats = dict(nops=T.nops, nwaits=T.nwaits)
    return nc


def _prep_shared(inp):
    f = lambda a: np.ascontiguousarray(np.asarray(a, dtype=np.float32))
    sh = {}
    sh["w_in"] = f(inp["w_in"])
    sh["w_a2"] = f(inp["w_a2"])
    sh["b_a"] = f(inp["b_a"]).reshape(DEPTH, 1, 256)
    sh["gng_t"] = f(np.asarray(inp["gla_norm_g"]).reshape(DEPTH, 4, 128).transpose(0, 2, 1))
    sh["w_sT"] = f(np.asarray(inp["w_s"]).transpose(0, 1, 3, 2))
    sh["b_s"] = f(inp["b_s"]).reshape(DEPTH, 1, 512)
    sh["slng_t"] = f(np.asarray(inp["sgu_ln_g"]).reshape(DEPTH, 4, 128).transpose(0, 2, 1))
    sh["slnb"] = f(inp["sgu_ln_b"]).reshape(DEPTH, 1, 512)
    for k in ["w_out", "wq_x", "wk_x", "wv_x", "wo_x", "w_router", "w_gate", "w_up", "w_down"]:
        sh[k] = f(inp[k])
    sh["b_router"] = f(inp["b_router"]).reshape(1, NEXP)
    sh["lng_t"] = f(np.asarray(inp["ln_g"]).reshape(DEPTH * 3, NCH, 128).transpose(2, 0, 1).reshape(128, DEPTH * 3 * NCH))
    sh["lnb_t"] = f(np.asarray(inp["ln_b"]).reshape(DEPTH * 3, NCH, 128).transpose(2, 0, 1).reshape(128, DEPTH * 3 * NCH))
    return sh


def kernel(**inputs):
    x = np.asarray(inputs["x"], dtype=np.float32)
    mem = np.asarray(inputs["mem"], dtype=np.float32)
    B, S, _ = x.shape
    sh = _prep_shared(inputs)
    nc = build_nc(S)
    in_maps = []
    for b in range(B):
        m = dict(sh)
        m["xT"] = np.ascontiguousarray(x[b].T)
        m["memT"] = np.ascontiguousarray(mem[b].T)
        in_maps.append(m)
    res = run_bass_kernel_spmd(nc, in_maps, core_ids=list(range(B)))
    out = np.empty((B, S, D), dtype=np.float32)
    for b in range(B):
        out[b] = res.results[b]["yT"].T
    return out
```
